# Optimizing a Trainium2 kernel written in Bass

```python
import math
import jax, jax.numpy as jnp
from jax import lax
import numpy as np

D_MODEL = 2048
BATCH = 4
SEQ = 2048
DEPTH = 1

RET_HEAD_DIM = 128
RET_HEADS = (D_MODEL // 2) // RET_HEAD_DIM
RET_WIDTH = RET_HEADS * RET_HEAD_DIM
LRU_WIDTH = D_MODEL - RET_WIDTH
LRU_BLOCKS = 8
LRU_BLOCK_DIM = LRU_WIDTH // LRU_BLOCKS
IN_COLS = 4 * RET_WIDTH + 2 * LRU_WIDTH
CONV_WIDTH = 4
LRU_C = 8.0
CHUNK = 128
ROPE_BASE = 10000.0
N_EXPERTS = 32
TOP_K = 4
D_EXPERT = D_MODEL
SWIGLU_LIMIT = 7.0
SWIGLU_ALPHA = 1.702
PLE_DIM = 256
EXPERT_BLOCK = 128
LN_EPS = 1e-5
DN_ALPHA = (2.0 * DEPTH) ** 0.25
DN_BETA = (8.0 * DEPTH) ** -0.25

kernel_name = 'hymba_retnet_rglru_moe_deepnorm_ple'


def layer_norm(x, w, b):
    x32 = x.astype(jnp.float32)
    mu = jnp.mean(x32, axis=-1, keepdims=True)
    var = jnp.mean(jnp.square(x32 - mu), axis=-1, keepdims=True)
    return (x32 - mu) * lax.rsqrt(var + LN_EPS) * w.astype(jnp.float32) + b.astype(jnp.float32)


def rms_norm(x, w):
    x32 = x.astype(jnp.float32)
    return x32 * lax.rsqrt(jnp.mean(jnp.square(x32), axis=-1, keepdims=True) + LN_EPS) * w.astype(jnp.float32)


def rope(x, pos):
    d = x.shape[-1]
    inv = ROPE_BASE ** (-jnp.arange(0, d, 2, dtype=jnp.float32) / d)
    ang = pos[:, None] * inv[None, :]
    cos = jnp.cos(ang)[:, None, :]
    sin = jnp.sin(ang)[:, None, :]
    x1, x2 = x[..., : d // 2], x[..., d // 2:]
    return jnp.concatenate([x1 * cos - x2 * sin, x1 * sin + x2 * cos], axis=-1)


def retention(q, k, v):
    b, s, h, d = q.shape
    n = s // CHUNK
    log_gamma = jnp.log1p(-jnp.exp2(-5.0 - jnp.arange(h, dtype=jnp.float32)))
    idx = jnp.arange(CHUNK, dtype=jnp.float32)
    diff = idx[:, None] - idx[None, :]
    decay_intra = jnp.where((diff >= 0)[None],
                            jnp.exp(jnp.maximum(diff, 0.0)[None] * log_gamma[:, None, None]), 0.0)
    q_dec = jnp.exp((idx[:, None] + 1.0) * log_gamma[None, :])
    k_dec = jnp.exp((CHUNK - 1.0 - idx)[:, None] * log_gamma[None, :])
    chunk_dec = jnp.exp(CHUNK * log_gamma)
    qc = q.reshape(b, n, CHUNK, h, d)
    kc = k.reshape(b, n, CHUNK, h, d)
    vc = v.reshape(b, n, CHUNK, h, d)
    scores = jnp.einsum('bnihd,bnjhd->bnhij', qc, kc) * decay_intra
    intra = jnp.einsum('bnhij,bnjhd->bnihd', scores, vc)
    kv = jnp.einsum('bnjhd,bnjhe->nbhde', kc * k_dec[:, :, None], vc)

    def step(state, kv_n):
        return chunk_dec[None, :, None, None] * state + kv_n, state

    _, prev = lax.scan(step, jnp.zeros((b, h, d, d), jnp.float32), kv)
    cross = jnp.einsum('bnihd,nbhde->bnihe', qc * q_dec[:, :, None], prev)
    return (intra + cross).reshape(b, s, h, d)


def causal_depthwise_conv(x, w, bias):
    s = x.shape[1]
    xp = jnp.pad(x, ((0, 0), (CONV_WIDTH - 1, 0), (0, 0)))
    out = bias.astype(jnp.float32)
    for j in range(CONV_WIDTH):
        out = out + xp[:, j:j + s] * w[j]
    return out


def rg_lru(x, w_a, b_a, w_x, b_x, lam):
    bsz, s, c = x.shape
    xb = x.reshape(bsz, s, LRU_BLOCKS, LRU_BLOCK_DIM)
    r = jax.nn.sigmoid(jnp.einsum('bshi,hij->bshj', xb, w_a) + b_a).reshape(bsz, s, c)
    gi = jax.nn.sigmoid(jnp.einsum('bshi,hij->bshj', xb, w_x) + b_x).reshape(bsz, s, c)
    log_a = LRU_C * r * jax.nn.log_sigmoid(lam.astype(jnp.float32))
    a = jnp.exp(log_a)
    bx = jnp.sqrt(-jnp.expm1(2.0 * log_a)) * (gi * x)

    def combine(left, right):
        a1, b1 = left
        a2, b2 = right
        return a1 * a2, a2 * b1 + b2

    _, h = lax.associative_scan(combine, (a, bx), axis=1)
    return h


def hybrid_mixer(x, w_in, ret_gn_w, conv_w, conv_b, lru_wa, lru_ba, lru_wx, lru_bx, lru_lam, w_out):
    bsz, s, _ = x.shape
    proj = jnp.einsum('bsd,de->bse', x, w_in).astype(jnp.float32)
    R = RET_WIDTH
    q, k, v, g, xr, yg = jnp.split(proj, [R, 2 * R, 3 * R, 4 * R, 4 * R + LRU_WIDTH], axis=-1)
    pos = jnp.arange(s, dtype=jnp.float32)
    q = rope(q.reshape(bsz, s, RET_HEADS, RET_HEAD_DIM), pos)
    k = rope(k.reshape(bsz, s, RET_HEADS, RET_HEAD_DIM), pos) * (RET_HEAD_DIM ** -0.5)
    ret = retention(q, k, v.reshape(bsz, s, RET_HEADS, RET_HEAD_DIM))
    mu = jnp.mean(ret, axis=-1, keepdims=True)
    var = jnp.mean(jnp.square(ret - mu), axis=-1, keepdims=True)
    ret = ((ret - mu) * lax.rsqrt(var + LN_EPS)).reshape(bsz, s, R) * ret_gn_w.astype(jnp.float32)
    ret_out = jax.nn.silu(g) * ret
    xr = causal_depthwise_conv(xr, conv_w.astype(jnp.float32), conv_b)
    h = rg_lru(xr, lru_wa.astype(jnp.float32), lru_ba.astype(jnp.float32),
               lru_wx.astype(jnp.float32), lru_bx.astype(jnp.float32), lru_lam)
    lru_out = jax.nn.gelu(yg) * h
    mixed = jnp.concatenate([ret_out, lru_out], axis=-1)
    return jnp.einsum('bse,ed->bsd', mixed, w_out.astype(jnp.float32))


def moe(x, w_router, b_router, w_gate, b_gate, w_up, b_up, w_down, b_down):
    bsz, s, d = x.shape
    t = bsz * s
    xf = x.reshape(t, d).astype(jnp.float32)
    logits = xf @ w_router.astype(jnp.float32) + b_router.astype(jnp.float32)
    top_logit, top_e = lax.top_k(logits, TOP_K)
    gates = jax.nn.softmax(top_logit, axis=-1)
    n_assign = t * TOP_K
    flat_e = top_e.reshape(-1).astype(jnp.int32)
    flat_tok = jnp.arange(n_assign, dtype=jnp.int32) // TOP_K
    flat_gate = gates.reshape(-1)
    order = jnp.argsort(flat_e)
    se, stok, sgate = flat_e[order], flat_tok[order], flat_gate[order]
    counts = jnp.zeros((N_EXPERTS,), jnp.int32).at[flat_e].add(1)
    starts = jnp.cumsum(counts) - counts
    padded = (counts + EXPERT_BLOCK - 1) // EXPERT_BLOCK * EXPERT_BLOCK
    pad_ends = jnp.cumsum(padded)
    pad_starts = pad_ends - padded
    dest = pad_starts[se] + (jnp.arange(n_assign, dtype=jnp.int32) - starts[se])
    n_pad = n_assign + N_EXPERTS * EXPERT_BLOCK
    n_blocks = n_pad // EXPERT_BLOCK
    buf_tok = jnp.full((n_pad,), t, jnp.int32).at[dest].set(stok)
    buf_gate = jnp.zeros((n_pad,), jnp.float32).at[dest].set(sgate)
    block_start = jnp.arange(n_blocks, dtype=jnp.int32) * EXPERT_BLOCK
    block_e = jnp.minimum(jnp.searchsorted(pad_ends, block_start, side='right'), N_EXPERTS - 1)
    xpad = jnp.concatenate([xf, jnp.zeros((1, d), jnp.float32)], axis=0)
    xb = xpad[buf_tok].reshape(n_blocks, EXPERT_BLOCK, d)

    def expert_block(args):
        xblk, e = args
        gt = xblk @ w_gate[e].astype(jnp.float32) + b_gate[e].astype(jnp.float32)
        up = xblk @ w_up[e].astype(jnp.float32) + b_up[e].astype(jnp.float32)
        gt = jnp.minimum(gt, SWIGLU_LIMIT)
        up = jnp.clip(up, -SWIGLU_LIMIT, SWIGLU_LIMIT)
        hid = (up + 1.0) * gt * jax.nn.sigmoid(SWIGLU_ALPHA * gt)
        return hid @ w_down[e].astype(jnp.float32) + b_down[e].astype(jnp.float32)

    yb = lax.map(expert_block, (xb, block_e)).reshape(n_pad, d)
    out = jnp.zeros((t + 1, d), jnp.float32).at[buf_tok].add(yb * buf_gate[:, None])
    return out[:t].reshape(bsz, s, d)


def setup_inputs(seed: int = 0) -> dict:
    key = jax.random.key(seed)
    ks = jax.random.split(key, 32)
    f32 = jnp.float32
    nrm = lambda k, shape, scale: jax.random.normal(k, shape, f32) * scale
    col_scale = jnp.concatenate([jnp.ones((2 * RET_WIDTH,), f32),
                                 jnp.full((RET_WIDTH,), DN_BETA, f32),
                                 jnp.ones((RET_WIDTH + 2 * LRU_WIDTH,), f32)])
    lam_u = jax.random.uniform(ks[9], (DEPTH, LRU_WIDTH), f32, 0.9, 0.999)
    lam_a = lam_u ** (1.0 / LRU_C)
    return {
        'x': nrm(ks[0], (BATCH, SEQ, D_MODEL), 1.0),
        'p': nrm(ks[1], (DEPTH, BATCH, SEQ, PLE_DIM), 1.0),
        'w_in': nrm(ks[2], (DEPTH, D_MODEL, IN_COLS), D_MODEL ** -0.5) * col_scale,
        'ret_gn_w': 1.0 + nrm(ks[3], (DEPTH, RET_WIDTH), 0.02),
        'conv_w': nrm(ks[4], (DEPTH, CONV_WIDTH, LRU_WIDTH), CONV_WIDTH ** -0.5),
        'conv_b': nrm(ks[5], (DEPTH, LRU_WIDTH), 0.01),
        'lru_wa': nrm(ks[6], (DEPTH, LRU_BLOCKS, LRU_BLOCK_DIM, LRU_BLOCK_DIM), LRU_BLOCK_DIM ** -0.5),
        'lru_ba': nrm(ks[7], (DEPTH, LRU_BLOCKS, LRU_BLOCK_DIM), 0.01),
        'lru_wx': nrm(ks[8], (DEPTH, LRU_BLOCKS, LRU_BLOCK_DIM, LRU_BLOCK_DIM), LRU_BLOCK_DIM ** -0.5),
        'lru_bx': nrm(ks[10], (DEPTH, LRU_BLOCKS, LRU_BLOCK_DIM), 0.01),
        'lru_lam': jnp.log(lam_a) - jnp.log1p(-lam_a),
        'w_out': nrm(ks[11], (DEPTH, D_MODEL, D_MODEL), D_MODEL ** -0.5 * DN_BETA),
        'ln1_w': 1.0 + nrm(ks[12], (DEPTH, D_MODEL), 0.02),
        'ln1_b': nrm(ks[13], (DEPTH, D_MODEL), 0.01),
        'w_router': nrm(ks[14], (DEPTH, D_MODEL, N_EXPERTS), D_MODEL ** -0.5),
        'b_router': nrm(ks[15], (DEPTH, N_EXPERTS), 0.01),
        'w_gate': nrm(ks[16], (DEPTH, N_EXPERTS, D_MODEL, D_EXPERT), D_MODEL ** -0.5),
        'b_gate': nrm(ks[17], (DEPTH, N_EXPERTS, D_EXPERT), 0.01),
        'w_up': nrm(ks[18], (DEPTH, N_EXPERTS, D_MODEL, D_EXPERT), D_MODEL ** -0.5),
        'b_up': nrm(ks[19], (DEPTH, N_EXPERTS, D_EXPERT), 0.01),
        'w_down': nrm(ks[20], (DEPTH, N_EXPERTS, D_EXPERT, D_MODEL), D_EXPERT ** -0.5 * DN_BETA),
        'b_down': nrm(ks[21], (DEPTH, N_EXPERTS, D_MODEL), 0.01),
        'ln2_w': 1.0 + nrm(ks[22], (DEPTH, D_MODEL), 0.02),
        'ln2_b': nrm(ks[23], (DEPTH, D_MODEL), 0.01),
        'w_ple_proj': nrm(ks[24], (DEPTH, PLE_DIM, D_MODEL), PLE_DIM ** -0.5 * DN_BETA),
        'ple_norm_w': 1.0 + nrm(ks[25], (DEPTH, D_MODEL), 0.02),
        'w_ple_gate': nrm(ks[26], (DEPTH, D_MODEL, D_MODEL), D_MODEL ** -0.5),
    }


def reference(x, p, w_in, ret_gn_w, conv_w, conv_b, lru_wa, lru_ba, lru_wx, lru_bx, lru_lam, w_out,
              ln1_w, ln1_b, w_router, b_router, w_gate, b_gate, w_up, b_up, w_down, b_down,
              ln2_w, ln2_b, w_ple_proj, ple_norm_w, w_ple_gate):
    h = x.astype(jnp.float32)
    for i in range(DEPTH):
        m = hybrid_mixer(h, w_in[i], ret_gn_w[i], conv_w[i], conv_b[i], lru_wa[i], lru_ba[i],
                         lru_wx[i], lru_bx[i], lru_lam[i], w_out[i])
        h = layer_norm(DN_ALPHA * h + m, ln1_w[i], ln1_b[i])
        f = moe(h, w_router[i], b_router[i], w_gate[i], b_gate[i], w_up[i], b_up[i], w_down[i], b_down[i])
        h = layer_norm(DN_ALPHA * h + f, ln2_w[i], ln2_b[i])
        e = rms_norm(jnp.einsum('bsp,pd->bsd', p[i].astype(jnp.float32), w_ple_proj[i].astype(jnp.float32)), ple_norm_w[i])
        gate = jax.nn.sigmoid(jnp.einsum('bsd,de->bse', h, w_ple_gate[i].astype(jnp.float32)))
        h = h + gate * e
    return h.astype(x.dtype)
```

```python
import math
from contextlib import ExitStack

import numpy as np
import ml_dtypes

import concourse.bass as bass
import concourse.mybir as mybir
from concourse.bass_utils import run_bass_kernel_spmd

F32 = mybir.dt.float32
BF16 = mybir.dt.bfloat16
ALU = mybir.AluOpType
AF = mybir.ActivationFunctionType

NCORES = 8
DM = 2048
T = 1024
NTC = 8
NDC = 16
NE = 32
CAP = 256
LN_EPS = 1e-5
DN_ALPHA = 2.0 ** 0.25
NSLOT = 4
EG = 8


class Sched:
    ENG = ("pe", "act", "dve", "pool", "sp")

    def __init__(self, nc, es):
        self.nc = nc
        self.es = es
        self.sem = {e: es.enter_context(nc.semaphore("s_" + e)) for e in self.ENG}
        self.cnt = {e: 0 for e in self.ENG}
        self.waited = {e: {} for e in self.ENG}
        self.ops = {e: [] for e in self.ENG}
        self.last_w = {}
        self.readers = {}
        self.dsem = {}
        self.dcnt = {}

    def _deps(self, eng, reads, writes):
        deps = {}

        def add(d):
            if d is None:
                return
            s, v = d
            if deps.get(id(s), (None, 0))[1] < v:
                deps[id(s)] = (s, v)

        for k in reads:
            add(self.last_w.get(k))
        for k in writes:
            add(self.last_w.get(k))
            for d in self.readers.get(k, {}).values():
                add(d)
        waits = []
        for sid, (s, v) in deps.items():
            if self.waited[eng].get(sid, 0) < v:
                self.waited[eng][sid] = v
                waits.append((s, v))
        return waits

    def _mark(self, me, reads, writes):
        for k in reads:
            self.readers.setdefault(k, {})[id(me[0])] = me
        for k in writes:
            self.last_w[k] = me
            self.readers[k] = {}

    def op(self, eng, fn, reads=(), writes=()):
        waits = self._deps(eng, reads, writes)
        self.cnt[eng] += 1
        me = (self.sem[eng], self.cnt[eng])
        self.ops[eng].append((waits, fn, self.sem[eng], 1))
        self._mark(me, reads, writes)

    def dma(self, eng, fn, ndma, semname, reads=(), writes=()):
        if semname not in self.dsem:
            self.dsem[semname] = self.es.enter_context(self.nc.semaphore("d_" + semname))
            self.dcnt[semname] = 0
        waits = self._deps(eng, reads, writes)
        self.dcnt[semname] += 16 * ndma
        me = (self.dsem[semname], self.dcnt[semname])
        self.ops[eng].append((waits, fn, self.dsem[semname], None))
        self._mark(me, reads, writes)

    def barrier(self):
        allsem = [(self.sem[e], self.cnt[e]) for e in self.ENG if self.cnt[e] > 0]
        allsem += [(self.dsem[k], self.dcnt[k]) for k in self.dsem if self.dcnt[k] > 0]
        for e in self.ENG:
            waits = []
            for s, v in allsem:
                if self.waited[e].get(id(s), 0) < v:
                    self.waited[e][id(s)] = v
                    waits.append((s, v))
            if waits:
                self.ops[e].append((waits, None, None, 0))

    def flush(self):
        with self.nc.Block() as block:
            decos = {"pe": block.tensor, "act": block.scalar, "dve": block.vector,
                     "pool": block.gpsimd, "sp": block.sync}
            for e in self.ENG:
                ops = self.ops[e]
                if not ops:
                    continue

                def body(engine, ops=ops):
                    for waits, fn, sem, inc in ops:
                        for s, v in waits:
                            engine.wait_ge(s, v)
                        if fn is None:
                            continue
                        if inc is None:
                            fn(engine, sem)
                        else:
                            fn(engine).then_inc(sem, inc)

                decos[e](body)
                self.ops[e] = []


class _Stop(Exception):
    pass


def build(stop_after=None, n_exp_run=None, n_exp_decl=None):
    nc = bass.Bass("TRN2", target_bir_lowering=False)
    es = ExitStack()
    try:
        _body(nc, es, stop_after, n_exp_run, n_exp_decl)
    except _Stop:
        pass
    return nc


def _body(nc, es, stop_after, n_exp_run, n_exp_decl=None):
    S = Sched(nc, es)

    def din(name, shape, dt=F32):
        return nc.dram_tensor(name, list(shape), dt, kind="ExternalInput").ap()

    xT_own = din("xT_own", [DM, T])
    xT_pre = din("xT_pre", [DM, T])
    x_own = din("x_own", [T, DM])
    pT = din("pT", [256, T])
    w_in = din("w_in", [DM, 6144])
    w_out = din("w_out", [DM, DM])
    n_exp = NE if stop_after in (None, "moe", "ln2", "s5pre", "ple") else 0
    if n_exp_run is not None:
        n_exp = n_exp_run
    NEW = max(n_exp, 1) if n_exp_decl is None else n_exp_decl
    NG = (NEW + EG - 1) // EG
    w_gate = [din("w_gate_%d" % g, [min(EG, NEW - g * EG), DM, DM]) for g in range(NG)]
    w_up = [din("w_up_%d" % g, [min(EG, NEW - g * EG), DM, DM]) for g in range(NG)]
    w_down = [din("w_down_%d" % g, [min(EG, NEW - g * EG), DM, DM]) for g in range(NG)]
    w_pg = din("w_ple_gate", [DM, DM])
    w_pp = din("w_ple_proj", [256, DM])
    w_r = din("w_router", [DM, NE])
    lru_wa = din("lru_wa", [8, 128, 128])
    lru_wx = din("lru_wx", [8, 128, 128])
    lruvec = din("lruvec", [128, 8, 8])
    bgu = din("bgu", [128, 2, NE, 16])
    b_down = din("b_down", [NE, DM])
    rep_ln1w = din("rep_ln1w", [128, DM])
    rep_ln1b = din("rep_ln1b", [128, DM])
    rep_ln2w = din("rep_ln2w", [128, DM])
    rep_ln2b = din("rep_ln2b", [128, DM])
    rep_plew = din("rep_plew", [128, DM])
    rep_gnw = din("rep_gnw", [128, 1024])
    rep_br = din("rep_br", [128, NE])
    pf_in = din("pf", [128, 1])
    rope_own = din("rope_own", [128, 4, NTC, 64])
    rope_pre = din("rope_pre", [128, 2, NTC, 64])
    decT = din("decT", [128, 8, 128])
    kdec = din("kdec", [128, 8])
    qdiag = din("qdiag", [128, 9, 128], BF16)
    ident_f = din("ident_f", [128, 128])
    tri = din("tri", [128, 2, 128], BF16)
    iota_c = din("iota_c", [128, CAP])
    iota_p = din("iota_p", [128, 2])
    pidx_in = din("pidx", [NE, 128])

    out = nc.dram_tensor("out", [T, DM], F32, kind="ExternalOutput").ap()
    dbg = None
    if stop_after is not None:
        dbg = nc.dram_tensor("dbg", [128, 8192], F32, kind="ExternalOutput").ap()

    def sb(name, shape, dt=F32, stack=None):
        return (stack or es).enter_context(nc.sbuf_tensor(name, list(shape), dt))

    ps = [es.enter_context(nc.psum_tensor("ps%d" % i, [128, 512], F32)) for i in range(8)]
    wslot = [sb("wslot%d" % i, [128, NDC, 512], BF16) for i in range(NSLOT)]
    c_identf = sb("c_identf", [128, 128])
    c_qdiag = sb("c_qdiag", [128, 9, 128], BF16)

    piece_no = [0]

    def load_piece(src_ap):
        i = piece_no[0] % NSLOT
        piece_no[0] += 1
        key = "wslot%d" % i
        dst = wslot[i]

        def fn(eng, sem, src_ap=src_ap, dst=dst):
            v = src_ap.rearrange("(c p) f -> p c f", p=128)
            for hh in range(2):
                eng.dma_start(out=dst[:, hh * 8:(hh + 1) * 8, :], in_=v[:, hh * 8:(hh + 1) * 8, :]).then_inc(sem, 16)

        S.dma("pool", fn, 2, key, reads=(), writes=(key,))
        return i

    consts = []

    def cload(dst_t, src_ap, key, eng="sp"):
        def fn(engine, sem, dst_t=dst_t, src_ap=src_ap):
            engine.dma_start(out=dst_t[:], in_=src_ap).then_inc(sem, 16)
        S.dma(eng, fn, 1, "c_" + eng, writes=(key,))
        consts.append(key)

    def cfinal():
        for eng in ("sp", "pool"):
            nm = "c_" + eng
            if nm in S.dsem:
                for k in consts:
                    if S.last_w[k][0] is S.dsem[nm]:
                        S.last_w[k] = (S.dsem[nm], S.dcnt[nm])
        consts.clear()

    cload(c_identf, ident_f, "c_identf")
    cload(c_qdiag, qdiag, "c_qdiag")
    c_eps = sb("c_eps", [128, 1])

    def f_eps(pool):
        return pool.memset(c_eps[:, :], LN_EPS)
    S.op("pool", f_eps, writes=("c_eps",))
    c_dummy = sb("c_dummy", [128, 8], BF16)
    cload(c_dummy, kdec, "c_dummy", eng="pool")
    cfinal()
    S.flush()

    def mm(out_ap, pairs, reads, writes):
        def fn(pe, out_ap=out_ap, pairs=pairs):
            n = len(pairs)
            ins = None
            for i, (l, r) in enumerate(pairs):
                ins = pe.matmul(out_ap, l, r, start=(i == 0), stop=(i == n - 1))
            return ins
        S.op("pe", fn, reads=reads, writes=writes)

    def dump(ap_list, keys):
        off = 0
        for ap, n in ap_list:
            def fn(engine, sem, ap=ap, off=off, n=n):
                engine.dma_start(out=dbg[:, off:off + n], in_=ap).then_inc(sem, 16)
            S.dma("sp", fn, 1, "dbg", reads=keys)
            off += n
        S.barrier()
        S.flush()
        raise _Stop()

    arA = sb("arA", [128, 16384])
    arB = sb("arB", [128, 8192])
    xTo = arA[:, 0:8192].bitcast(BF16).rearrange("p (c t) -> p c t", c=NDC)
    xTp = arA[:, 8192:16384].bitcast(BF16).rearrange("p (c t) -> p c t", c=NDC)
    mixL = arB[:, 0:4096].bitcast(BF16).rearrange("p (c t) -> p c t", c=8)
    mixR = arB[:, 4096:8192].bitcast(BF16).rearrange("p (c t) -> p c t", c=8)
    acc = arA[:, :].rearrange("p (c d) -> p c d", c=NTC)
    x1bf = arB[:, :].bitcast(BF16).rearrange("p (c d) -> p c d", c=NTC)
    s12 = ExitStack()
    c_lruvec = sb("c_lruvec", [128, 8, 8], F32, s12)
    c_pf = sb("c_pf", [128, 1], F32, s12)
    state_f = sb("state_f", [128, 8, 128], F32, s12)
    state_b = sb("state_b", [128, 8, 128], BF16, s12)
    cload(c_lruvec, lruvec, "c_lruvec")
    cload(c_pf, pf_in, "c_pf")

    def load_xT(dst, src, key):
        def fn(eng, sem):
            v = src.rearrange("(c p) t -> p c t", p=128)
            for q4 in range(4):
                eng.dma_start(out=dst[:, q4 * 4:(q4 + 1) * 4, :], in_=v[:, q4 * 4:(q4 + 1) * 4, :]).then_inc(sem, 16)
        S.dma("pool", fn, 4, key, writes=(key,))

    with ExitStack() as s1:
        load_xT(xTp, xT_pre, "xTp")
        load_xT(xTo, xT_own, "xTo")
        c_wa = sb("c_wa", [128, 8, 128], BF16, s1)
        c_wx = sb("c_wx", [128, 8, 128], BF16, s1)
        cload(c_wa, lru_wa.rearrange("h i j -> i h j"), "c_wa", eng="pool")
        cload(c_wx, lru_wx.rearrange("h i j -> i h j"), "c_wx", eng="pool")
        cfinal()
        c8 = sb("c8", [128, 8, 2], F32, s1)

        def f_sig(act):
            return act.activation(out=c8[:, :, 0], in_=c_lruvec[:, :, 7], func=AF.Sigmoid)
        S.op("act", f_sig, reads=("c_lruvec",), writes=("c8a",))

        def f_ln(act):
            return act.activation(out=c8[:, :, 1], in_=c8[:, :, 0], func=AF.Ln)
        S.op("act", f_ln, reads=("c8a",), writes=("c8b",))

        def f_c8(dve):
            return dve.tensor_scalar(out=c8[:, :, 0], in0=c8[:, :, 1], scalar1=8.0, scalar2=None, op0=ALU.mult)
        S.op("dve", f_c8, reads=("c8b",), writes=("c8a",))

        def f_c16(dve):
            return dve.tensor_scalar(out=c8[:, :, 1], in0=c8[:, :, 0], scalar1=2.0, scalar2=None, op0=ALU.mult)
        S.op("dve", f_c16, reads=("c8a",), writes=("c8",))

        with ExitStack() as s1a:
            X = sb("lX", [128, 3 + 2 * T], F32, s1a)
            C = sb("lC", [128, 2 * T], F32, s1a)
            R = arB[:, 4096:6144]
            CB = arB[:, 6144:7168].bitcast(BF16)
            G = sb("lG", [128, 2 * T], F32, s1a)
            hb0 = sb("lh0", [128, 1], F32, s1a)

            def f_zero(pool):
                return pool.memset(X[:, 0:3], 0.0)
            S.op("pool", f_zero, writes=("X",))

            for hb in range(8):
                cg = 8 + hb // 4
                cc = hb % 4
                if cc == 0:
                    sl_x = load_piece(w_in[:, cg * 512:(cg + 1) * 512])
                    sl_y = load_piece(w_in[:, (cg + 2) * 512:(cg + 3) * 512])
                kx, ky = "wslot%d" % sl_x, "wslot%d" % sl_y
                for tt in range(4):
                    src, skey = (xTp, "xTp") if tt < 2 else (xTo, "xTo")
                    t0 = (tt % 2) * 512
                    pk = "ps%d" % (tt % 2)
                    mm(ps[tt % 2][:, :],
                       [(wslot[sl_x][:, dc, cc * 128:(cc + 1) * 128], src[:, dc, t0:t0 + 512]) for dc in range(NDC)],
                       reads=(kx, skey), writes=(pk,))

                    def f_ev(act, tt=tt):
                        return act.copy(out=X[:, 3 + tt * 512:3 + (tt + 1) * 512], in_=ps[tt % 2][:, :])
                    S.op("act", f_ev, reads=(pk,), writes=("X",))
                lv = c_lruvec

                def f_c0(dve, hb=hb):
                    return dve.tensor_scalar(out=C[:, :], in0=X[:, 0:2 * T], scalar1=lv[:, hb, 0:1], scalar2=lv[:, hb, 4:5],
                                             op0=ALU.mult, op1=ALU.add)
                S.op("dve", f_c0, reads=("X", "c_lruvec"), writes=("C",))
                for j in range(1, 4):
                    def f_cj(dve, hb=hb, j=j):
                        return dve.scalar_tensor_tensor(out=C[:, :], in0=X[:, j:j + 2 * T], scalar=lv[:, hb, j:j + 1], in1=C[:, :],
                                                        op0=ALU.mult, op1=ALU.add)
                    S.op("dve", f_cj, reads=("X", "C"), writes=("C",))

                def f_cb(act):
                    return act.copy(out=CB[:, :], in_=C[:, :])
                S.op("act", f_cb, reads=("C",), writes=("CB",))
                for gi_, (wt, wk, dstb, dk, bcol) in enumerate(((c_wa, "c_wa", R, "R", 5), (c_wx, "c_wx", G, "G", 6))):
                    for tt in range(4):
                        pk = "ps%d" % (2 + (gi_ * 4 + tt) % 4)
                        pst = ps[2 + (gi_ * 4 + tt) % 4]
                        mm(pst[:, :], [(wt[:, hb, :], CB[:, tt * 512:(tt + 1) * 512])], reads=(wk, "CB"), writes=(pk,))

                        def f_sg(act, pst=pst, dstb=dstb, tt=tt, hb=hb, bcol=bcol):
                            return act.activation(out=dstb[:, tt * 512:(tt + 1) * 512], in_=pst[:, :], func=AF.Sigmoid,
                                                  bias=lv[:, hb, bcol:bcol + 1], scale=1.0)
                        S.op("act", f_sg, reads=(pk, "c_lruvec"), writes=(dk,))

                def f_a(act, hb=hb):
                    return act.activation(out=X[:, 3:3 + 2 * T], in_=R[:, :], func=AF.Exp, scale=c8[:, hb, 0:1])
                S.op("act", f_a, reads=("R", "c8"), writes=("X",))

                def f_a2(act, hb=hb):
                    return act.activation(out=R[:, :], in_=R[:, :], func=AF.Exp, scale=c8[:, hb, 1:2])
                S.op("act", f_a2, reads=("R", "c8"), writes=("R",))

                def f_om(dve):
                    return dve.tensor_scalar(out=R[:, :], in0=R[:, :], scalar1=1.0, scalar2=-1.0, op0=ALU.min, op1=ALU.mult)
                S.op("dve", f_om, reads=("R",), writes=("R",))

                def f_sq(act):
                    return act.activation(out=R[:, :], in_=R[:, :], func=AF.Sqrt, bias=1.0, scale=1.0)
                S.op("act", f_sq, reads=("R",), writes=("R",))

                def f_g2(pool):
                    return pool.tensor_tensor(out=G[:, :], in0=G[:, :], in1=C[:, :], op=ALU.mult)
                S.op("pool", f_g2, reads=("G", "C"), writes=("G",))

                def f_bx(dve):
                    return dve.tensor_tensor(out=G[:, :], in0=G[:, :], in1=R[:, :], op=ALU.mult)
                S.op("dve", f_bx, reads=("G", "R"), writes=("G",))

                def f_s1(dve):
                    return dve.tensor_tensor_scan(out=C[:, 0:T], data0=X[:, 3:3 + T], data1=G[:, 0:T], initial=0.0,
                                                  op0=ALU.mult, op1=ALU.add)
                S.op("dve", f_s1, reads=("X", "G", "C"), writes=("C",))

                def f_h0(dve):
                    return dve.tensor_scalar(out=hb0[:, :], in0=C[:, T - 1:T], scalar1=c_pf[:, 0:1], scalar2=None, op0=ALU.mult)
                S.op("dve", f_h0, reads=("C", "c_pf"), writes=("hb0",))

                def f_s2(dve):
                    return dve.tensor_tensor_scan(out=C[:, T:2 * T], data0=X[:, 3 + T:3 + 2 * T], data1=G[:, T:2 * T],
                                                  initial=hb0[:, 0:1], op0=ALU.mult, op1=ALU.add)
                S.op("dve", f_s2, reads=("X", "G", "C", "hb0"), writes=("C",))

                for tt in range(2):
                    pk = "ps%d" % (6 + tt)
                    mm(ps[6 + tt][:, :],
                       [(wslot[sl_y][:, dc, cc * 128:(cc + 1) * 128], xTo[:, dc, tt * 512:(tt + 1) * 512]) for dc in range(NDC)],
                       reads=(ky, "xTo"), writes=(pk,))
                    ysl = slice(tt * 512, (tt + 1) * 512)

                    def f_y(act, tt=tt, ysl=ysl):
                        return act.copy(out=R[:, ysl], in_=ps[6 + tt][:, :])
                    S.op("act", f_y, reads=(pk,), writes=("R",))

                    def f_y2(act, tt=tt, ysl=ysl):
                        return act.activation(out=G[:, ysl], in_=ps[6 + tt][:, :], func=AF.Square)
                    S.op("act", f_y2, reads=(pk,), writes=("G",))
                ysl = slice(0, T)

                def f_y3(dve):
                    return dve.scalar_tensor_tensor(out=G[:, ysl], in0=G[:, ysl], scalar=0.044715, in1=R[:, ysl],
                                                    op0=ALU.mult, op1=ALU.mult)
                S.op("dve", f_y3, reads=("G", "R"), writes=("G",))

                def f_y4(pool):
                    return pool.tensor_tensor(out=G[:, ysl], in0=G[:, ysl], in1=R[:, ysl], op=ALU.add)
                S.op("pool", f_y4, reads=("G", "R"), writes=("G",))

                def f_y5(act):
                    return act.activation(out=G[:, ysl], in_=G[:, ysl], func=AF.Sigmoid, scale=1.5957691216057308)
                S.op("act", f_y5, reads=("G",), writes=("G",))

                def f_y6(pool):
                    return pool.tensor_tensor(out=G[:, ysl], in0=G[:, ysl], in1=R[:, ysl], op=ALU.mult)
                S.op("pool", f_y6, reads=("G", "R"), writes=("G",))

                def f_y7(dve, hb=hb):
                    return dve.tensor_tensor(out=mixL[:, hb, :], in0=G[:, ysl], in1=C[:, T:2 * T], op=ALU.mult)
                S.op("dve", f_y7, reads=("G", "C"), writes=("mixL",))

            if stop_after == "lru":
                def f_d(dve):
                    return dve.tensor_copy(out=R[:, 0:T], in_=mixL[:, 7, :])
                S.op("dve", f_d, reads=("mixL",), writes=("R",))

                def f_d2(dve):
                    return dve.tensor_copy(out=R[:, T:2 * T], in_=mixL[:, 0, :])
                S.op("dve", f_d2, reads=("mixL",), writes=("R",))
                return dump([(R[:, 0:2 * T], 2 * T), (C[:, :], 2 * T)], ("R", "C"))
            S.barrier()
            S.flush()

        _, chunk_dec = _consts()
        with ExitStack() as s1b:
            c_ropep = sb("c_ropep", [128, 2, NTC, 64], F32, s1b)
            c_kdec = sb("c_kdec", [128, 8], F32, s1b)
            cload(c_ropep, rope_pre, "c_ropep")
            cload(c_kdec, kdec, "c_kdec")
            cfinal()
            k_p = arB[:, 4096:8192].bitcast(BF16).rearrange("p (c t) -> p c t", c=NTC)
            vd_p = sb("vd_p", [128, NTC, 1024], BF16, s1b)
            rt = [sb("rt%d" % i, [128, 4, 64], F32, s1b) for i in range(4)]

            def f_z(pool):
                return pool.memset(state_f[:, :, :], 0.0)
            S.op("pool", f_z, writes=tuple("st%d" % h for h in range(8)))

            def f_zb(pool):
                return pool.memset(state_b[:, :, :], 0.0)
            S.op("pool", f_zb, writes=tuple("sb%d" % h for h in range(8)))

            def rope_evac(pst, pk, cos_ap, sin_ap, dst_ap, dkey):
                pv = pst[:, :].rearrange("p (h two d) -> p h two d", h=4, two=2)
                dv = dst_ap.rearrange("p (h two d) -> p h two d", h=4, two=2)
                cb = cos_ap.broadcast_to([128, 4, 64])
                sn = sin_ap.broadcast_to([128, 4, 64])
                x1, x2 = pv[:, :, 0, :], pv[:, :, 1, :]
                for (ta, a, ca) in ((0, x1, cb), (1, x2, sn), (2, x1, sn), (3, x2, cb)):
                    def f(dve, ta=ta, a=a, ca=ca):
                        return dve.tensor_tensor(out=rt[ta][:, :, :], in0=a, in1=ca, op=ALU.mult)
                    S.op("dve", f, reads=(pk, "c_rope"), writes=("rt%d" % ta,))

                def f1(pool):
                    return pool.tensor_tensor(out=dv[:, :, 0, :], in0=rt[0][:, :, :], in1=rt[1][:, :, :], op=ALU.subtract)
                S.op("pool", f1, reads=("rt0", "rt1"), writes=(dkey,))

                def f2(pool):
                    return pool.tensor_tensor(out=dv[:, :, 1, :], in0=rt[2][:, :, :], in1=rt[3][:, :, :], op=ALU.add)
                S.op("pool", f2, reads=("rt2", "rt3"), writes=(dkey,))

            S.last_w["c_rope"] = S.last_w["c_ropep"]
            for half in range(2):
                slk = load_piece(w_in[:, (2 + half) * 512:(3 + half) * 512])
                slv = load_piece(w_in[:, (4 + half) * 512:(5 + half) * 512])
                for tc in range(NTC):
                    pk = "ps%d" % (tc % 2)
                    mm(ps[tc % 2][:, :], [(xTp[:, dc, tc * 128:(tc + 1) * 128], wslot[slk][:, dc, :]) for dc in range(NDC)],
                       reads=("xTp", "wslot%d" % slk), writes=(pk,))
                    rope_evac(ps[tc % 2], pk, c_ropep[:, 0, tc:tc + 1, :], c_ropep[:, 1, tc:tc + 1, :],
                              k_p[:, tc, half * 512:(half + 1) * 512], "k_p")
                for tc in range(NTC):
                    pk = "ps%d" % (2 + tc % 2)
                    pst = ps[2 + tc % 2]
                    mm(pst[:, :], [(xTp[:, dc, tc * 128:(tc + 1) * 128], wslot[slv][:, dc, :]) for dc in range(NDC)],
                       reads=("xTp", "wslot%d" % slv), writes=(pk,))
                    for h4 in range(4):
                        def f(act, pst=pst, tc=tc, h4=h4, half=half):
                            h = half * 4 + h4
                            return act.activation(out=vd_p[:, tc, h * 128:(h + 1) * 128], in_=pst[:, h4 * 128:(h4 + 1) * 128],
                                                  func=AF.Copy, scale=c_kdec[:, h:h + 1])
                        S.op("act", f, reads=(pk, "c_kdec"), writes=("vd_p",))
            for n in range(NTC):
                for h in range(8):
                    pk = "ps%d" % (4 + h % 4)
                    pst = ps[4 + h % 4]
                    mm(pst[:, 0:128], [(k_p[:, n, h * 128:(h + 1) * 128], vd_p[:, n, h * 128:(h + 1) * 128])],
                       reads=("k_p", "vd_p"), writes=(pk,))

                    def f(dve, pst=pst, h=h):
                        return dve.scalar_tensor_tensor(out=state_f[:, h, :], in0=state_f[:, h, :], scalar=float(chunk_dec[h]),
                                                        in1=pst[:, 0:128], op0=ALU.mult, op1=ALU.add)
                    S.op("dve", f, reads=(pk, "st%d" % h), writes=("st%d" % h,))
            for h in range(8):
                def f(act, h=h):
                    return act.copy(out=state_b[:, h, :], in_=state_f[:, h, :])
                S.op("act", f, reads=("st%d" % h,), writes=("sb%d" % h,))
            if stop_after == "pre":
                return dump([(state_f[:, :, :].rearrange("p h e -> p (h e)"), 1024)], tuple("st%d" % h for h in range(8)))
            S.barrier()
            S.flush()

    with ExitStack() as s2:
        c_rope = sb("c_rope", [128, 4, NTC, 64], F32, s2)
        c_kdec = sb("c_kdec2", [128, 8], F32, s2)
        c_decT = sb("c_decT", [128, 8, 128], F32, s2)
        c_gnw = sb("c_gnw", [128, 1024], F32, s2)
        cload(c_rope, rope_own, "c_rope")
        cload(c_kdec, kdec, "c_kdec")
        cload(c_decT, decT, "c_decT")
        cload(c_gnw, rep_gnw, "c_gnw")
        cfinal()
        qkvv = arA[:, 8192:16384].bitcast(BF16).rearrange("p (w c t) -> p w c t", w=4, c=NTC)
        q_o, k_o, v_o, vd_o = qkvv[:, 0], qkvv[:, 1], qkvv[:, 2], qkvv[:, 3]
        sg_o = sb("sg_o", [128, NTC, 512], BF16, s2)
        rt = [sb("rt2_%d" % i, [128, 4, 64], F32, s2) for i in range(4)]
        trb = [sb("trb%d" % i, [128, 384], BF16, s2) for i in range(2)]
        sdt = [sb("sdt%d" % i, [128, 128], BF16, s2) for i in range(2)]
        gst = sb("gst", [128, 4, 6], F32, s2)
        gmv = sb("gmv", [128, 4, 2], F32, s2)
        grs = sb("grs", [128, 4], F32, s2)
        rn = sb("rn", [128, 512], F32, s2)
        mtok = sb("mtok", [128, 512], BF16, s2)

        def rope_evac2(pst, pk, cos_ap, sin_ap, dst_ap, dkey):
            pv = pst[:, :].rearrange("p (h two d) -> p h two d", h=4, two=2)
            dv = dst_ap.rearrange("p (h two d) -> p h two d", h=4, two=2)
            cb = cos_ap.broadcast_to([128, 4, 64])
            sn = sin_ap.broadcast_to([128, 4, 64])
            x1, x2 = pv[:, :, 0, :], pv[:, :, 1, :]
            for (ta, a, ca) in ((0, x1, cb), (1, x2, sn), (2, x1, sn), (3, x2, cb)):
                def f(dve, ta=ta, a=a, ca=ca):
                    return dve.tensor_tensor(out=rt[ta][:, :, :], in0=a, in1=ca, op=ALU.mult)
                S.op("dve", f, reads=(pk, "c_rope"), writes=("rt%d" % ta,))

            def f1(pool):
                return pool.tensor_tensor(out=dv[:, :, 0, :], in0=rt[0][:, :, :], in1=rt[1][:, :, :], op=ALU.subtract)
            S.op("pool", f1, reads=("rt0", "rt1"), writes=(dkey,))

            def f2(pool):
                return pool.tensor_tensor(out=dv[:, :, 1, :], in0=rt[2][:, :, :], in1=rt[3][:, :, :], op=ALU.add)
            S.op("pool", f2, reads=("rt2", "rt3"), writes=(dkey,))

        ident_b = c_qdiag[:, 0, :]
        for hg in range(2):
            slq = load_piece(w_in[:, (0 + hg) * 512:(1 + hg) * 512])
            slk = load_piece(w_in[:, (2 + hg) * 512:(3 + hg) * 512])
            slv = load_piece(w_in[:, (4 + hg) * 512:(5 + hg) * 512])
            slg = load_piece(w_in[:, (6 + hg) * 512:(7 + hg) * 512])
            cnt = 0
            for which, sl in (("q", slq), ("k", slk), ("v", slv), ("g", slg)):
                for tc in range(NTC):
                    pb = cnt % 2
                    cnt += 1
                    pk = "ps%d" % pb
                    pst = ps[pb]
                    mm(pst[:, :], [(xTo[:, dc, tc * 128:(tc + 1) * 128], wslot[sl][:, dc, :]) for dc in range(NDC)],
                       reads=("xTo", "wslot%d" % sl), writes=(pk,))
                    if which == "q":
                        rope_evac2(pst, pk, c_rope[:, 0, tc:tc + 1, :], c_rope[:, 1, tc:tc + 1, :], q_o[:, tc, :], "q_o")
                    elif which == "k":
                        rope_evac2(pst, pk, c_rope[:, 2, tc:tc + 1, :], c_rope[:, 3, tc:tc + 1, :], k_o[:, tc, :], "k_o")
                    elif which == "v":
                        def f(act, pst=pst, tc=tc):
                            return act.copy(out=v_o[:, tc, :], in_=pst[:, :])
                        S.op("act", f, reads=(pk,), writes=("v_o",))
                        for h4 in range(4):
                            def f(act, pst=pst, tc=tc, h4=h4, hg=hg):
                                h = hg * 4 + h4
                                return act.activation(out=vd_o[:, tc, h4 * 128:(h4 + 1) * 128], in_=pst[:, h4 * 128:(h4 + 1) * 128],
                                                      func=AF.Copy, scale=c_kdec[:, h:h + 1])
                            S.op("act", f, reads=(pk, "c_kdec"), writes=("vd_o",))
                    else:
                        def f(act, pst=pst, tc=tc):
                            return act.activation(out=sg_o[:, tc, :], in_=pst[:, :], func=AF.Silu)
                        S.op("act", f, reads=(pk,), writes=("sg_o",))
            for n in range(NTC):
                rb = 6 + n % 2
                rk = "ps%d" % rb
                for h4 in range(4):
                    h = hg * 4 + h4
                    hs = slice(h4 * 128, (h4 + 1) * 128)
                    tb = 2 + h4 % 2
                    tk = "ps%d" % tb
                    trs = trb[h4 % 2]
                    trk = "trb%d" % (h4 % 2)

                    def f_tr(pe, tb=tb, n=n, hs=hs, h=h):
                        pe.matmul(ps[tb][:, 0:128], q_o[:, n, hs], ident_b, start=True, stop=True)
                        pe.matmul(ps[tb][:, 128:256], q_o[:, n, hs], c_qdiag[:, 1 + h, :], start=True, stop=True)
                        return pe.matmul(ps[tb][:, 256:384], k_o[:, n, hs], ident_b, start=True, stop=True)
                    S.op("pe", f_tr, reads=("q_o", "k_o", "c_qdiag"), writes=(tk,))

                    def f_te(act, tb=tb, trs=trs):
                        return act.copy(out=trs[:, :], in_=ps[tb][:, 0:384])
                    S.op("act", f_te, reads=(tk,), writes=(trk,))
                    sb_ = 4 + h4 % 2
                    sk = "ps%d" % sb_
                    mm(ps[sb_][:, 0:128], [(trs[:, 256:384], trs[:, 0:128])], reads=(trk,), writes=(sk,))
                    sd = sdt[h4 % 2]
                    sdk = "sdt%d" % (h4 % 2)

                    def f_sd(dve, sb_=sb_, sd=sd, h=h):
                        return dve.tensor_tensor(out=sd[:, :], in0=ps[sb_][:, 0:128], in1=c_decT[:, h, :], op=ALU.mult)
                    S.op("dve", f_sd, reads=(sk, "c_decT"), writes=(sdk,))
                    mm(ps[rb][:, hs], [(sd[:, :], v_o[:, n, hs]), (trs[:, 128:256], state_b[:, h, :])],
                       reads=(sdk, "v_o", trk, "sb%d" % h), writes=(rk,))
                    mm(ps[tb][:, 384:512], [(k_o[:, n, hs], vd_o[:, n, hs])], reads=("k_o", "vd_o"), writes=(tk,))

                    def f_su(dve, tb=tb, h=h):
                        return dve.scalar_tensor_tensor(out=state_f[:, h, :], in0=state_f[:, h, :], scalar=float(chunk_dec[h]),
                                                        in1=ps[tb][:, 384:512], op0=ALU.mult, op1=ALU.add)
                    S.op("dve", f_su, reads=(tk, "st%d" % h), writes=("st%d" % h,))

                    def f_sbc(act, h=h):
                        return act.copy(out=state_b[:, h, :], in_=state_f[:, h, :])
                    S.op("act", f_sbc, reads=("st%d" % h,), writes=("sb%d" % h,))
                for h4 in range(4):
                    def f_bs(dve, h4=h4, rb=rb):
                        return dve.bn_stats(out=gst[:, h4, :], in_=ps[rb][:, h4 * 128:(h4 + 1) * 128])
                    S.op("dve", f_bs, reads=(rk,), writes=("gst",))
                for h4 in range(4):
                    def f_ba(dve, h4=h4):
                        return dve.bn_aggr(out=gmv[:, h4, :], in_=gst[:, h4, :])
                    S.op("dve", f_ba, reads=("gst",), writes=("gmv",))

                def f_sd2(act):
                    return act.activation(out=grs[:, :], in_=gmv[:, :, 1], func=AF.Sqrt, bias=c_eps[:, 0:1], scale=1.0)
                S.op("act", f_sd2, reads=("gmv", "c_eps"), writes=("grs",))

                def f_rc(dve):
                    return dve.reciprocal(out=grs[:, :], in_=grs[:, :])
                S.op("dve", f_rc, reads=("grs",), writes=("grs",))
                for h4 in range(4):
                    def f_nm(dve, h4=h4, rb=rb):
                        return dve.tensor_scalar(out=rn[:, h4 * 128:(h4 + 1) * 128], in0=ps[rb][:, h4 * 128:(h4 + 1) * 128],
                                                 scalar1=gmv[:, h4, 0:1], scalar2=grs[:, h4:h4 + 1], op0=ALU.subtract, op1=ALU.mult)
                    S.op("dve", f_nm, reads=(rk, "gmv", "grs"), writes=("rn",))

                def f_gw(pool, hg=hg):
                    return pool.tensor_tensor(out=rn[:, :], in0=rn[:, :], in1=c_gnw[:, hg * 512:(hg + 1) * 512], op=ALU.mult)
                S.op("pool", f_gw, reads=("rn", "c_gnw"), writes=("rn",))

                def f_sgm(pool, n=n):
                    return pool.tensor_tensor(out=mtok[:, :], in0=rn[:, :], in1=sg_o[:, n, :], op=ALU.mult)
                S.op("pool", f_sgm, reads=("rn", "sg_o"), writes=("mtok",))
                mb = n % 2
                mk = "ps%d" % mb

                def f_mt(pe, mb=mb):
                    ins = None
                    for h4 in range(4):
                        ins = pe.matmul(ps[mb][:, h4 * 128:(h4 + 1) * 128], mtok[:, h4 * 128:(h4 + 1) * 128], ident_b, start=True, stop=True)
                    return ins
                S.op("pe", f_mt, reads=("mtok", "c_qdiag"), writes=(mk,))

                def f_me(act, mb=mb, hg=hg, n=n):
                    return act.copy(out=mixR[:, hg * 4:hg * 4 + 4, n * 128:(n + 1) * 128],
                                    in_=ps[mb][:, :].rearrange("p (h i) -> p h i", h=4))
                S.op("act", f_me, reads=(mk,), writes=("mixR",))
        if stop_after == "ret":
            dbgbuf = sb("dbgbuf", [128, T], F32, s2)

            def f_d(dve):
                return dve.tensor_copy(out=dbgbuf[:, 0:512], in_=mixR[:, 0, 0:512])
            S.op("dve", f_d, reads=("mixR",), writes=("dbgbuf",))

            def f_d2(dve):
                return dve.tensor_copy(out=dbgbuf[:, 512:T], in_=mixR[:, 5, 512:T])
            S.op("dve", f_d2, reads=("mixR",), writes=("dbgbuf",))
            return dump([(dbgbuf[:, 0:T], T)], ("dbgbuf",))
        S.barrier()
        S.flush()

    s12.close()

    srcs = [w_out[:, dg * 512:(dg + 1) * 512] for dg in range(4)]
    for e in range(n_exp):
        for fg in range(4):
            ee = e % NEW
            srcs.append(w_gate[ee // EG][ee % EG, :, fg * 512:(fg + 1) * 512])
            srcs.append(w_up[ee // EG][ee % EG, :, fg * 512:(fg + 1) * 512])
        for dg in range(4):
            srcs.append(w_down[ee // EG][ee % EG, :, dg * 512:(dg + 1) * 512])
    for dg in range(4):
        srcs.append(w_pg[:, dg * 512:(dg + 1) * 512])
    stream = {"issued": 0, "slots": {}}

    def get_pieces(k, n=1):
        while stream["issued"] < min(k + NSLOT, len(srcs)):
            i = stream["issued"]
            stream["slots"][i] = load_piece(srcs[i])
            stream["issued"] += 1
        return [stream["slots"][k + i] for i in range(n)]

    def get_piece(k):
        return get_pieces(k, 1)[0]

    def layer_norm(src_ap, skey, w_t, b_t, wkeys, dst_ap, dkey, lst, lmv, lrs, tag):
        for k4 in range(4):
            def f(dve, k4=k4):
                return dve.bn_stats(out=lst[:, k4, :], in_=src_ap[:, k4 * 512:(k4 + 1) * 512])
            S.op("dve", f, reads=(skey,), writes=("lst" + tag,))

        def f(dve):
            return dve.bn_aggr(out=lmv[:, :], in_=lst[:, :, :].rearrange("p a b -> p (a b)"))
        S.op("dve", f, reads=("lst" + tag,), writes=("lmv" + tag,))

        def f(act):
            return act.activation(out=lrs[:, :], in_=lmv[:, 1:2], func=AF.Sqrt, bias=c_eps[:, 0:1], scale=1.0)
        S.op("act", f, reads=("lmv" + tag, "c_eps"), writes=("lrs" + tag,))

        def f(dve):
            return dve.reciprocal(out=lrs[:, :], in_=lrs[:, :])
        S.op("dve", f, reads=("lrs" + tag,), writes=("lrs" + tag,))

        def f(dve):
            return dve.tensor_scalar(out=dst_ap, in0=src_ap, scalar1=lmv[:, 0:1], scalar2=lrs[:, 0:1],
                                     op0=ALU.subtract, op1=ALU.mult)
        S.op("dve", f, reads=(skey, "lmv" + tag, "lrs" + tag), writes=(dkey,))

        def f(pool):
            return pool.tensor_tensor(out=dst_ap, in0=dst_ap, in1=w_t[:, :], op=ALU.mult)
        S.op("pool", f, reads=(dkey,) + wkeys, writes=(dkey,))

        def f(pool):
            return pool.tensor_tensor(out=dst_ap, in0=dst_ap, in1=b_t[:, :], op=ALU.add)
        S.op("pool", f, reads=(dkey,) + wkeys, writes=(dkey,))

    gates = sb("gates", [128, NTC, NE])
    posm = sb("posm", [128, NTC, NE])
    posmT = sb("posmT", [NE, T], BF16)
    c_bgu = sb("c_bgu", [128, 2, NE, 16])
    c_iotac = sb("c_iotac", [128, CAP])
    c_iotap = sb("c_iotap", [128, 2])
    c_pidx = sb("c_pidx", [NE, 128])
    cload(c_bgu, bgu, "c_bgu")
    cload(c_iotac, iota_c, "c_iotac")
    cload(c_iotap, iota_p, "c_iotap")
    cload(c_pidx, pidx_in, "c_pidx")
    cfinal()

    with ExitStack() as s3:
        xcb = [sb("xcb%d" % i, [128, 512], F32, s3) for i in range(2)]
        cnt = 0
        for dg in range(4):
            sl = get_piece(dg)
            for tc in range(NTC):
                pb = cnt % 2
                xb = xcb[cnt % 2]
                xk = "xcb%d" % (cnt % 2)
                cnt += 1

                def f(eng, sem, xb=xb, tc=tc, dg=dg):
                    eng.dma_start(out=xb[:, :], in_=x_own[tc * 128:(tc + 1) * 128, dg * 512:(dg + 1) * 512]).then_inc(sem, 16)
                S.dma("sp", f, 1, xk, writes=(xk,))
                pk = "ps%d" % pb
                mm(ps[pb][:, :],
                   [((mixR if fc < 8 else mixL)[:, fc % 8, tc * 128:(tc + 1) * 128], wslot[sl][:, fc, :]) for fc in range(NDC)],
                   reads=("mixR", "mixL", "wslot%d" % sl), writes=(pk,))

                def f(dve, xb=xb, pb=pb, tc=tc, dg=dg):
                    return dve.scalar_tensor_tensor(out=acc[:, tc, dg * 512:(dg + 1) * 512], in0=xb[:, :], scalar=DN_ALPHA,
                                                    in1=ps[pb][:, :], op0=ALU.mult, op1=ALU.add)
                S.op("dve", f, reads=(xk, pk), writes=("acc%d" % tc,))
        S.barrier()
        S.flush()

    with ExitStack() as s3b:
        c_lw = sb("c_lw", [128, DM], F32, s3b)
        c_lb = sb("c_lb", [128, DM], F32, s3b)
        c_wr = sb("c_wr", [128, NDC, NE], F32, s3b)
        c_br = sb("c_br", [128, NE], F32, s3b)
        c_tri = sb("c_tri", [128, 2, 128], BF16, s3b)
        cload(c_lw, rep_ln1w, "c_lw")
        cload(c_lb, rep_ln1b, "c_lb")
        cload(c_wr, w_r.rearrange("(c p) e -> p c e", p=128), "c_wr")
        cload(c_br, rep_br, "c_br")
        cload(c_tri, tri, "c_tri")
        cfinal()
        lst = sb("lst", [128, 4, 6], F32, s3b)
        lmv = sb("lmv", [128, 2], F32, s3b)
        lrs = sb("lrs", [128, 1], F32, s3b)
        x1Tq = [sb("x1Tq%d" % i, [128, 2, 4, 128], BF16, s3b) for i in range(2)]
        xlo = sb("xlo", [128, DM], BF16, s3b)
        wr_hl = sb("wr_hl", [128, 2, NDC, NE], BF16, s3b)
        logit = sb("logit", [128, NTC, NE], F32, s3b)
        top8 = sb("top8", [128, NTC, 8], F32, s3b)
        nmx = sb("nmx", [128, NTC], F32, s3b)
        mask = sb("mask", [128, NTC, NE], F32, s3b)
        maskb = sb("maskb", [128, NTC, NE], BF16, s3b)
        posmb = sb("posmb", [128, NTC, NE], BF16, s3b)
        den = sb("den", [128, NTC], F32, s3b)
        def f(act):
            return act.copy(out=wr_hl[:, 0, :, :], in_=c_wr[:, :, :])
        S.op("act", f, reads=("c_wr",), writes=("wr_hi",))

        def f(dve):
            return dve.tensor_tensor(out=wr_hl[:, 1, :, :], in0=c_wr[:, :, :], in1=wr_hl[:, 0, :, :], op=ALU.subtract)
        S.op("dve", f, reads=("c_wr", "wr_hi"), writes=("wr_lo",))
        for tc in range(NTC):
            ak = "acc%d" % tc
            x1f = acc[:, tc, :]
            layer_norm(x1f, ak, c_lw, c_lb, ("c_lw", "c_lb"), x1f, ak, lst, lmv, lrs, "1")

            def f(act, tc=tc, x1f=x1f):
                return act.copy(out=x1bf[:, tc, :], in_=x1f)
            S.op("act", f, reads=(ak,), writes=("x1bf",))
            def f(dve, tc=tc, x1f=x1f):
                return dve.tensor_tensor(out=xlo[:, :], in0=x1f, in1=x1bf[:, tc, :], op=ALU.subtract)
            S.op("dve", f, reads=(ak, "x1bf"), writes=("xlo",))
            for q4 in range(4):
                xq = x1Tq[q4 % 2]
                xqk = "x1Tq%d" % (q4 % 2)
                for hl in range(2):
                    pb = hl
                    pk = "ps%d" % pb

                    def f(pe, q4=q4, pb=pb, hl=hl, tc=tc):
                        ins = None
                        for i in range(4):
                            dc = q4 * 4 + i
                            src = x1bf[:, tc, dc * 128:(dc + 1) * 128] if hl == 0 else xlo[:, dc * 128:(dc + 1) * 128]
                            ins = pe.matmul(ps[pb][:, i * 128:(i + 1) * 128], src, c_qdiag[:, 0, :], start=True, stop=True)
                        return ins
                    S.op("pe", f, reads=("x1bf", "xlo", "c_qdiag"), writes=(pk,))

                    def f(act, xq=xq, pb=pb, hl=hl):
                        return act.copy(out=xq[:, hl, :, :], in_=ps[pb][:, :].rearrange("p (a t) -> p a t", a=4))
                    S.op("act", f, reads=(pk,), writes=(xqk,))

                def f(pe, q4=q4, xq=xq):
                    ins = None
                    for i in range(4):
                        dc = q4 * 4 + i
                        for j, (xh, wh) in enumerate(((0, 0), (0, 1), (1, 0))):
                            ins = pe.matmul(ps[2][:, 0:NE], xq[:, xh, i, :], wr_hl[:, wh, dc, :],
                                            start=(dc == 0 and j == 0), stop=(dc == NDC - 1 and j == 2))
                    return ins
                S.op("pe", f, reads=(xqk, "wr_hi", "wr_lo"), writes=("ps2",))

            def f(act, tc=tc, x1f=x1f):
                return act.mul(out=x1f, in_=x1f, mul=DN_ALPHA)
            S.op("act", f, reads=(ak,), writes=(ak,))

            def f(dve, tc=tc):
                return dve.tensor_tensor(out=logit[:, tc, :], in0=ps[2][:, 0:NE], in1=c_br[:, :], op=ALU.add)
            S.op("dve", f, reads=("ps2", "c_br"), writes=("logit",))
        if stop_after == "ln1":
            return dump([(acc[:, 0, :], DM), (acc[:, 7, :], DM), (logit[:, :, :].rearrange("p a b -> p (a b)"), NTC * NE)],
                        ("acc0", "acc7", "logit"))
        for tc in range(NTC):
            def f(dve, tc=tc):
                return dve.max(out=top8[:, tc, :], in_=logit[:, tc, :])
            S.op("dve", f, reads=("logit",), writes=("top8",))
        for tc in range(NTC):
            def f(dve, tc=tc):
                return dve.tensor_scalar(out=mask[:, tc, :], in0=logit[:, tc, :], scalar1=top8[:, tc, 3:4], scalar2=None, op0=ALU.is_ge)
            S.op("dve", f, reads=("logit", "top8"), writes=("mask",))

        def f(dve):
            return dve.tensor_scalar(out=nmx[:, :], in0=top8[:, :, 0], scalar1=-1.0, scalar2=None, op0=ALU.mult)
        S.op("dve", f, reads=("top8",), writes=("nmx",))
        for tc in range(NTC):
            def f(act, tc=tc):
                return act.activation(out=gates[:, tc, :], in_=logit[:, tc, :], func=AF.Exp, bias=nmx[:, tc:tc + 1], scale=1.0)
            S.op("act", f, reads=("logit", "nmx"), writes=("gates",))

        def f(dve):
            return dve.tensor_tensor(out=gates[:, :, :], in0=gates[:, :, :], in1=mask[:, :, :], op=ALU.mult)
        S.op("dve", f, reads=("gates", "mask"), writes=("gates",))

        def f(dve):
            return dve.tensor_reduce(out=den[:, :], in_=gates[:, :, :], axis=mybir.AxisListType.X, op=ALU.add)
        S.op("dve", f, reads=("gates",), writes=("den",))

        def f(dve):
            return dve.reciprocal(out=den[:, :], in_=den[:, :])
        S.op("dve", f, reads=("den",), writes=("den",))
        for tc in range(NTC):
            def f(dve, tc=tc):
                return dve.tensor_scalar(out=gates[:, tc, :], in0=gates[:, tc, :], scalar1=den[:, tc:tc + 1], scalar2=None, op0=ALU.mult)
            S.op("dve", f, reads=("gates", "den"), writes=("gates",))

        def f(act):
            return act.copy(out=maskb[:, :, :], in_=mask[:, :, :])
        S.op("act", f, reads=("mask",), writes=("maskb",))
        for tc in range(NTC):
            pb = 4 + tc % 2
            pk = "ps%d" % pb
            pairs = [(c_tri[:, 0, :], maskb[:, t2, :]) for t2 in range(tc)] + [(c_tri[:, 1, :], maskb[:, tc, :])]
            mm(ps[pb][:, 0:NE], pairs, reads=("maskb", "c_tri"), writes=(pk,))

            def f(dve, tc=tc, pb=pb):
                return dve.scalar_tensor_tensor(out=posm[:, tc, :], in0=ps[pb][:, 0:NE], scalar=1.0, in1=mask[:, tc, :],
                                                op0=ALU.add, op1=ALU.mult)
            S.op("dve", f, reads=(pk, "mask"), writes=("posm",))

        def f(dve):
            return dve.tensor_scalar(out=posm[:, :, :], in0=posm[:, :, :], scalar1=-1.0, scalar2=float(CAP), op0=ALU.add, op1=ALU.min)
        S.op("dve", f, reads=("posm",), writes=("posm",))

        def f(act):
            return act.copy(out=posmb[:, :, :], in_=posm[:, :, :])
        S.op("act", f, reads=("posm",), writes=("posmb",))
        for half in range(2):
            pb = 6 + half
            pk = "ps%d" % pb

            def f(pe, half=half, pb=pb):
                ins = None
                for i in range(4):
                    tc = half * 4 + i
                    ins = pe.matmul(ps[pb][0:NE, i * 128:(i + 1) * 128], posmb[:, tc, :], c_qdiag[:, 0, :], start=True, stop=True)
                return ins
            S.op("pe", f, reads=("posmb", "c_qdiag"), writes=(pk,))

            def f(act, half=half, pb=pb):
                return act.copy(out=posmT[:, half * 512:(half + 1) * 512], in_=ps[pb][0:NE, :])
            S.op("act", f, reads=(pk,), writes=("posmT",))
        if stop_after == "route":
            return dump([(gates[:, :, :].rearrange("p a b -> p (a b)"), NTC * NE), (posm[:, :, :].rearrange("p a b -> p (a b)"), NTC * NE)],
                        ("gates", "posm"))
        S.barrier()
        S.flush()

    with ExitStack() as s4:
        sel_e = [sb("sel_e%d" % i, [NE, 128], BF16, s4) for i in range(2)]
        Sg = sb("Sg", [128, NTC, CAP], BF16, s4)
        ST = sb("ST", [128, 2, T], BF16, s4)
        XeT = sb("XeT", [128, NDC, CAP], BF16, s4)
        HT = sb("HT", [128, NDC, CAP], BF16, s4)
        Yb = [sb("Yb%d" % i, [128, 2, 512], BF16, s4) for i in range(2)]
        tA = [sb("tA%d" % i, [128, CAP], F32, s4) for i in range(2)]
        tB = [sb("tB0", [128, CAP], F32, s4)] * 2
        tC = [sb("tC%d" % i, [128, CAP], F32, s4) for i in range(2)]
        tD = [sb("tD0", [128, CAP], F32, s4)] * 2

        def scatter(e, dg):
            yb = Yb[dg % 2]
            yk = "Yb%d" % (dg % 2)
            for tc in range(NTC):
                pb = 6 + tc % 2
                pk = "ps%d" % pb
                mm(ps[pb][:, :], [(ST[:, jc, tc * 128:(tc + 1) * 128], yb[:, jc, :]) for jc in range(2)],
                   reads=("ST", yk), writes=(pk,))

                def f(dve, pb=pb, tc=tc, e=e, dg=dg):
                    return dve.scalar_tensor_tensor(out=acc[:, tc, dg * 512:(dg + 1) * 512], in0=ps[pb][:, :],
                                                    scalar=gates[:, tc, e:e + 1], in1=acc[:, tc, dg * 512:(dg + 1) * 512],
                                                    op0=ALU.mult, op1=ALU.add)
                S.op("dve", f, reads=(pk, "gates", "acc%d" % tc), writes=("acc%d" % tc,))

        for e in range(n_exp):
            base = 4 + e * 12
            for tc in range(NTC):
                def f(pool, tc=tc, e=e):
                    return pool.tensor_scalar(out=Sg[:, tc, :], in0=c_iotac[:, :], scalar1=posm[:, tc, e:e + 1], scalar2=None, op0=ALU.is_equal)
                S.op("pool", f, reads=("c_iotac", "posm"), writes=("Sg",))
            se = sel_e[e % 2]
            sek = "sel_e%d" % (e % 2)

            def f(pool, se=se, e=e):
                return pool.tensor_scalar(out=se[:, :], in0=c_pidx[:, :], scalar1=float(e), scalar2=None, op0=ALU.is_equal)
            S.op("pool", f, reads=("c_pidx",), writes=(sek,))
            for half in range(2):
                pb = half
                pk = "ps%d" % pb
                mm(ps[pb][:, :], [(se[:, :], posmT[:, half * 512:(half + 1) * 512])], reads=(sek, "posmT"), writes=(pk,))
                for jc in range(2):
                    def f(dve, pb=pb, jc=jc, half=half):
                        return dve.tensor_scalar(out=ST[:, jc, half * 512:(half + 1) * 512], in0=ps[pb][:, :],
                                                 scalar1=c_iotap[:, jc:jc + 1], scalar2=None, op0=ALU.is_equal)
                    S.op("dve", f, reads=(pk, "c_iotap"), writes=("ST",))
            for dp in range(NDC // 2):
                pb = dp % 2
                pk = "ps%d" % pb

                def f(pe, dp=dp, pb=pb):
                    ins = None
                    for i in range(2):
                        dc = dp * 2 + i
                        for tc in range(NTC):
                            ins = pe.matmul(ps[pb][:, i * CAP:(i + 1) * CAP], x1bf[:, tc, dc * 128:(dc + 1) * 128], Sg[:, tc, :],
                                            start=(tc == 0), stop=(tc == NTC - 1))
                    return ins
                S.op("pe", f, reads=("x1bf", "Sg"), writes=(pk,))

                def f(act, dp=dp, pb=pb):
                    return act.copy(out=XeT[:, dp * 2:dp * 2 + 2, :], in_=ps[pb][:, :].rearrange("p (a j) -> p a j", a=2))
                S.op("act", f, reads=(pk,), writes=("XeT",))
            for fg in range(4):
                slg, slu = get_pieces(base + fg * 2, 2)
                for fcl in range(4):
                    fc = fg * 4 + fcl
                    par = fc % 2
                    pb = 2 + par
                    pk = "ps%d" % pb

                    def f(pe, pb=pb, slg=slg, slu=slu, fcl=fcl):
                        ins = None
                        for dc in range(NDC):
                            ins = pe.matmul(ps[pb][:, 0:CAP], wslot[slg][:, dc, fcl * 128:(fcl + 1) * 128], XeT[:, dc, :],
                                            start=(dc == 0), stop=(dc == NDC - 1))
                        for dc in range(NDC):
                            ins = pe.matmul(ps[pb][:, CAP:2 * CAP], wslot[slu][:, dc, fcl * 128:(fcl + 1) * 128], XeT[:, dc, :],
                                            start=(dc == 0), stop=(dc == NDC - 1))
                        return ins
                    S.op("pe", f, reads=("XeT", "wslot%d" % slg, "wslot%d" % slu), writes=(pk,))
                    a_, b_, c_, d_ = tA[par], tB[par], tC[par], tD[par]
                    ka, kb, kc, kd = "tA%d" % par, "tB0", "tC%d" % par, "tD0"

                    def f(dve, pb=pb, a_=a_, e=e, fc=fc):
                        return dve.tensor_scalar(out=a_[:, :], in0=ps[pb][:, 0:CAP], scalar1=c_bgu[:, 0, e, fc:fc + 1], scalar2=7.0,
                                                 op0=ALU.add, op1=ALU.min)
                    S.op("dve", f, reads=(pk, "c_bgu"), writes=(ka,))

                    def f(act, a_=a_, b_=b_):
                        return act.activation(out=b_[:, :], in_=a_[:, :], func=AF.Sigmoid, scale=1.702)
                    S.op("act", f, reads=(ka,), writes=(kb,))

                    def f(dve, pb=pb, c_=c_, e=e, fc=fc):
                        return dve.tensor_scalar(out=c_[:, :], in0=ps[pb][:, CAP:2 * CAP], scalar1=c_bgu[:, 1, e, fc:fc + 1], scalar2=7.0,
                                                 op0=ALU.add, op1=ALU.min)
                    S.op("dve", f, reads=(pk, "c_bgu"), writes=(kc,))

                    def f(dve, c_=c_):
                        return dve.tensor_scalar(out=c_[:, :], in0=c_[:, :], scalar1=-7.0, scalar2=1.0, op0=ALU.max, op1=ALU.add)
                    S.op("dve", f, reads=(kc,), writes=(kc,))

                    def f(pool, a_=a_, b_=b_, d_=d_):
                        return pool.tensor_tensor(out=d_[:, :], in0=a_[:, :], in1=b_[:, :], op=ALU.mult)
                    S.op("pool", f, reads=(ka, kb), writes=(kd,))

                    def f(pool, c_=c_, d_=d_, fc=fc):
                        return pool.tensor_tensor(out=HT[:, fc, :], in0=d_[:, :], in1=c_[:, :], op=ALU.mult)
                    S.op("pool", f, reads=(kc, kd), writes=("HT",))
            for dg in range(4):
                sld = get_piece(base + 8 + dg)
                yb = Yb[dg % 2]
                yk = "Yb%d" % (dg % 2)
                for jc in range(2):
                    pb = 4 + jc
                    pk = "ps%d" % pb
                    mm(ps[pb][:, :], [(HT[:, fc, jc * 128:(jc + 1) * 128], wslot[sld][:, fc, :]) for fc in range(NDC)],
                       reads=("HT", "wslot%d" % sld), writes=(pk,))

                    def f(act, pb=pb, yb=yb, jc=jc):
                        return act.copy(out=yb[:, jc, :], in_=ps[pb][:, :])
                    S.op("act", f, reads=(pk,), writes=(yk,))
                if dg >= 1:
                    scatter(e, dg - 1)
            scatter(e, 3)
        S.barrier()
        S.flush()
    with ExitStack() as s4:
        gatesT = sb("gatesT", [NE, 2, T], BF16, s4)
        g_hl = sb("g_hl", [128, 2, NTC, NE], BF16, s4)
        c_bdn = sb("c_bdn", [NE, DM], F32, s4)
        b_hl = sb("b_hl", [NE, 2, DM], BF16, s4)
        cload(c_bdn, b_down, "c_bdn")
        cfinal()

        def f(act):
            return act.copy(out=g_hl[:, 0, :, :], in_=gates[:, :, :])
        S.op("act", f, reads=("gates",), writes=("g_hi",))

        def f(dve):
            return dve.tensor_tensor(out=g_hl[:, 1, :, :], in0=gates[:, :, :], in1=g_hl[:, 0, :, :], op=ALU.subtract)
        S.op("dve", f, reads=("gates", "g_hi"), writes=("g_lo",))

        def f(act):
            return act.copy(out=b_hl[:, 0, :], in_=c_bdn[:, :])
        S.op("act", f, reads=("c_bdn",), writes=("b_hi",))

        def f(dve):
            return dve.tensor_tensor(out=b_hl[:, 1, :], in0=c_bdn[:, :], in1=b_hl[:, 0, :], op=ALU.subtract)
        S.op("dve", f, reads=("c_bdn", "b_hi"), writes=("b_lo",))
        for hl in range(2):
            for half in range(2):
                pb = 2 + half
                pk = "ps%d" % pb

                def f(pe, half=half, pb=pb, hl=hl):
                    ins = None
                    for i in range(4):
                        tc = half * 4 + i
                        ins = pe.matmul(ps[pb][0:NE, i * 128:(i + 1) * 128], g_hl[:, hl, tc, :], c_qdiag[:, 0, :], start=True, stop=True)
                    return ins
                S.op("pe", f, reads=("g_hi", "g_lo", "c_qdiag"), writes=(pk,))

                def f(act, half=half, pb=pb, hl=hl):
                    return act.copy(out=gatesT[:, hl, half * 512:(half + 1) * 512], in_=ps[pb][0:NE, :])
                S.op("act", f, reads=(pk,), writes=("gatesT",))
        for tc in range(NTC):
            for dg in range(4):
                pb = (tc * 4 + dg) % 2
                pk = "ps%d" % pb
                mm(ps[pb][:, :], [(gatesT[:, gh, tc * 128:(tc + 1) * 128], b_hl[:, bh, dg * 512:(dg + 1) * 512])
                                  for gh, bh in ((0, 0), (0, 1), (1, 0))],
                   reads=("gatesT", "b_hi", "b_lo"), writes=(pk,))

                def f(dve, pb=pb, tc=tc, dg=dg):
                    return dve.tensor_tensor(out=acc[:, tc, dg * 512:(dg + 1) * 512], in0=ps[pb][:, :],
                                             in1=acc[:, tc, dg * 512:(dg + 1) * 512], op=ALU.add)
                S.op("dve", f, reads=(pk, "acc%d" % tc), writes=("acc%d" % tc,))
        if stop_after == "moe":
            return dump([(acc[:, 0, :], DM), (acc[:, 7, :], DM)], ("acc0", "acc7"))
        S.barrier()
        S.flush()

    with ExitStack() as s5:
        c_lw = sb("c_lw2", [128, DM], F32, s5)
        c_lb = sb("c_lb2", [128, DM], F32, s5)
        c_pw = arB[:, 0:2048]
        c_pp = arB[:, 2048:4096].bitcast(BF16).rearrange("p (c d) -> p c d", c=2)
        c_pT = sb("c_pT", [128, 2, T], BF16, s5)
        cload(c_lw, rep_ln2w, "c_lw2")
        cload(c_lb, rep_ln2b, "c_lb2")
        cload(c_pw, rep_plew, "c_pw")
        cload(c_pp, w_pp.rearrange("(c p) d -> p c d", p=128), "c_pp", eng="pool")
        cload(c_pT, pT.rearrange("(c p) t -> p c t", p=128), "c_pT", eng="pool")
        cfinal()
        lst = sb("lst2", [128, 4, 6], F32, s5)
        lmv = sb("lmv2", [128, 2], F32, s5)
        lrs = sb("lrs2", [128, 1], F32, s5)
        x2b = sb("x2b", [128, DM], BF16, s5)
        x2T = sb("x2T", [128, NDC, 128], BF16, s5)
        ebuf = arB[:, 4096:6144]
        esq = sb("esq", [128, 512], F32, s5)
        ess = sb("ess", [128, 4], F32, s5)
        ers = sb("ers", [128, 1], F32, s5)
        gbuf = arB[:, 6144:8192]
        pslots = get_pieces(len(srcs) - 4, 4)
        import os
        ndummy = int(os.environ.get("K_DUMMY_MM", "0"))
        if ndummy:
            def f(pe):
                ins = None
                for i in range(ndummy):
                    ins = pe.matmul(ps[7][:, 0:8], c_qdiag[:, 0, :], c_qdiag[:, 0, 0:8], start=True, stop=True)
                return ins
            S.op("pe", f, reads=("c_qdiag",), writes=("ps7",))
        if stop_after == "s5pre":
            return dump([(acc[:, 0, :], DM), (c_pw[:, :], DM)], ("acc0", "c_pw", "c_pp", "c_pT", "c_lw2", "c_lb2"))
        for tc in range(NTC):
            ak = "acc%d" % tc
            x2 = acc[:, tc, :]
            layer_norm(x2, ak, c_lw, c_lb, ("c_lw2", "c_lb2"), x2, ak, lst, lmv, lrs, "2")

            def f(act, x2=x2):
                return act.copy(out=x2b[:, :], in_=x2)
            S.op("act", f, reads=(ak,), writes=("x2b",))
            if stop_after == "ln2":
                continue
            for q4 in range(4):
                pb = q4 % 2
                pk = "ps%d" % pb

                def f(pe, q4=q4, pb=pb):
                    ins = None
                    for i in range(4):
                        dc = q4 * 4 + i
                        ins = pe.matmul(ps[pb][:, i * 128:(i + 1) * 128], x2b[:, dc * 128:(dc + 1) * 128], c_qdiag[:, 0, :], start=True, stop=True)
                    return ins
                S.op("pe", f, reads=("x2b", "c_qdiag"), writes=(pk,))

                def f(act, q4=q4, pb=pb):
                    return act.copy(out=x2T[:, q4 * 4:(q4 + 1) * 4, :], in_=ps[pb][:, :].rearrange("p (a t) -> p a t", a=4))
                S.op("act", f, reads=(pk,), writes=("x2T",))
            for dg in range(4):
                pb = 2 + dg % 2
                pk = "ps%d" % pb
                mm(ps[pb][:, :], [(c_pT[:, pc, tc * 128:(tc + 1) * 128], c_pp[:, pc, dg * 512:(dg + 1) * 512]) for pc in range(2)],
                   reads=("c_pT", "c_pp"), writes=(pk,))

                def f(act, pb=pb, dg=dg):
                    return act.copy(out=ebuf[:, dg * 512:(dg + 1) * 512], in_=ps[pb][:, :])
                S.op("act", f, reads=(pk,), writes=("ebuf",))

                def f(dve, dg=dg):
                    return dve.tensor_tensor(out=esq[:, :], in0=ebuf[:, dg * 512:(dg + 1) * 512], in1=ebuf[:, dg * 512:(dg + 1) * 512], op=ALU.mult)
                S.op("dve", f, reads=("ebuf",), writes=("esq",))

                def f(dve, dg=dg):
                    return dve.tensor_reduce(out=ess[:, dg:dg + 1], in_=esq[:, :], axis=mybir.AxisListType.X, op=ALU.add)
                S.op("dve", f, reads=("esq",), writes=("ess",))

            def f(dve):
                return dve.tensor_reduce(out=ers[:, :], in_=ess[:, :], axis=mybir.AxisListType.X, op=ALU.add)
            S.op("dve", f, reads=("ess",), writes=("ers",))

            def f(act):
                return act.activation(out=ers[:, :], in_=ers[:, :], func=AF.Sqrt, bias=c_eps[:, 0:1], scale=1.0 / DM)
            S.op("act", f, reads=("ers", "c_eps"), writes=("ers",))

            def f(dve):
                return dve.reciprocal(out=ers[:, :], in_=ers[:, :])
            S.op("dve", f, reads=("ers",), writes=("ers",))

            def f(dve):
                return dve.scalar_tensor_tensor(out=ebuf[:, :], in0=ebuf[:, :], scalar=ers[:, 0:1], in1=c_pw[:, :], op0=ALU.mult, op1=ALU.mult)
            S.op("dve", f, reads=("ebuf", "ers", "c_pw"), writes=("ebuf",))
            for dg in range(4):
                pb = 4 + dg % 2
                pk = "ps%d" % pb
                sl = pslots[dg]
                mm(ps[pb][:, :], [(x2T[:, dc, :], wslot[sl][:, dc, :]) for dc in range(NDC)],
                   reads=("x2T", "wslot%d" % sl), writes=(pk,))

                def f(act, pb=pb, dg=dg):
                    return act.activation(out=gbuf[:, dg * 512:(dg + 1) * 512], in_=ps[pb][:, :], func=AF.Sigmoid)
                S.op("act", f, reads=(pk,), writes=("gbuf",))

            def f(pool):
                return pool.tensor_tensor(out=gbuf[:, :], in0=gbuf[:, :], in1=ebuf[:, :], op=ALU.mult)
            S.op("pool", f, reads=("gbuf", "ebuf"), writes=("gbuf",))

            def f(pool, x2=x2):
                return pool.tensor_tensor(out=x2, in0=x2, in1=gbuf[:, :], op=ALU.add)
            S.op("pool", f, reads=("gbuf", ak), writes=(ak,))

            if stop_after == "ple":
                continue

            def f(eng, sem, tc=tc, x2=x2):
                eng.dma_start(out=out[tc * 128:(tc + 1) * 128, :], in_=x2).then_inc(sem, 16)
            S.dma("pool", f, 1, "outst", reads=(ak,))
        if stop_after in ("ln2", "ple"):
            return dump([(acc[:, 0, :], DM), (acc[:, 7, :], DM)], ("acc0", "acc7"))
        S.barrier()
        S.flush()


def _consts():
    h = np.arange(8, dtype=np.float64)
    log_gamma = np.log1p(-np.exp2(-5.0 - h))
    idx = np.arange(128, dtype=np.float64)
    diff = idx[None, :] - idx[:, None]
    decT = np.where(diff[:, None, :] >= 0, np.exp(np.maximum(diff, 0.0)[:, None, :] * log_gamma[None, :, None]), 0.0)
    kdec = np.exp((127.0 - idx)[:, None] * log_gamma[None, :])
    qdec = np.exp((idx[:, None] + 1.0) * log_gamma[None, :])
    chunk_dec = np.exp(128.0 * log_gamma)
    qdiag = np.zeros((128, 9, 128), np.float32)
    qdiag[:, 0, :] = np.eye(128)
    for hh in range(8):
        qdiag[:, 1 + hh, :] = np.diag(qdec[:, hh])
    tri = np.zeros((128, 2, 128), np.float32)
    tri[:, 0, :] = 1.0
    tri[:, 1, :] = (idx[:, None] < idx[None, :]).astype(np.float32)
    return dict(
        decT=decT.astype(np.float32), kdec=kdec.astype(np.float32),
        qdiag=qdiag.astype(ml_dtypes.bfloat16), ident_f=np.eye(128, dtype=np.float32),
        tri=tri.astype(ml_dtypes.bfloat16),
        iota_c=np.broadcast_to(np.arange(CAP, dtype=np.float32)[None, :], (128, CAP)).copy(),
        iota_p=np.stack([np.arange(128, dtype=np.float32), np.arange(128, dtype=np.float32) + 128], axis=1),
        pidx=np.broadcast_to(np.arange(NE, dtype=np.float32)[:, None], (NE, 128)).copy(),
    ), chunk_dec


def _rope_tables(pos0):
    inv = 10000.0 ** (-np.arange(0, 128, 2, dtype=np.float32) / 128)
    pos = pos0 + np.arange(T, dtype=np.float32)
    ang = pos[:, None] * inv[None, :]
    cos = np.cos(ang).astype(np.float32)
    sin = np.sin(ang).astype(np.float32)
    return cos, sin


def _prep_inputs(inp, n_exp_decl=None):
    f = lambda a: np.ascontiguousarray(np.asarray(a, dtype=np.float32))
    x = f(inp["x"])
    p = f(inp["p"])[0]
    cst, _ = _consts()
    rep = lambda v, n=128: np.ascontiguousarray(np.broadcast_to(f(v).reshape(1, -1), (n, f(v).size)))
    lruvec = np.zeros((128, 8, 8), np.float32)
    cw = f(inp["conv_w"])[0]
    for j in range(4):
        lruvec[:, :, j] = cw[j].reshape(8, 128).T
    lruvec[:, :, 4] = f(inp["conv_b"])[0].reshape(8, 128).T
    lruvec[:, :, 5] = f(inp["lru_ba"])[0].T
    lruvec[:, :, 6] = f(inp["lru_bx"])[0].T
    lruvec[:, :, 7] = f(inp["lru_lam"])[0].reshape(8, 128).T
    bgu = np.zeros((128, 2, NE, 16), np.float32)
    bgu[:, 0] = f(inp["b_gate"])[0].reshape(NE, 16, 128).transpose(2, 0, 1)
    bgu[:, 1] = f(inp["b_up"])[0].reshape(NE, 16, 128).transpose(2, 0, 1)
    shared = dict(
        w_in=f(inp["w_in"])[0], w_out=f(inp["w_out"])[0], w_ple_gate=f(inp["w_ple_gate"])[0], w_ple_proj=f(inp["w_ple_proj"])[0],
        w_router=f(inp["w_router"])[0], lru_wa=f(inp["lru_wa"])[0], lru_wx=f(inp["lru_wx"])[0],
        lruvec=lruvec, bgu=bgu, b_down=f(inp["b_down"])[0],
        rep_ln1w=rep(inp["ln1_w"]), rep_ln1b=rep(inp["ln1_b"]), rep_ln2w=rep(inp["ln2_w"]), rep_ln2b=rep(inp["ln2_b"]),
        rep_plew=rep(inp["ple_norm_w"]), rep_gnw=rep(inp["ret_gn_w"]), rep_br=rep(inp["b_router"]),
        **cst,
    )
    for nm in ("w_gate", "w_up", "w_down"):
        w = f(inp[nm])[0]
        ne = NE if n_exp_decl is None else n_exp_decl
        for g in range((ne + EG - 1) // EG):
            shared["%s_%d" % (nm, g)] = w[g * EG:min((g + 1) * EG, ne)]
    scale_k = 128.0 ** -0.5
    in_maps = []
    for c in range(NCORES):
        b, hf = c // 2, c % 2
        own = slice(hf * T, (hf + 1) * T)
        m = dict(shared)
        m["xT_own"] = np.ascontiguousarray(x[b, own, :].T)
        m["xT_pre"] = np.ascontiguousarray(x[b, 0:T, :].T) if hf == 1 else np.zeros((DM, T), np.float32)
        m["x_own"] = np.ascontiguousarray(x[b, own, :])
        m["pT"] = np.ascontiguousarray(p[b, own, :].T)
        m["pf"] = np.full((128, 1), float(hf), np.float32)
        cos_o, sin_o = _rope_tables(float(hf * T))
        cos_p, sin_p = _rope_tables(0.0)
        lay = lambda a: a.reshape(NTC, 128, 64).transpose(1, 0, 2)
        m["rope_own"] = np.ascontiguousarray(np.stack([lay(cos_o), lay(sin_o), lay(cos_o * scale_k), lay(sin_o * scale_k)], axis=1))
        m["rope_pre"] = np.ascontiguousarray(np.stack([lay(cos_p * scale_k), lay(sin_p * scale_k)], axis=1))
        in_maps.append(m)
    return in_maps


_NC_CACHE = {}


def kernel(**inputs):
    in_maps = _prep_inputs(inputs)
    if "nc" not in _NC_CACHE:
        _NC_CACHE["nc"] = build()
    nc = _NC_CACHE["nc"]
    res = run_bass_kernel_spmd(nc, in_maps, core_ids=list(range(NCORES)))
    outp = np.zeros((4, 2048, DM), np.float32)
    for c in range(NCORES):
        b, hf = c // 2, c % 2
        outp[b, hf * T:(hf + 1) * T, :] = res.results[c]["out"]
    return outp
```

```python
import math
from contextlib import ExitStack

import numpy as np
import ml_dtypes

import concourse.bass as bass
import concourse.mybir as mybir
from concourse.bass_utils import run_bass_kernel_spmd

F32 = mybir.dt.float32
BF16 = mybir.dt.bfloat16
ALU = mybir.AluOpType
AF = mybir.ActivationFunctionType

NCORES = 8
DM = 2048
T = 1024
NTC = 8
NDC = 16
NE = 32
CAP = 256
LN_EPS = 1e-5
DN_ALPHA = 2.0 ** 0.25
NSLOT = 4
EG = 8


class Sched:
    ENG = ("pe", "act", "dve", "pool", "sp")

    def __init__(self, nc, es):
        self.nc = nc
        self.es = es
        self.sem = {e: es.enter_context(nc.semaphore("s_" + e)) for e in self.ENG}
        self.cnt = {e: 0 for e in self.ENG}
        self.waited = {e: {} for e in self.ENG}
        self.ops = {e: [] for e in self.ENG}
        self.last_w = {}
        self.readers = {}
        self.dsem = {}
        self.dcnt = {}

    def _deps(self, eng, reads, writes):
        deps = {}

        def add(d):
            if d is None:
                return
            s, v = d
            if deps.get(id(s), (None, 0))[1] < v:
                deps[id(s)] = (s, v)

        for k in reads:
            add(self.last_w.get(k))
        for k in writes:
            add(self.last_w.get(k))
            for d in self.readers.get(k, {}).values():
                add(d)
        waits = []
        for sid, (s, v) in deps.items():
            if self.waited[eng].get(sid, 0) < v:
                self.waited[eng][sid] = v
                waits.append((s, v))
        return waits

    def _mark(self, me, reads, writes):
        for k in reads:
            self.readers.setdefault(k, {})[id(me[0])] = me
        for k in writes:
            self.last_w[k] = me
            self.readers[k] = {}

    def op(self, eng, fn, reads=(), writes=()):
        waits = self._deps(eng, reads, writes)
        self.cnt[eng] += 1
        me = (self.sem[eng], self.cnt[eng])
        self.ops[eng].append((waits, fn, self.sem[eng], 1))
        self._mark(me, reads, writes)

    def dma(self, eng, fn, ndma, semname, reads=(), writes=()):
        if semname not in self.dsem:
            self.dsem[semname] = self.es.enter_context(self.nc.semaphore("d_" + semname))
            self.dcnt[semname] = 0
        waits = self._deps(eng, reads, writes)
        self.dcnt[semname] += 16 * ndma
        me = (self.dsem[semname], self.dcnt[semname])
        self.ops[eng].append((waits, fn, self.dsem[semname], None))
        self._mark(me, reads, writes)

    def barrier(self):
        allsem = [(self.sem[e], self.cnt[e]) for e in self.ENG if self.cnt[e] > 0]
        allsem += [(self.dsem[k], self.dcnt[k]) for k in self.dsem if self.dcnt[k] > 0]
        for e in self.ENG:
            waits = []
            for s, v in allsem:
                if self.waited[e].get(id(s), 0) < v:
                    self.waited[e][id(s)] = v
                    waits.append((s, v))
            if waits:
                self.ops[e].append((waits, None, None, 0))

    def flush(self):
        with self.nc.Block() as block:
            decos = {"pe": block.tensor, "act": block.scalar, "dve": block.vector,
                     "pool": block.gpsimd, "sp": block.sync}
            for e in self.ENG:
                ops = self.ops[e]
                if not ops:
                    continue

                def body(engine, ops=ops):
                    for waits, fn, sem, inc in ops:
                        for s, v in waits:
                            engine.wait_ge(s, v)
                        if fn is None:
                            continue
                        if inc is None:
                            fn(engine, sem)
                        else:
                            fn(engine).then_inc(sem, inc)

                decos[e](body)
                self.ops[e] = []


class _Stop(Exception):
    pass


def build(stop_after=None, n_exp_run=None, n_exp_decl=None):
    nc = bass.Bass("TRN2", target_bir_lowering=False)
    es = ExitStack()
    try:
        _body(nc, es, stop_after, n_exp_run, n_exp_decl)
    except _Stop:
        pass
    return nc


def _body(nc, es, stop_after, n_exp_run, n_exp_decl=None):
    S = Sched(nc, es)

    def din(name, shape, dt=F32):
        return nc.dram_tensor(name, list(shape), dt, kind="ExternalInput").ap()

    xT_own = din("xT_own", [DM, T])
    xT_pre = din("xT_pre", [DM, T])
    x_own = din("x_own", [T, DM])
    pT = din("pT", [256, T])
    w_in = din("w_in", [DM, 6144])
    w_out = din("w_out", [DM, DM])
    n_exp = NE if stop_after in (None, "moe", "ln2", "s5pre", "ple") else 0
    if n_exp_run is not None:
        n_exp = n_exp_run
    NEW = max(n_exp, 1) if n_exp_decl is None else n_exp_decl
    NG = (NEW + EG - 1) // EG
    w_gate = [din("w_gate_%d" % g, [min(EG, NEW - g * EG), 4, 128, 8192]) for g in range(NG)]
    w_up = [din("w_up_%d" % g, [min(EG, NEW - g * EG), 4, 128, 8192]) for g in range(NG)]
    w_down = [din("w_down_%d" % g, [min(EG, NEW - g * EG), 4, 128, 8192]) for g in range(NG)]
    w_pg = din("w_ple_gate", [DM, DM])
    w_pp = din("w_ple_proj", [256, DM])
    w_r = din("w_router", [DM, NE])
    lru_wa = din("lru_wa", [8, 128, 128])
    lru_wx = din("lru_wx", [8, 128, 128])
    lruvec = din("lruvec", [128, 8, 8])
    bgu = din("bgu", [128, 2, NE, 16])
    b_down = din("b_down", [NE, DM])
    rep_ln1w = din("rep_ln1w", [128, DM])
    rep_ln1b = din("rep_ln1b", [128, DM])
    rep_ln2w = din("rep_ln2w", [128, DM])
    rep_ln2b = din("rep_ln2b", [128, DM])
    rep_plew = din("rep_plew", [128, DM])
    rep_gnw = din("rep_gnw", [128, 1024])
    rep_br = din("rep_br", [128, NE])
    pf_in = din("pf", [128, 1])
    rope_own = din("rope_own", [128, 4, NTC, 64])
    rope_pre = din("rope_pre", [128, 2, NTC, 64])
    decT = din("decT", [128, 8, 128])
    kdec = din("kdec", [128, 8])
    qdiag = din("qdiag", [128, 9, 128], BF16)
    ident_f = din("ident_f", [128, 128])
    tri = din("tri", [128, 2, 128], BF16)
    iota_c = din("iota_c", [128, CAP])
    iota_p = din("iota_p", [128, 2])
    pidx_in = din("pidx", [NE, 128])

    out = nc.dram_tensor("out", [T, DM], F32, kind="ExternalOutput").ap()
    dbg = None
    if stop_after is not None:
        dbg = nc.dram_tensor("dbg", [128, 8192], F32, kind="ExternalOutput").ap()

    def sb(name, shape, dt=F32, stack=None):
        return (stack or es).enter_context(nc.sbuf_tensor(name, list(shape), dt))

    ps = [es.enter_context(nc.psum_tensor("ps%d" % i, [128, 512], F32)) for i in range(8)]
    wslot = [sb("wslot%d" % i, [128, NDC, 512], BF16) for i in range(NSLOT)]
    c_identf = sb("c_identf", [128, 128])
    c_qdiag = sb("c_qdiag", [128, 9, 128], BF16)

    piece_no = [0]

    def load_piece(src_ap):
        i = piece_no[0] % NSLOT
        piece_no[0] += 1
        key = "wslot%d" % i
        dst = wslot[i]

        def fn(eng, sem, src_ap=src_ap, dst=dst):
            if tuple(src_ap.shape) == (128, 8192):
                v = src_ap.rearrange("p (c f) -> p c f", c=NDC)
            else:
                v = src_ap.rearrange("(c p) f -> p c f", p=128)
            for hh in range(2):
                eng.dma_start(out=dst[:, hh * 8:(hh + 1) * 8, :], in_=v[:, hh * 8:(hh + 1) * 8, :]).then_inc(sem, 16)

        S.dma("pool", fn, 2, key, reads=(), writes=(key,))
        return i

    consts = []

    def cload(dst_t, src_ap, key, eng="sp"):
        def fn(engine, sem, dst_t=dst_t, src_ap=src_ap):
            engine.dma_start(out=dst_t[:], in_=src_ap).then_inc(sem, 16)
        S.dma(eng, fn, 1, "c_" + eng, writes=(key,))
        consts.append(key)

    def cfinal():
        for eng in ("sp", "pool"):
            nm = "c_" + eng
            if nm in S.dsem:
                for k in consts:
                    if S.last_w[k][0] is S.dsem[nm]:
                        S.last_w[k] = (S.dsem[nm], S.dcnt[nm])
        consts.clear()

    cload(c_identf, ident_f, "c_identf")
    cload(c_qdiag, qdiag, "c_qdiag")
    c_eps = sb("c_eps", [128, 1])

    def f_eps(pool):
        return pool.memset(c_eps[:, :], LN_EPS)
    S.op("pool", f_eps, writes=("c_eps",))
    c_dummy = sb("c_dummy", [128, 8], BF16)
    cload(c_dummy, kdec, "c_dummy", eng="pool")
    cfinal()
    S.flush()

    def mm(out_ap, pairs, reads, writes):
        def fn(pe, out_ap=out_ap, pairs=pairs):
            n = len(pairs)
            ins = None
            for i, (l, r) in enumerate(pairs):
                ins = pe.matmul(out_ap, l, r, start=(i == 0), stop=(i == n - 1))
            return ins
        S.op("pe", fn, reads=reads, writes=writes)

    def dump(ap_list, keys):
        off = 0
        for ap, n in ap_list:
            def fn(engine, sem, ap=ap, off=off, n=n):
                engine.dma_start(out=dbg[:, off:off + n], in_=ap).then_inc(sem, 16)
            S.dma("sp", fn, 1, "dbg", reads=keys)
            off += n
        S.barrier()
        S.flush()
        raise _Stop()

    arA = sb("arA", [128, 16384])
    arB = sb("arB", [128, 8192])
    xTo = arA[:, 0:8192].bitcast(BF16).rearrange("p (c t) -> p c t", c=NDC)
    xTp = arA[:, 8192:16384].bitcast(BF16).rearrange("p (c t) -> p c t", c=NDC)
    mixL = arB[:, 0:4096].bitcast(BF16).rearrange("p (c t) -> p c t", c=8)
    mixR = arB[:, 4096:8192].bitcast(BF16).rearrange("p (c t) -> p c t", c=8)
    acc = arA[:, :].rearrange("p (c d) -> p c d", c=NTC)
    x1bf = arB[:, :].bitcast(BF16).rearrange("p (c d) -> p c d", c=NTC)
    s12 = ExitStack()
    c_lruvec = sb("c_lruvec", [128, 8, 8], F32, s12)
    c_pf = sb("c_pf", [128, 1], F32, s12)
    state_f = sb("state_f", [128, 8, 128], F32, s12)
    state_b = sb("state_b", [128, 8, 128], BF16, s12)
    cload(c_lruvec, lruvec, "c_lruvec")
    cload(c_pf, pf_in, "c_pf")

    def load_xT(dst, src, key):
        def fn(eng, sem):
            v = src.rearrange("(c p) t -> p c t", p=128)
            for q4 in range(4):
                eng.dma_start(out=dst[:, q4 * 4:(q4 + 1) * 4, :], in_=v[:, q4 * 4:(q4 + 1) * 4, :]).then_inc(sem, 16)
        S.dma("pool", fn, 4, key, writes=(key,))

    with ExitStack() as s1:
        load_xT(xTp, xT_pre, "xTp")
        load_xT(xTo, xT_own, "xTo")
        c_wa = sb("c_wa", [128, 8, 128], BF16, s1)
        c_wx = sb("c_wx", [128, 8, 128], BF16, s1)
        cload(c_wa, lru_wa.rearrange("h i j -> i h j"), "c_wa", eng="pool")
        cload(c_wx, lru_wx.rearrange("h i j -> i h j"), "c_wx", eng="pool")
        cfinal()
        c8 = sb("c8", [128, 8, 2], F32, s1)

        def f_sig(act):
            return act.activation(out=c8[:, :, 0], in_=c_lruvec[:, :, 7], func=AF.Sigmoid)
        S.op("act", f_sig, reads=("c_lruvec",), writes=("c8a",))

        def f_ln(act):
            return act.activation(out=c8[:, :, 1], in_=c8[:, :, 0], func=AF.Ln)
        S.op("act", f_ln, reads=("c8a",), writes=("c8b",))

        def f_c8(dve):
            return dve.tensor_scalar(out=c8[:, :, 0], in0=c8[:, :, 1], scalar1=8.0, scalar2=None, op0=ALU.mult)
        S.op("dve", f_c8, reads=("c8b",), writes=("c8a",))

        def f_c16(dve):
            return dve.tensor_scalar(out=c8[:, :, 1], in0=c8[:, :, 0], scalar1=2.0, scalar2=None, op0=ALU.mult)
        S.op("dve", f_c16, reads=("c8a",), writes=("c8",))

        with ExitStack() as s1a:
            X = sb("lX", [128, 3 + 2 * T], F32, s1a)
            C = sb("lC", [128, 2 * T], F32, s1a)
            R = arB[:, 4096:6144]
            CB = arB[:, 6144:7168].bitcast(BF16)
            G = sb("lG", [128, 2 * T], F32, s1a)
            hb0 = sb("lh0", [128, 1], F32, s1a)

            def f_zero(pool):
                return pool.memset(X[:, 0:3], 0.0)
            S.op("pool", f_zero, writes=("X",))

            for hb in range(8):
                cg = 8 + hb // 4
                cc = hb % 4
                if cc == 0:
                    sl_x = load_piece(w_in[:, cg * 512:(cg + 1) * 512])
                    sl_y = load_piece(w_in[:, (cg + 2) * 512:(cg + 3) * 512])
                kx, ky = "wslot%d" % sl_x, "wslot%d" % sl_y
                for tt in range(4):
                    src, skey = (xTp, "xTp") if tt < 2 else (xTo, "xTo")
                    t0 = (tt % 2) * 512
                    pk = "ps%d" % (tt % 2)
                    mm(ps[tt % 2][:, :],
                       [(wslot[sl_x][:, dc, cc * 128:(cc + 1) * 128], src[:, dc, t0:t0 + 512]) for dc in range(NDC)],
                       reads=(kx, skey), writes=(pk,))

                    def f_ev(act, tt=tt):
                        return act.copy(out=X[:, 3 + tt * 512:3 + (tt + 1) * 512], in_=ps[tt % 2][:, :])
                    S.op("act", f_ev, reads=(pk,), writes=("X",))
                lv = c_lruvec

                def f_c0(dve, hb=hb):
                    return dve.tensor_scalar(out=C[:, :], in0=X[:, 0:2 * T], scalar1=lv[:, hb, 0:1], scalar2=lv[:, hb, 4:5],
                                             op0=ALU.mult, op1=ALU.add)
                S.op("dve", f_c0, reads=("X", "c_lruvec"), writes=("C",))
                for j in range(1, 4):
                    def f_cj(dve, hb=hb, j=j):
                        return dve.scalar_tensor_tensor(out=C[:, :], in0=X[:, j:j + 2 * T], scalar=lv[:, hb, j:j + 1], in1=C[:, :],
                                                        op0=ALU.mult, op1=ALU.add)
                    S.op("dve", f_cj, reads=("X", "C"), writes=("C",))

                def f_cb(act):
                    return act.copy(out=CB[:, :], in_=C[:, :])
                S.op("act", f_cb, reads=("C",), writes=("CB",))
                for gi_, (wt, wk, dstb, dk, bcol) in enumerate(((c_wa, "c_wa", R, "R", 5), (c_wx, "c_wx", G, "G", 6))):
                    for tt in range(4):
                        pk = "ps%d" % (2 + (gi_ * 4 + tt) % 4)
                        pst = ps[2 + (gi_ * 4 + tt) % 4]
                        mm(pst[:, :], [(wt[:, hb, :], CB[:, tt * 512:(tt + 1) * 512])], reads=(wk, "CB"), writes=(pk,))

                        def f_sg(act, pst=pst, dstb=dstb, tt=tt, hb=hb, bcol=bcol):
                            return act.activation(out=dstb[:, tt * 512:(tt + 1) * 512], in_=pst[:, :], func=AF.Sigmoid,
                                                  bias=lv[:, hb, bcol:bcol + 1], scale=1.0)
                        S.op("act", f_sg, reads=(pk, "c_lruvec"), writes=(dk,))

                def f_a(act, hb=hb):
                    return act.activation(out=X[:, 3:3 + 2 * T], in_=R[:, :], func=AF.Exp, scale=c8[:, hb, 0:1])
                S.op("act", f_a, reads=("R", "c8"), writes=("X",))

                def f_a2(act, hb=hb):
                    return act.activation(out=R[:, :], in_=R[:, :], func=AF.Exp, scale=c8[:, hb, 1:2])
                S.op("act", f_a2, reads=("R", "c8"), writes=("R",))

                def f_om(dve):
                    return dve.tensor_scalar(out=R[:, :], in0=R[:, :], scalar1=1.0, scalar2=-1.0, op0=ALU.min, op1=ALU.mult)
                S.op("dve", f_om, reads=("R",), writes=("R",))

                def f_sq(act):
                    return act.activation(out=R[:, :], in_=R[:, :], func=AF.Sqrt, bias=1.0, scale=1.0)
                S.op("act", f_sq, reads=("R",), writes=("R",))

                def f_g2(pool):
                    return pool.tensor_tensor(out=G[:, :], in0=G[:, :], in1=C[:, :], op=ALU.mult)
                S.op("pool", f_g2, reads=("G", "C"), writes=("G",))

                def f_bx(dve):
                    return dve.tensor_tensor(out=G[:, :], in0=G[:, :], in1=R[:, :], op=ALU.mult)
                S.op("dve", f_bx, reads=("G", "R"), writes=("G",))

                def f_s1(dve):
                    return dve.tensor_tensor_scan(out=C[:, 0:T], data0=X[:, 3:3 + T], data1=G[:, 0:T], initial=0.0,
                                                  op0=ALU.mult, op1=ALU.add)
                S.op("dve", f_s1, reads=("X", "G", "C"), writes=("C",))

                def f_h0(dve):
                    return dve.tensor_scalar(out=hb0[:, :], in0=C[:, T - 1:T], scalar1=c_pf[:, 0:1], scalar2=None, op0=ALU.mult)
                S.op("dve", f_h0, reads=("C", "c_pf"), writes=("hb0",))

                def f_s2(dve):
                    return dve.tensor_tensor_scan(out=C[:, T:2 * T], data0=X[:, 3 + T:3 + 2 * T], data1=G[:, T:2 * T],
                                                  initial=hb0[:, 0:1], op0=ALU.mult, op1=ALU.add)
                S.op("dve", f_s2, reads=("X", "G", "C", "hb0"), writes=("C",))

                for tt in range(2):
                    pk = "ps%d" % (6 + tt)
                    mm(ps[6 + tt][:, :],
                       [(wslot[sl_y][:, dc, cc * 128:(cc + 1) * 128], xTo[:, dc, tt * 512:(tt + 1) * 512]) for dc in range(NDC)],
                       reads=(ky, "xTo"), writes=(pk,))
                    ysl = slice(tt * 512, (tt + 1) * 512)

                    def f_y(act, tt=tt, ysl=ysl):
                        return act.copy(out=R[:, ysl], in_=ps[6 + tt][:, :])
                    S.op("act", f_y, reads=(pk,), writes=("R",))

                    def f_y2(act, tt=tt, ysl=ysl):
                        return act.activation(out=G[:, ysl], in_=ps[6 + tt][:, :], func=AF.Square)
                    S.op("act", f_y2, reads=(pk,), writes=("G",))
                ysl = slice(0, T)

                def f_y3(dve):
                    return dve.scalar_tensor_tensor(out=G[:, ysl], in0=G[:, ysl], scalar=0.044715, in1=R[:, ysl],
                                                    op0=ALU.mult, op1=ALU.mult)
                S.op("dve", f_y3, reads=("G", "R"), writes=("G",))

                def f_y4(pool):
                    return pool.tensor_tensor(out=G[:, ysl], in0=G[:, ysl], in1=R[:, ysl], op=ALU.add)
                S.op("pool", f_y4, reads=("G", "R"), writes=("G",))

                def f_y5(act):
                    return act.activation(out=G[:, ysl], in_=G[:, ysl], func=AF.Sigmoid, scale=1.5957691216057308)
                S.op("act", f_y5, reads=("G",), writes=("G",))

                def f_y6(pool):
                    return pool.tensor_tensor(out=G[:, ysl], in0=G[:, ysl], in1=R[:, ysl], op=ALU.mult)
                S.op("pool", f_y6, reads=("G", "R"), writes=("G",))

                def f_y7(dve, hb=hb):
                    return dve.tensor_tensor(out=mixL[:, hb, :], in0=G[:, ysl], in1=C[:, T:2 * T], op=ALU.mult)
                S.op("dve", f_y7, reads=("G", "C"), writes=("mixL",))

            if stop_after == "lru":
                def f_d(dve):
                    return dve.tensor_copy(out=R[:, 0:T], in_=mixL[:, 7, :])
                S.op("dve", f_d, reads=("mixL",), writes=("R",))

                def f_d2(dve):
                    return dve.tensor_copy(out=R[:, T:2 * T], in_=mixL[:, 0, :])
                S.op("dve", f_d2, reads=("mixL",), writes=("R",))
                return dump([(R[:, 0:2 * T], 2 * T), (C[:, :], 2 * T)], ("R", "C"))
            S.barrier()
            S.flush()

        _, chunk_dec = _consts()
        with ExitStack() as s1b:
            c_ropep = sb("c_ropep", [128, 2, NTC, 64], F32, s1b)
            c_kdec = sb("c_kdec", [128, 8], F32, s1b)
            cload(c_ropep, rope_pre, "c_ropep")
            cload(c_kdec, kdec, "c_kdec")
            cfinal()
            k_p = arB[:, 4096:8192].bitcast(BF16).rearrange("p (c t) -> p c t", c=NTC)
            vd_p = sb("vd_p", [128, NTC, 1024], BF16, s1b)
            rt = [sb("rt%d" % i, [128, 4, 64], F32, s1b) for i in range(4)]

            def f_z(pool):
                return pool.memset(state_f[:, :, :], 0.0)
            S.op("pool", f_z, writes=tuple("st%d" % h for h in range(8)))

            def f_zb(pool):
                return pool.memset(state_b[:, :, :], 0.0)
            S.op("pool", f_zb, writes=tuple("sb%d" % h for h in range(8)))

            def rope_evac(pst, pk, cos_ap, sin_ap, dst_ap, dkey):
                pv = pst[:, :].rearrange("p (h two d) -> p h two d", h=4, two=2)
                dv = dst_ap.rearrange("p (h two d) -> p h two d", h=4, two=2)
                cb = cos_ap.broadcast_to([128, 4, 64])
                sn = sin_ap.broadcast_to([128, 4, 64])
                x1, x2 = pv[:, :, 0, :], pv[:, :, 1, :]
                for (ta, a, ca) in ((0, x1, cb), (1, x2, sn), (2, x1, sn), (3, x2, cb)):
                    def f(dve, ta=ta, a=a, ca=ca):
                        return dve.tensor_tensor(out=rt[ta][:, :, :], in0=a, in1=ca, op=ALU.mult)
                    S.op("dve", f, reads=(pk, "c_rope"), writes=("rt%d" % ta,))

                def f1(pool):
                    return pool.tensor_tensor(out=dv[:, :, 0, :], in0=rt[0][:, :, :], in1=rt[1][:, :, :], op=ALU.subtract)
                S.op("pool", f1, reads=("rt0", "rt1"), writes=(dkey,))

                def f2(pool):
                    return pool.tensor_tensor(out=dv[:, :, 1, :], in0=rt[2][:, :, :], in1=rt[3][:, :, :], op=ALU.add)
                S.op("pool", f2, reads=("rt2", "rt3"), writes=(dkey,))

            S.last_w["c_rope"] = S.last_w["c_ropep"]
            for half in range(2):
                slk = load_piece(w_in[:, (2 + half) * 512:(3 + half) * 512])
                slv = load_piece(w_in[:, (4 + half) * 512:(5 + half) * 512])
                for tc in range(NTC):
                    pk = "ps%d" % (tc % 2)
                    mm(ps[tc % 2][:, :], [(xTp[:, dc, tc * 128:(tc + 1) * 128], wslot[slk][:, dc, :]) for dc in range(NDC)],
                       reads=("xTp", "wslot%d" % slk), writes=(pk,))
                    rope_evac(ps[tc % 2], pk, c_ropep[:, 0, tc:tc + 1, :], c_ropep[:, 1, tc:tc + 1, :],
                              k_p[:, tc, half * 512:(half + 1) * 512], "k_p")
                for tc in range(NTC):
                    pk = "ps%d" % (2 + tc % 2)
                    pst = ps[2 + tc % 2]
                    mm(pst[:, :], [(xTp[:, dc, tc * 128:(tc + 1) * 128], wslot[slv][:, dc, :]) for dc in range(NDC)],
                       reads=("xTp", "wslot%d" % slv), writes=(pk,))
                    for h4 in range(4):
                        def f(act, pst=pst, tc=tc, h4=h4, half=half):
                            h = half * 4 + h4
                            return act.activation(out=vd_p[:, tc, h * 128:(h + 1) * 128], in_=pst[:, h4 * 128:(h4 + 1) * 128],
                                                  func=AF.Copy, scale=c_kdec[:, h:h + 1])
                        S.op("act", f, reads=(pk, "c_kdec"), writes=("vd_p",))
            for n in range(NTC):
                for h in range(8):
                    pk = "ps%d" % (4 + h % 4)
                    pst = ps[4 + h % 4]
                    mm(pst[:, 0:128], [(k_p[:, n, h * 128:(h + 1) * 128], vd_p[:, n, h * 128:(h + 1) * 128])],
                       reads=("k_p", "vd_p"), writes=(pk,))

                    def f(dve, pst=pst, h=h):
                        return dve.scalar_tensor_tensor(out=state_f[:, h, :], in0=state_f[:, h, :], scalar=float(chunk_dec[h]),
                                                        in1=pst[:, 0:128], op0=ALU.mult, op1=ALU.add)
                    S.op("dve", f, reads=(pk, "st%d" % h), writes=("st%d" % h,))
            for h in range(8):
                def f(act, h=h):
                    return act.copy(out=state_b[:, h, :], in_=state_f[:, h, :])
                S.op("act", f, reads=("st%d" % h,), writes=("sb%d" % h,))
            if stop_after == "pre":
                return dump([(state_f[:, :, :].rearrange("p h e -> p (h e)"), 1024)], tuple("st%d" % h for h in range(8)))
            S.barrier()
            S.flush()

    with ExitStack() as s2:
        c_rope = sb("c_rope", [128, 4, NTC, 64], F32, s2)
        c_kdec = sb("c_kdec2", [128, 8], F32, s2)
        c_decT = sb("c_decT", [128, 8, 128], F32, s2)
        c_gnw = sb("c_gnw", [128, 1024], F32, s2)
        cload(c_rope, rope_own, "c_rope")
        cload(c_kdec, kdec, "c_kdec")
        cload(c_decT, decT, "c_decT")
        cload(c_gnw, rep_gnw, "c_gnw")
        cfinal()
        qkvv = arA[:, 8192:16384].bitcast(BF16).rearrange("p (w c t) -> p w c t", w=4, c=NTC)
        q_o, k_o, v_o, vd_o = qkvv[:, 0], qkvv[:, 1], qkvv[:, 2], qkvv[:, 3]
        sg_o = sb("sg_o", [128, NTC, 512], BF16, s2)
        rt = [sb("rt2_%d" % i, [128, 4, 64], F32, s2) for i in range(4)]
        trb = [sb("trb%d" % i, [128, 384], BF16, s2) for i in range(2)]
        sdt = [sb("sdt%d" % i, [128, 128], BF16, s2) for i in range(2)]
        gst = sb("gst", [128, 4, 6], F32, s2)
        gmv = sb("gmv", [128, 4, 2], F32, s2)
        grs = sb("grs", [128, 4], F32, s2)
        rn = sb("rn", [128, 512], F32, s2)
        mtok = sb("mtok", [128, 512], BF16, s2)

        def rope_evac2(pst, pk, cos_ap, sin_ap, dst_ap, dkey):
            pv = pst[:, :].rearrange("p (h two d) -> p h two d", h=4, two=2)
            dv = dst_ap.rearrange("p (h two d) -> p h two d", h=4, two=2)
            cb = cos_ap.broadcast_to([128, 4, 64])
            sn = sin_ap.broadcast_to([128, 4, 64])
            x1, x2 = pv[:, :, 0, :], pv[:, :, 1, :]
            for (ta, a, ca) in ((0, x1, cb), (1, x2, sn), (2, x1, sn), (3, x2, cb)):
                def f(dve, ta=ta, a=a, ca=ca):
                    return dve.tensor_tensor(out=rt[ta][:, :, :], in0=a, in1=ca, op=ALU.mult)
                S.op("dve", f, reads=(pk, "c_rope"), writes=("rt%d" % ta,))

            def f1(pool):
                return pool.tensor_tensor(out=dv[:, :, 0, :], in0=rt[0][:, :, :], in1=rt[1][:, :, :], op=ALU.subtract)
            S.op("pool", f1, reads=("rt0", "rt1"), writes=(dkey,))

            def f2(pool):
                return pool.tensor_tensor(out=dv[:, :, 1, :], in0=rt[2][:, :, :], in1=rt[3][:, :, :], op=ALU.add)
            S.op("pool", f2, reads=("rt2", "rt3"), writes=(dkey,))

        ident_b = c_qdiag[:, 0, :]
        for hg in range(2):
            slq = load_piece(w_in[:, (0 + hg) * 512:(1 + hg) * 512])
            slk = load_piece(w_in[:, (2 + hg) * 512:(3 + hg) * 512])
            slv = load_piece(w_in[:, (4 + hg) * 512:(5 + hg) * 512])
            slg = load_piece(w_in[:, (6 + hg) * 512:(7 + hg) * 512])
            cnt = 0
            for which, sl in (("q", slq), ("k", slk), ("v", slv), ("g", slg)):
                for tc in range(NTC):
                    pb = cnt % 2
                    cnt += 1
                    pk = "ps%d" % pb
                    pst = ps[pb]
                    mm(pst[:, :], [(xTo[:, dc, tc * 128:(tc + 1) * 128], wslot[sl][:, dc, :]) for dc in range(NDC)],
                       reads=("xTo", "wslot%d" % sl), writes=(pk,))
                    if which == "q":
                        rope_evac2(pst, pk, c_rope[:, 0, tc:tc + 1, :], c_rope[:, 1, tc:tc + 1, :], q_o[:, tc, :], "q_o")
                    elif which == "k":
                        rope_evac2(pst, pk, c_rope[:, 2, tc:tc + 1, :], c_rope[:, 3, tc:tc + 1, :], k_o[:, tc, :], "k_o")
                    elif which == "v":
                        def f(act, pst=pst, tc=tc):
                            return act.copy(out=v_o[:, tc, :], in_=pst[:, :])
                        S.op("act", f, reads=(pk,), writes=("v_o",))
                        for h4 in range(4):
                            def f(act, pst=pst, tc=tc, h4=h4, hg=hg):
                                h = hg * 4 + h4
                                return act.activation(out=vd_o[:, tc, h4 * 128:(h4 + 1) * 128], in_=pst[:, h4 * 128:(h4 + 1) * 128],
                                                      func=AF.Copy, scale=c_kdec[:, h:h + 1])
                            S.op("act", f, reads=(pk, "c_kdec"), writes=("vd_o",))
                    else:
                        def f(act, pst=pst, tc=tc):
                            return act.activation(out=sg_o[:, tc, :], in_=pst[:, :], func=AF.Silu)
                        S.op("act", f, reads=(pk,), writes=("sg_o",))
            for n in range(NTC):
                rb = 6 + n % 2
                rk = "ps%d" % rb
                for h4 in range(4):
                    h = hg * 4 + h4
                    hs = slice(h4 * 128, (h4 + 1) * 128)
                    tb = 2 + h4 % 2
                    tk = "ps%d" % tb
                    trs = trb[h4 % 2]
                    trk = "trb%d" % (h4 % 2)

                    def f_tr(pe, tb=tb, n=n, hs=hs, h=h):
                        pe.matmul(ps[tb][:, 0:128], q_o[:, n, hs], ident_b, start=True, stop=True)
                        pe.matmul(ps[tb][:, 128:256], q_o[:, n, hs], c_qdiag[:, 1 + h, :], start=True, stop=True)
                        return pe.matmul(ps[tb][:, 256:384], k_o[:, n, hs], ident_b, start=True, stop=True)
                    S.op("pe", f_tr, reads=("q_o", "k_o", "c_qdiag"), writes=(tk,))

                    def f_te(act, tb=tb, trs=trs):
                        return act.copy(out=trs[:, :], in_=ps[tb][:, 0:384])
                    S.op("act", f_te, reads=(tk,), writes=(trk,))
                    sb_ = 4 + h4 % 2
                    sk = "ps%d" % sb_
                    mm(ps[sb_][:, 0:128], [(trs[:, 256:384], trs[:, 0:128])], reads=(trk,), writes=(sk,))
                    sd = sdt[h4 % 2]
                    sdk = "sdt%d" % (h4 % 2)

                    def f_sd(dve, sb_=sb_, sd=sd, h=h):
                        return dve.tensor_tensor(out=sd[:, :], in0=ps[sb_][:, 0:128], in1=c_decT[:, h, :], op=ALU.mult)
                    S.op("dve", f_sd, reads=(sk, "c_decT"), writes=(sdk,))
                    mm(ps[rb][:, hs], [(sd[:, :], v_o[:, n, hs]), (trs[:, 128:256], state_b[:, h, :])],
                       reads=(sdk, "v_o", trk, "sb%d" % h), writes=(rk,))
                    mm(ps[tb][:, 384:512], [(k_o[:, n, hs], vd_o[:, n, hs])], reads=("k_o", "vd_o"), writes=(tk,))

                    def f_su(dve, tb=tb, h=h):
                        return dve.scalar_tensor_tensor(out=state_f[:, h, :], in0=state_f[:, h, :], scalar=float(chunk_dec[h]),
                                                        in1=ps[tb][:, 384:512], op0=ALU.mult, op1=ALU.add)
                    S.op("dve", f_su, reads=(tk, "st%d" % h), writes=("st%d" % h,))

                    def f_sbc(act, h=h):
                        return act.copy(out=state_b[:, h, :], in_=state_f[:, h, :])
                    S.op("act", f_sbc, reads=("st%d" % h,), writes=("sb%d" % h,))
                for h4 in range(4):
                    def f_bs(dve, h4=h4, rb=rb):
                        return dve.bn_stats(out=gst[:, h4, :], in_=ps[rb][:, h4 * 128:(h4 + 1) * 128])
                    S.op("dve", f_bs, reads=(rk,), writes=("gst",))
                for h4 in range(4):
                    def f_ba(dve, h4=h4):
                        return dve.bn_aggr(out=gmv[:, h4, :], in_=gst[:, h4, :])
                    S.op("dve", f_ba, reads=("gst",), writes=("gmv",))

                def f_sd2(act):
                    return act.activation(out=grs[:, :], in_=gmv[:, :, 1], func=AF.Sqrt, bias=c_eps[:, 0:1], scale=1.0)
                S.op("act", f_sd2, reads=("gmv", "c_eps"), writes=("grs",))

                def f_rc(dve):
                    return dve.reciprocal(out=grs[:, :], in_=grs[:, :])
                S.op("dve", f_rc, reads=("grs",), writes=("grs",))
                for h4 in range(4):
                    def f_nm(dve, h4=h4, rb=rb):
                        return dve.tensor_scalar(out=rn[:, h4 * 128:(h4 + 1) * 128], in0=ps[rb][:, h4 * 128:(h4 + 1) * 128],
                                                 scalar1=gmv[:, h4, 0:1], scalar2=grs[:, h4:h4 + 1], op0=ALU.subtract, op1=ALU.mult)
                    S.op("dve", f_nm, reads=(rk, "gmv", "grs"), writes=("rn",))

                def f_gw(pool, hg=hg):
                    return pool.tensor_tensor(out=rn[:, :], in0=rn[:, :], in1=c_gnw[:, hg * 512:(hg + 1) * 512], op=ALU.mult)
                S.op("pool", f_gw, reads=("rn", "c_gnw"), writes=("rn",))

                def f_sgm(pool, n=n):
                    return pool.tensor_tensor(out=mtok[:, :], in0=rn[:, :], in1=sg_o[:, n, :], op=ALU.mult)
                S.op("pool", f_sgm, reads=("rn", "sg_o"), writes=("mtok",))
                mb = n % 2
                mk = "ps%d" % mb

                def f_mt(pe, mb=mb):
                    ins = None
                    for h4 in range(4):
                        ins = pe.matmul(ps[mb][:, h4 * 128:(h4 + 1) * 128], mtok[:, h4 * 128:(h4 + 1) * 128], ident_b, start=True, stop=True)
                    return ins
                S.op("pe", f_mt, reads=("mtok", "c_qdiag"), writes=(mk,))

                def f_me(act, mb=mb, hg=hg, n=n):
                    return act.copy(out=mixR[:, hg * 4:hg * 4 + 4, n * 128:(n + 1) * 128],
                                    in_=ps[mb][:, :].rearrange("p (h i) -> p h i", h=4))
                S.op("act", f_me, reads=(mk,), writes=("mixR",))
        if stop_after == "ret":
            dbgbuf = sb("dbgbuf", [128, T], F32, s2)

            def f_d(dve):
                return dve.tensor_copy(out=dbgbuf[:, 0:512], in_=mixR[:, 0, 0:512])
            S.op("dve", f_d, reads=("mixR",), writes=("dbgbuf",))

            def f_d2(dve):
                return dve.tensor_copy(out=dbgbuf[:, 512:T], in_=mixR[:, 5, 512:T])
            S.op("dve", f_d2, reads=("mixR",), writes=("dbgbuf",))
            return dump([(dbgbuf[:, 0:T], T)], ("dbgbuf",))
        S.barrier()
        S.flush()

    s12.close()

    srcs = [w_out[:, dg * 512:(dg + 1) * 512] for dg in range(4)]
    for e in range(n_exp):
        for fg in range(4):
            ee = e % NEW
            srcs.append(w_gate[ee // EG][ee % EG, fg])
            srcs.append(w_up[ee // EG][ee % EG, fg])
        for dg in range(4):
            srcs.append(w_down[ee // EG][ee % EG, dg])
    for dg in range(4):
        srcs.append(w_pg[:, dg * 512:(dg + 1) * 512])
    stream = {"issued": 0, "slots": {}}

    def get_pieces(k, n=1):
        while stream["issued"] < min(k + NSLOT, len(srcs)):
            i = stream["issued"]
            stream["slots"][i] = load_piece(srcs[i])
            stream["issued"] += 1
        return [stream["slots"][k + i] for i in range(n)]

    def get_piece(k):
        return get_pieces(k, 1)[0]

    def layer_norm(src_ap, skey, w_t, b_t, wkeys, dst_ap, dkey, lst, lmv, lrs, tag):
        for k4 in range(4):
            def f(dve, k4=k4):
                return dve.bn_stats(out=lst[:, k4, :], in_=src_ap[:, k4 * 512:(k4 + 1) * 512])
            S.op("dve", f, reads=(skey,), writes=("lst" + tag,))

        def f(dve):
            return dve.bn_aggr(out=lmv[:, :], in_=lst[:, :, :].rearrange("p a b -> p (a b)"))
        S.op("dve", f, reads=("lst" + tag,), writes=("lmv" + tag,))

        def f(act):
            return act.activation(out=lrs[:, :], in_=lmv[:, 1:2], func=AF.Sqrt, bias=c_eps[:, 0:1], scale=1.0)
        S.op("act", f, reads=("lmv" + tag, "c_eps"), writes=("lrs" + tag,))

        def f(dve):
            return dve.reciprocal(out=lrs[:, :], in_=lrs[:, :])
        S.op("dve", f, reads=("lrs" + tag,), writes=("lrs" + tag,))

        def f(dve):
            return dve.tensor_scalar(out=dst_ap, in0=src_ap, scalar1=lmv[:, 0:1], scalar2=lrs[:, 0:1],
                                     op0=ALU.subtract, op1=ALU.mult)
        S.op("dve", f, reads=(skey, "lmv" + tag, "lrs" + tag), writes=(dkey,))

        def f(pool):
            return pool.tensor_tensor(out=dst_ap, in0=dst_ap, in1=w_t[:, :], op=ALU.mult)
        S.op("pool", f, reads=(dkey,) + wkeys, writes=(dkey,))

        def f(pool):
            return pool.tensor_tensor(out=dst_ap, in0=dst_ap, in1=b_t[:, :], op=ALU.add)
        S.op("pool", f, reads=(dkey,) + wkeys, writes=(dkey,))

    gates = sb("gates", [128, NTC, NE])
    posm = sb("posm", [128, NTC, NE])
    posmT = sb("posmT", [NE, T], BF16)
    c_bgu = sb("c_bgu", [128, 2, NE, 16])
    c_iotac = sb("c_iotac", [128, CAP])
    c_iotap = sb("c_iotap", [128, 2])
    c_pidx = sb("c_pidx", [NE, 128])
    cload(c_bgu, bgu, "c_bgu")
    cload(c_iotac, iota_c, "c_iotac")
    cload(c_iotap, iota_p, "c_iotap")
    cload(c_pidx, pidx_in, "c_pidx")
    cfinal()

    with ExitStack() as s3:
        xcb = [sb("xcb%d" % i, [128, 512], F32, s3) for i in range(2)]
        cnt = 0
        for dg in range(4):
            sl = get_piece(dg)
            for tc in range(NTC):
                pb = cnt % 2
                xb = xcb[cnt % 2]
                xk = "xcb%d" % (cnt % 2)
                cnt += 1

                def f(eng, sem, xb=xb, tc=tc, dg=dg):
                    eng.dma_start(out=xb[:, :], in_=x_own[tc * 128:(tc + 1) * 128, dg * 512:(dg + 1) * 512]).then_inc(sem, 16)
                S.dma("sp", f, 1, xk, writes=(xk,))
                pk = "ps%d" % pb
                mm(ps[pb][:, :],
                   [((mixR if fc < 8 else mixL)[:, fc % 8, tc * 128:(tc + 1) * 128], wslot[sl][:, fc, :]) for fc in range(NDC)],
                   reads=("mixR", "mixL", "wslot%d" % sl), writes=(pk,))

                def f(dve, xb=xb, pb=pb, tc=tc, dg=dg):
                    return dve.scalar_tensor_tensor(out=acc[:, tc, dg * 512:(dg + 1) * 512], in0=xb[:, :], scalar=DN_ALPHA,
                                                    in1=ps[pb][:, :], op0=ALU.mult, op1=ALU.add)
                S.op("dve", f, reads=(xk, pk), writes=("acc%d" % tc,))
        S.barrier()
        S.flush()

    with ExitStack() as s3b:
        c_lw = sb("c_lw", [128, DM], F32, s3b)
        c_lb = sb("c_lb", [128, DM], F32, s3b)
        c_wr = sb("c_wr", [128, NDC, NE], F32, s3b)
        c_br = sb("c_br", [128, NE], F32, s3b)
        c_tri = sb("c_tri", [128, 2, 128], BF16, s3b)
        cload(c_lw, rep_ln1w, "c_lw")
        cload(c_lb, rep_ln1b, "c_lb")
        cload(c_wr, w_r.rearrange("(c p) e -> p c e", p=128), "c_wr")
        cload(c_br, rep_br, "c_br")
        cload(c_tri, tri, "c_tri")
        cfinal()
        lst = sb("lst", [128, 4, 6], F32, s3b)
        lmv = sb("lmv", [128, 2], F32, s3b)
        lrs = sb("lrs", [128, 1], F32, s3b)
        x1Tq = [sb("x1Tq%d" % i, [128, 2, 4, 128], BF16, s3b) for i in range(2)]
        xlo = sb("xlo", [128, DM], BF16, s3b)
        wr_hl = sb("wr_hl", [128, 2, NDC, NE], BF16, s3b)
        logit = sb("logit", [128, NTC, NE], F32, s3b)
        top8 = sb("top8", [128, NTC, 8], F32, s3b)
        nmx = sb("nmx", [128, NTC], F32, s3b)
        mask = sb("mask", [128, NTC, NE], F32, s3b)
        maskb = sb("maskb", [128, NTC, NE], BF16, s3b)
        posmb = sb("posmb", [128, NTC, NE], BF16, s3b)
        den = sb("den", [128, NTC], F32, s3b)
        def f(act):
            return act.copy(out=wr_hl[:, 0, :, :], in_=c_wr[:, :, :])
        S.op("act", f, reads=("c_wr",), writes=("wr_hi",))

        def f(dve):
            return dve.tensor_tensor(out=wr_hl[:, 1, :, :], in0=c_wr[:, :, :], in1=wr_hl[:, 0, :, :], op=ALU.subtract)
        S.op("dve", f, reads=("c_wr", "wr_hi"), writes=("wr_lo",))
        for tc in range(NTC):
            ak = "acc%d" % tc
            x1f = acc[:, tc, :]
            layer_norm(x1f, ak, c_lw, c_lb, ("c_lw", "c_lb"), x1f, ak, lst, lmv, lrs, "1")

            def f(act, tc=tc, x1f=x1f):
                return act.copy(out=x1bf[:, tc, :], in_=x1f)
            S.op("act", f, reads=(ak,), writes=("x1bf",))
            def f(dve, tc=tc, x1f=x1f):
                return dve.tensor_tensor(out=xlo[:, :], in0=x1f, in1=x1bf[:, tc, :], op=ALU.subtract)
            S.op("dve", f, reads=(ak, "x1bf"), writes=("xlo",))
            for q4 in range(4):
                xq = x1Tq[q4 % 2]
                xqk = "x1Tq%d" % (q4 % 2)
                for hl in range(2):
                    pb = hl
                    pk = "ps%d" % pb

                    def f(pe, q4=q4, pb=pb, hl=hl, tc=tc):
                        ins = None
                        for i in range(4):
                            dc = q4 * 4 + i
                            src = x1bf[:, tc, dc * 128:(dc + 1) * 128] if hl == 0 else xlo[:, dc * 128:(dc + 1) * 128]
                            ins = pe.matmul(ps[pb][:, i * 128:(i + 1) * 128], src, c_qdiag[:, 0, :], start=True, stop=True)
                        return ins
                    S.op("pe", f, reads=("x1bf", "xlo", "c_qdiag"), writes=(pk,))

                    def f(act, xq=xq, pb=pb, hl=hl):
                        return act.copy(out=xq[:, hl, :, :], in_=ps[pb][:, :].rearrange("p (a t) -> p a t", a=4))
                    S.op("act", f, reads=(pk,), writes=(xqk,))

                def f(pe, q4=q4, xq=xq):
                    ins = None
                    for i in range(4):
                        dc = q4 * 4 + i
                        for j, (xh, wh) in enumerate(((0, 0), (0, 1), (1, 0))):
                            ins = pe.matmul(ps[2][:, 0:NE], xq[:, xh, i, :], wr_hl[:, wh, dc, :],
                                            start=(dc == 0 and j == 0), stop=(dc == NDC - 1 and j == 2))
                    return ins
                S.op("pe", f, reads=(xqk, "wr_hi", "wr_lo"), writes=("ps2",))

            def f(act, tc=tc, x1f=x1f):
                return act.mul(out=x1f, in_=x1f, mul=DN_ALPHA)
            S.op("act", f, reads=(ak,), writes=(ak,))

            def f(dve, tc=tc):
                return dve.tensor_tensor(out=logit[:, tc, :], in0=ps[2][:, 0:NE], in1=c_br[:, :], op=ALU.add)
            S.op("dve", f, reads=("ps2", "c_br"), writes=("logit",))
        if stop_after == "ln1":
            return dump([(acc[:, 0, :], DM), (acc[:, 7, :], DM), (logit[:, :, :].rearrange("p a b -> p (a b)"), NTC * NE)],
                        ("acc0", "acc7", "logit"))
        for tc in range(NTC):
            def f(dve, tc=tc):
                return dve.max(out=top8[:, tc, :], in_=logit[:, tc, :])
            S.op("dve", f, reads=("logit",), writes=("top8",))
        for tc in range(NTC):
            def f(dve, tc=tc):
                return dve.tensor_scalar(out=mask[:, tc, :], in0=logit[:, tc, :], scalar1=top8[:, tc, 3:4], scalar2=None, op0=ALU.is_ge)
            S.op("dve", f, reads=("logit", "top8"), writes=("mask",))

        def f(dve):
            return dve.tensor_scalar(out=nmx[:, :], in0=top8[:, :, 0], scalar1=-1.0, scalar2=None, op0=ALU.mult)
        S.op("dve", f, reads=("top8",), writes=("nmx",))
        for tc in range(NTC):
            def f(act, tc=tc):
                return act.activation(out=gates[:, tc, :], in_=logit[:, tc, :], func=AF.Exp, bias=nmx[:, tc:tc + 1], scale=1.0)
            S.op("act", f, reads=("logit", "nmx"), writes=("gates",))

        def f(dve):
            return dve.tensor_tensor(out=gates[:, :, :], in0=gates[:, :, :], in1=mask[:, :, :], op=ALU.mult)
        S.op("dve", f, reads=("gates", "mask"), writes=("gates",))

        def f(dve):
            return dve.tensor_reduce(out=den[:, :], in_=gates[:, :, :], axis=mybir.AxisListType.X, op=ALU.add)
        S.op("dve", f, reads=("gates",), writes=("den",))

        def f(dve):
            return dve.reciprocal(out=den[:, :], in_=den[:, :])
        S.op("dve", f, reads=("den",), writes=("den",))
        for tc in range(NTC):
            def f(dve, tc=tc):
                return dve.tensor_scalar(out=gates[:, tc, :], in0=gates[:, tc, :], scalar1=den[:, tc:tc + 1], scalar2=None, op0=ALU.mult)
            S.op("dve", f, reads=("gates", "den"), writes=("gates",))

        def f(act):
            return act.copy(out=maskb[:, :, :], in_=mask[:, :, :])
        S.op("act", f, reads=("mask",), writes=("maskb",))
        for tc in range(NTC):
            pb = 4 + tc % 2
            pk = "ps%d" % pb
            pairs = [(c_tri[:, 0, :], maskb[:, t2, :]) for t2 in range(tc)] + [(c_tri[:, 1, :], maskb[:, tc, :])]
            mm(ps[pb][:, 0:NE], pairs, reads=("maskb", "c_tri"), writes=(pk,))

            def f(dve, tc=tc, pb=pb):
                return dve.scalar_tensor_tensor(out=posm[:, tc, :], in0=ps[pb][:, 0:NE], scalar=1.0, in1=mask[:, tc, :],
                                                op0=ALU.add, op1=ALU.mult)
            S.op("dve", f, reads=(pk, "mask"), writes=("posm",))

        def f(dve):
            return dve.tensor_scalar(out=posm[:, :, :], in0=posm[:, :, :], scalar1=-1.0, scalar2=float(CAP), op0=ALU.add, op1=ALU.min)
        S.op("dve", f, reads=("posm",), writes=("posm",))

        def f(act):
            return act.copy(out=posmb[:, :, :], in_=posm[:, :, :])
        S.op("act", f, reads=("posm",), writes=("posmb",))
        for half in range(2):
            pb = 6 + half
            pk = "ps%d" % pb

            def f(pe, half=half, pb=pb):
                ins = None
                for i in range(4):
                    tc = half * 4 + i
                    ins = pe.matmul(ps[pb][0:NE, i * 128:(i + 1) * 128], posmb[:, tc, :], c_qdiag[:, 0, :], start=True, stop=True)
                return ins
            S.op("pe", f, reads=("posmb", "c_qdiag"), writes=(pk,))

            def f(act, half=half, pb=pb):
                return act.copy(out=posmT[:, half * 512:(half + 1) * 512], in_=ps[pb][0:NE, :])
            S.op("act", f, reads=(pk,), writes=("posmT",))
        if stop_after == "route":
            return dump([(gates[:, :, :].rearrange("p a b -> p (a b)"), NTC * NE), (posm[:, :, :].rearrange("p a b -> p (a b)"), NTC * NE)],
                        ("gates", "posm"))
        S.barrier()
        S.flush()

    with ExitStack() as s4:
        sel_e = [sb("sel_e%d" % i, [NE, 128], BF16, s4) for i in range(2)]
        Sg = sb("Sg", [128, NTC, CAP], BF16, s4)
        ST = sb("ST", [128, 2, T], BF16, s4)
        XeT = sb("XeT", [128, NDC, CAP], BF16, s4)
        HT = sb("HT", [128, NDC, CAP], BF16, s4)
        Yb = [sb("Yb%d" % i, [128, 2, 512], BF16, s4) for i in range(2)]
        tA = [sb("tA%d" % i, [128, CAP], F32, s4) for i in range(2)]
        tB = [sb("tB0", [128, CAP], F32, s4)] * 2
        tC = [sb("tC%d" % i, [128, CAP], F32, s4) for i in range(2)]
        tD = [sb("tD0", [128, CAP], F32, s4)] * 2

        def scatter(e, dg):
            yb = Yb[dg % 2]
            yk = "Yb%d" % (dg % 2)
            for tc in range(NTC):
                pb = 6 + tc % 2
                pk = "ps%d" % pb
                mm(ps[pb][:, :], [(ST[:, jc, tc * 128:(tc + 1) * 128], yb[:, jc, :]) for jc in range(2)],
                   reads=("ST", yk), writes=(pk,))

                def f(dve, pb=pb, tc=tc, e=e, dg=dg):
                    return dve.scalar_tensor_tensor(out=acc[:, tc, dg * 512:(dg + 1) * 512], in0=ps[pb][:, :],
                                                    scalar=gates[:, tc, e:e + 1], in1=acc[:, tc, dg * 512:(dg + 1) * 512],
                                                    op0=ALU.mult, op1=ALU.add)
                S.op("dve", f, reads=(pk, "gates", "acc%d" % tc), writes=("acc%d" % tc,))

        for e in range(n_exp):
            base = 4 + e * 12
            for tc in range(NTC):
                def f(pool, tc=tc, e=e):
                    return pool.tensor_scalar(out=Sg[:, tc, :], in0=c_iotac[:, :], scalar1=posm[:, tc, e:e + 1], scalar2=None, op0=ALU.is_equal)
                S.op("pool", f, reads=("c_iotac", "posm"), writes=("Sg",))
            se = sel_e[e % 2]
            sek = "sel_e%d" % (e % 2)

            def f(pool, se=se, e=e):
                return pool.tensor_scalar(out=se[:, :], in0=c_pidx[:, :], scalar1=float(e), scalar2=None, op0=ALU.is_equal)
            S.op("pool", f, reads=("c_pidx",), writes=(sek,))
            for half in range(2):
                pb = half
                pk = "ps%d" % pb
                mm(ps[pb][:, :], [(se[:, :], posmT[:, half * 512:(half + 1) * 512])], reads=(sek, "posmT"), writes=(pk,))
                for jc in range(2):
                    def f(dve, pb=pb, jc=jc, half=half):
                        return dve.tensor_scalar(out=ST[:, jc, half * 512:(half + 1) * 512], in0=ps[pb][:, :],
                                                 scalar1=c_iotap[:, jc:jc + 1], scalar2=None, op0=ALU.is_equal)
                    S.op("dve", f, reads=(pk, "c_iotap"), writes=("ST",))
            for dp in range(NDC // 2):
                pb = dp % 2
                pk = "ps%d" % pb

                def f(pe, dp=dp, pb=pb):
                    ins = None
                    for i in range(2):
                        dc = dp * 2 + i
                        for tc in range(NTC):
                            ins = pe.matmul(ps[pb][:, i * CAP:(i + 1) * CAP], x1bf[:, tc, dc * 128:(dc + 1) * 128], Sg[:, tc, :],
                                            start=(tc == 0), stop=(tc == NTC - 1))
                    return ins
                S.op("pe", f, reads=("x1bf", "Sg"), writes=(pk,))

                def f(act, dp=dp, pb=pb):
                    return act.copy(out=XeT[:, dp * 2:dp * 2 + 2, :], in_=ps[pb][:, :].rearrange("p (a j) -> p a j", a=2))
                S.op("act", f, reads=(pk,), writes=("XeT",))
            for fg in range(4):
                slg, slu = get_pieces(base + fg * 2, 2)
                for fcl in range(4):
                    fc = fg * 4 + fcl
                    par = fc % 2
                    pb = 2 + par
                    pk = "ps%d" % pb

                    def f(pe, pb=pb, slg=slg, slu=slu, fcl=fcl):
                        ins = None
                        for dc in range(NDC):
                            ins = pe.matmul(ps[pb][:, 0:CAP], wslot[slg][:, dc, fcl * 128:(fcl + 1) * 128], XeT[:, dc, :],
                                            start=(dc == 0), stop=(dc == NDC - 1))
                        for dc in range(NDC):
                            ins = pe.matmul(ps[pb][:, CAP:2 * CAP], wslot[slu][:, dc, fcl * 128:(fcl + 1) * 128], XeT[:, dc, :],
                                            start=(dc == 0), stop=(dc == NDC - 1))
                        return ins
                    S.op("pe", f, reads=("XeT", "wslot%d" % slg, "wslot%d" % slu), writes=(pk,))
                    a_, b_, c_, d_ = tA[par], tB[par], tC[par], tD[par]
                    ka, kb, kc, kd = "tA%d" % par, "tB0", "tC%d" % par, "tD0"

                    def f(dve, pb=pb, a_=a_, e=e, fc=fc):
                        return dve.tensor_scalar(out=a_[:, :], in0=ps[pb][:, 0:CAP], scalar1=c_bgu[:, 0, e, fc:fc + 1], scalar2=7.0,
                                                 op0=ALU.add, op1=ALU.min)
                    S.op("dve", f, reads=(pk, "c_bgu"), writes=(ka,))

                    def f(act, a_=a_, b_=b_):
                        return act.activation(out=b_[:, :], in_=a_[:, :], func=AF.Sigmoid, scale=1.702)
                    S.op("act", f, reads=(ka,), writes=(kb,))

                    def f(dve, pb=pb, c_=c_, e=e, fc=fc):
                        return dve.tensor_scalar(out=c_[:, :], in0=ps[pb][:, CAP:2 * CAP], scalar1=c_bgu[:, 1, e, fc:fc + 1], scalar2=7.0,
                                                 op0=ALU.add, op1=ALU.min)
                    S.op("dve", f, reads=(pk, "c_bgu"), writes=(kc,))

                    def f(dve, c_=c_):
                        return dve.tensor_scalar(out=c_[:, :], in0=c_[:, :], scalar1=-7.0, scalar2=1.0, op0=ALU.max, op1=ALU.add)
                    S.op("dve", f, reads=(kc,), writes=(kc,))

                    def f(pool, a_=a_, b_=b_, d_=d_):
                        return pool.tensor_tensor(out=d_[:, :], in0=a_[:, :], in1=b_[:, :], op=ALU.mult)
                    S.op("pool", f, reads=(ka, kb), writes=(kd,))

                    def f(pool, c_=c_, d_=d_, fc=fc):
                        return pool.tensor_tensor(out=HT[:, fc, :], in0=d_[:, :], in1=c_[:, :], op=ALU.mult)
                    S.op("pool", f, reads=(kc, kd), writes=("HT",))
            for dg in range(4):
                sld = get_piece(base + 8 + dg)
                yb = Yb[dg % 2]
                yk = "Yb%d" % (dg % 2)
                for jc in range(2):
                    pb = 4 + jc
                    pk = "ps%d" % pb
                    mm(ps[pb][:, :], [(HT[:, fc, jc * 128:(jc + 1) * 128], wslot[sld][:, fc, :]) for fc in range(NDC)],
                       reads=("HT", "wslot%d" % sld), writes=(pk,))

                    def f(act, pb=pb, yb=yb, jc=jc):
                        return act.copy(out=yb[:, jc, :], in_=ps[pb][:, :])
                    S.op("act", f, reads=(pk,), writes=(yk,))
                if dg >= 1:
                    scatter(e, dg - 1)
            scatter(e, 3)
        S.barrier()
        S.flush()
    with ExitStack() as s4:
        gatesT = sb("gatesT", [NE, 2, T], BF16, s4)
        g_hl = sb("g_hl", [128, 2, NTC, NE], BF16, s4)
        c_bdn = sb("c_bdn", [NE, DM], F32, s4)
        b_hl = sb("b_hl", [NE, 2, DM], BF16, s4)
        cload(c_bdn, b_down, "c_bdn")
        cfinal()

        def f(act):
            return act.copy(out=g_hl[:, 0, :, :], in_=gates[:, :, :])
        S.op("act", f, reads=("gates",), writes=("g_hi",))

        def f(dve):
            return dve.tensor_tensor(out=g_hl[:, 1, :, :], in0=gates[:, :, :], in1=g_hl[:, 0, :, :], op=ALU.subtract)
        S.op("dve", f, reads=("gates", "g_hi"), writes=("g_lo",))

        def f(act):
            return act.copy(out=b_hl[:, 0, :], in_=c_bdn[:, :])
        S.op("act", f, reads=("c_bdn",), writes=("b_hi",))

        def f(dve):
            return dve.tensor_tensor(out=b_hl[:, 1, :], in0=c_bdn[:, :], in1=b_hl[:, 0, :], op=ALU.subtract)
        S.op("dve", f, reads=("c_bdn", "b_hi"), writes=("b_lo",))
        for hl in range(2):
            for half in range(2):
                pb = 2 + half
                pk = "ps%d" % pb

                def f(pe, half=half, pb=pb, hl=hl):
                    ins = None
                    for i in range(4):
                        tc = half * 4 + i
                        ins = pe.matmul(ps[pb][0:NE, i * 128:(i + 1) * 128], g_hl[:, hl, tc, :], c_qdiag[:, 0, :], start=True, stop=True)
                    return ins
                S.op("pe", f, reads=("g_hi", "g_lo", "c_qdiag"), writes=(pk,))

                def f(act, half=half, pb=pb, hl=hl):
                    return act.copy(out=gatesT[:, hl, half * 512:(half + 1) * 512], in_=ps[pb][0:NE, :])
                S.op("act", f, reads=(pk,), writes=("gatesT",))
        for tc in range(NTC):
            for dg in range(4):
                pb = (tc * 4 + dg) % 2
                pk = "ps%d" % pb
                mm(ps[pb][:, :], [(gatesT[:, gh, tc * 128:(tc + 1) * 128], b_hl[:, bh, dg * 512:(dg + 1) * 512])
                                  for gh, bh in ((0, 0), (0, 1), (1, 0))],
                   reads=("gatesT", "b_hi", "b_lo"), writes=(pk,))

                def f(dve, pb=pb, tc=tc, dg=dg):
                    return dve.tensor_tensor(out=acc[:, tc, dg * 512:(dg + 1) * 512], in0=ps[pb][:, :],
                                             in1=acc[:, tc, dg * 512:(dg + 1) * 512], op=ALU.add)
                S.op("dve", f, reads=(pk, "acc%d" % tc), writes=("acc%d" % tc,))
        if stop_after == "moe":
            return dump([(acc[:, 0, :], DM), (acc[:, 7, :], DM)], ("acc0", "acc7"))
        S.barrier()
        S.flush()

    with ExitStack() as s5:
        c_lw = sb("c_lw2", [128, DM], F32, s5)
        c_lb = sb("c_lb2", [128, DM], F32, s5)
        c_pw = arB[:, 0:2048]
        c_pp = arB[:, 2048:4096].bitcast(BF16).rearrange("p (c d) -> p c d", c=2)
        c_pT = sb("c_pT", [128, 2, T], BF16, s5)
        cload(c_lw, rep_ln2w, "c_lw2")
        cload(c_lb, rep_ln2b, "c_lb2")
        cload(c_pw, rep_plew, "c_pw")
        cload(c_pp, w_pp.rearrange("(c p) d -> p c d", p=128), "c_pp", eng="pool")
        cload(c_pT, pT.rearrange("(c p) t -> p c t", p=128), "c_pT", eng="pool")
        cfinal()
        lst = sb("lst2", [128, 4, 6], F32, s5)
        lmv = sb("lmv2", [128, 2], F32, s5)
        lrs = sb("lrs2", [128, 1], F32, s5)
        x2b = sb("x2b", [128, DM], BF16, s5)
        x2T = sb("x2T", [128, NDC, 128], BF16, s5)
        ebuf = arB[:, 4096:6144]
        esq = sb("esq", [128, 512], F32, s5)
        ess = sb("ess", [128, 4], F32, s5)
        ers = sb("ers", [128, 1], F32, s5)
        gbuf = arB[:, 6144:8192]
        pslots = get_pieces(len(srcs) - 4, 4)

        if stop_after == "s5pre":
            return dump([(acc[:, 0, :], DM), (c_pw[:, :], DM)], ("acc0", "c_pw", "c_pp", "c_pT", "c_lw2", "c_lb2"))
        for tc in range(NTC):
            ak = "acc%d" % tc
            x2 = acc[:, tc, :]
            layer_norm(x2, ak, c_lw, c_lb, ("c_lw2", "c_lb2"), x2, ak, lst, lmv, lrs, "2")

            def f(act, x2=x2):
                return act.copy(out=x2b[:, :], in_=x2)
            S.op("act", f, reads=(ak,), writes=("x2b",))
            if stop_after == "ln2":
                continue
            for q4 in range(4):
                pb = q4 % 2
                pk = "ps%d" % pb

                def f(pe, q4=q4, pb=pb):
                    ins = None
                    for i in range(4):
                        dc = q4 * 4 + i
                        ins = pe.matmul(ps[pb][:, i * 128:(i + 1) * 128], x2b[:, dc * 128:(dc + 1) * 128], c_qdiag[:, 0, :], start=True, stop=True)
                    return ins
                S.op("pe", f, reads=("x2b", "c_qdiag"), writes=(pk,))

                def f(act, q4=q4, pb=pb):
                    return act.copy(out=x2T[:, q4 * 4:(q4 + 1) * 4, :], in_=ps[pb][:, :].rearrange("p (a t) -> p a t", a=4))
                S.op("act", f, reads=(pk,), writes=("x2T",))
            for dg in range(4):
                pb = 2 + dg % 2
                pk = "ps%d" % pb
                mm(ps[pb][:, :], [(c_pT[:, pc, tc * 128:(tc + 1) * 128], c_pp[:, pc, dg * 512:(dg + 1) * 512]) for pc in range(2)],
                   reads=("c_pT", "c_pp"), writes=(pk,))

                def f(act, pb=pb, dg=dg):
                    return act.copy(out=ebuf[:, dg * 512:(dg + 1) * 512], in_=ps[pb][:, :])
                S.op("act", f, reads=(pk,), writes=("ebuf",))

                def f(dve, dg=dg):
                    return dve.tensor_tensor(out=esq[:, :], in0=ebuf[:, dg * 512:(dg + 1) * 512], in1=ebuf[:, dg * 512:(dg + 1) * 512], op=ALU.mult)
                S.op("dve", f, reads=("ebuf",), writes=("esq",))

                def f(dve, dg=dg):
                    return dve.tensor_reduce(out=ess[:, dg:dg + 1], in_=esq[:, :], axis=mybir.AxisListType.X, op=ALU.add)
                S.op("dve", f, reads=("esq",), writes=("ess",))

            def f(dve):
                return dve.tensor_reduce(out=ers[:, :], in_=ess[:, :], axis=mybir.AxisListType.X, op=ALU.add)
            S.op("dve", f, reads=("ess",), writes=("ers",))

            def f(act):
                return act.activation(out=ers[:, :], in_=ers[:, :], func=AF.Sqrt, bias=c_eps[:, 0:1], scale=1.0 / DM)
            S.op("act", f, reads=("ers", "c_eps"), writes=("ers",))

            def f(dve):
                return dve.reciprocal(out=ers[:, :], in_=ers[:, :])
            S.op("dve", f, reads=("ers",), writes=("ers",))

            def f(dve):
                return dve.scalar_tensor_tensor(out=ebuf[:, :], in0=ebuf[:, :], scalar=ers[:, 0:1], in1=c_pw[:, :], op0=ALU.mult, op1=ALU.mult)
            S.op("dve", f, reads=("ebuf", "ers", "c_pw"), writes=("ebuf",))
            for dg in range(4):
                pb = 4 + dg % 2
                pk = "ps%d" % pb
                sl = pslots[dg]
                mm(ps[pb][:, :], [(x2T[:, dc, :], wslot[sl][:, dc, :]) for dc in range(NDC)],
                   reads=("x2T", "wslot%d" % sl), writes=(pk,))

                def f(act, pb=pb, dg=dg):
                    return act.activation(out=gbuf[:, dg * 512:(dg + 1) * 512], in_=ps[pb][:, :], func=AF.Sigmoid)
                S.op("act", f, reads=(pk,), writes=("gbuf",))

            def f(pool):
                return pool.tensor_tensor(out=gbuf[:, :], in0=gbuf[:, :], in1=ebuf[:, :], op=ALU.mult)
            S.op("pool", f, reads=("gbuf", "ebuf"), writes=("gbuf",))

            def f(pool, x2=x2):
                return pool.tensor_tensor(out=x2, in0=x2, in1=gbuf[:, :], op=ALU.add)
            S.op("pool", f, reads=("gbuf", ak), writes=(ak,))

            if stop_after == "ple":
                continue

            def f(eng, sem, tc=tc, x2=x2):
                eng.dma_start(out=out[tc * 128:(tc + 1) * 128, :], in_=x2).then_inc(sem, 16)
            S.dma("pool", f, 1, "outst", reads=(ak,))
        if stop_after in ("ln2", "ple"):
            return dump([(acc[:, 0, :], DM), (acc[:, 7, :], DM)], ("acc0", "acc7"))
        S.barrier()
        S.flush()


def _consts():
    h = np.arange(8, dtype=np.float64)
    log_gamma = np.log1p(-np.exp2(-5.0 - h))
    idx = np.arange(128, dtype=np.float64)
    diff = idx[None, :] - idx[:, None]
    decT = np.where(diff[:, None, :] >= 0, np.exp(np.maximum(diff, 0.0)[:, None, :] * log_gamma[None, :, None]), 0.0)
    kdec = np.exp((127.0 - idx)[:, None] * log_gamma[None, :])
    qdec = np.exp((idx[:, None] + 1.0) * log_gamma[None, :])
    chunk_dec = np.exp(128.0 * log_gamma)
    qdiag = np.zeros((128, 9, 128), np.float32)
    qdiag[:, 0, :] = np.eye(128)
    for hh in range(8):
        qdiag[:, 1 + hh, :] = np.diag(qdec[:, hh])
    tri = np.zeros((128, 2, 128), np.float32)
    tri[:, 0, :] = 1.0
    tri[:, 1, :] = (idx[:, None] < idx[None, :]).astype(np.float32)
    return dict(
        decT=decT.astype(np.float32), kdec=kdec.astype(np.float32),
        qdiag=qdiag.astype(ml_dtypes.bfloat16), ident_f=np.eye(128, dtype=np.float32),
        tri=tri.astype(ml_dtypes.bfloat16),
        iota_c=np.broadcast_to(np.arange(CAP, dtype=np.float32)[None, :], (128, CAP)).copy(),
        iota_p=np.stack([np.arange(128, dtype=np.float32), np.arange(128, dtype=np.float32) + 128], axis=1),
        pidx=np.broadcast_to(np.arange(NE, dtype=np.float32)[:, None], (NE, 128)).copy(),
    ), chunk_dec


def _rope_tables(pos0):
    inv = 10000.0 ** (-np.arange(0, 128, 2, dtype=np.float32) / 128)
    pos = pos0 + np.arange(T, dtype=np.float32)
    ang = pos[:, None] * inv[None, :]
    cos = np.cos(ang).astype(np.float32)
    sin = np.sin(ang).astype(np.float32)
    return cos, sin


def _prep_inputs(inp, n_exp_decl=None):
    f = lambda a: np.ascontiguousarray(np.asarray(a, dtype=np.float32))
    x = f(inp["x"])
    p = f(inp["p"])[0]
    cst, _ = _consts()
    rep = lambda v, n=128: np.ascontiguousarray(np.broadcast_to(f(v).reshape(1, -1), (n, f(v).size)))
    lruvec = np.zeros((128, 8, 8), np.float32)
    cw = f(inp["conv_w"])[0]
    for j in range(4):
        lruvec[:, :, j] = cw[j].reshape(8, 128).T
    lruvec[:, :, 4] = f(inp["conv_b"])[0].reshape(8, 128).T
    lruvec[:, :, 5] = f(inp["lru_ba"])[0].T
    lruvec[:, :, 6] = f(inp["lru_bx"])[0].T
    lruvec[:, :, 7] = f(inp["lru_lam"])[0].reshape(8, 128).T
    bgu = np.zeros((128, 2, NE, 16), np.float32)
    bgu[:, 0] = f(inp["b_gate"])[0].reshape(NE, 16, 128).transpose(2, 0, 1)
    bgu[:, 1] = f(inp["b_up"])[0].reshape(NE, 16, 128).transpose(2, 0, 1)
    shared = dict(
        w_in=f(inp["w_in"])[0], w_out=f(inp["w_out"])[0], w_ple_gate=f(inp["w_ple_gate"])[0], w_ple_proj=f(inp["w_ple_proj"])[0],
        w_router=f(inp["w_router"])[0], lru_wa=f(inp["lru_wa"])[0], lru_wx=f(inp["lru_wx"])[0],
        lruvec=lruvec, bgu=bgu, b_down=f(inp["b_down"])[0],
        rep_ln1w=rep(inp["ln1_w"]), rep_ln1b=rep(inp["ln1_b"]), rep_ln2w=rep(inp["ln2_w"]), rep_ln2b=rep(inp["ln2_b"]),
        rep_plew=rep(inp["ple_norm_w"]), rep_gnw=rep(inp["ret_gn_w"]), rep_br=rep(inp["b_router"]),
        **cst,
    )
    for nm in ("w_gate", "w_up", "w_down"):
        ne = NE if n_exp_decl is None else n_exp_decl
        w = f(inp[nm])[0][:ne]
        w = np.ascontiguousarray(w.reshape(ne, NDC, 128, 4, 512).transpose(0, 3, 2, 1, 4)).reshape(ne, 4, 128, 8192)
        for g in range((ne + EG - 1) // EG):
            shared["%s_%d" % (nm, g)] = w[g * EG:min((g + 1) * EG, ne)]
    scale_k = 128.0 ** -0.5
    in_maps = []
    for c in range(NCORES):
        b, hf = c // 2, c % 2
        own = slice(hf * T, (hf + 1) * T)
        m = dict(shared)
        m["xT_own"] = np.ascontiguousarray(x[b, own, :].T)
        m["xT_pre"] = np.ascontiguousarray(x[b, 0:T, :].T) if hf == 1 else np.zeros((DM, T), np.float32)
        m["x_own"] = np.ascontiguousarray(x[b, own, :])
        m["pT"] = np.ascontiguousarray(p[b, own, :].T)
        m["pf"] = np.full((128, 1), float(hf), np.float32)
        cos_o, sin_o = _rope_tables(float(hf * T))
        cos_p, sin_p = _rope_tables(0.0)
        lay = lambda a: a.reshape(NTC, 128, 64).transpose(1, 0, 2)
        m["rope_own"] = np.ascontiguousarray(np.stack([lay(cos_o), lay(sin_o), lay(cos_o * scale_k), lay(sin_o * scale_k)], axis=1))
        m["rope_pre"] = np.ascontiguousarray(np.stack([lay(cos_p * scale_k), lay(sin_p * scale_k)], axis=1))
        in_maps.append(m)
    return in_maps


_NC_CACHE = {}


def kernel(**inputs):
    in_maps = _prep_inputs(inputs)
    if "nc" not in _NC_CACHE:
        _NC_CACHE["nc"] = build()
    nc = _NC_CACHE["nc"]
    res = run_bass_kernel_spmd(nc, in_maps, core_ids=list(range(NCORES)))
    outp = np.zeros((4, 2048, DM), np.float32)
    for c in range(NCORES):
        b, hf = c // 2, c % 2
        outp[b, hf * T:(hf + 1) * T, :] = res.results[c]["out"]
    return outp
```

```python
import math
from contextlib import ExitStack

import numpy as np
import ml_dtypes

import concourse.bass as bass
import concourse.mybir as mybir
from concourse.bass_utils import run_bass_kernel_spmd

F32 = mybir.dt.float32
BF16 = mybir.dt.bfloat16
ALU = mybir.AluOpType
AF = mybir.ActivationFunctionType

NCORES = 8
DM = 2048
T = 1024
NTC = 8
NDC = 16
NE = 32
CAP = 192
JB = ((0, 128), (128, CAP - 128))
LN_EPS = 1e-5
DN_ALPHA = 2.0 ** 0.25
NSLOT = 4
EG = 8


class Sched:
    ENG = ("pe", "act", "dve", "pool", "sp")

    def __init__(self, nc, es):
        self.nc = nc
        self.es = es
        self.sem = {e: es.enter_context(nc.semaphore("s_" + e)) for e in self.ENG}
        self.cnt = {e: 0 for e in self.ENG}
        self.waited = {e: {} for e in self.ENG}
        self.ops = {e: [] for e in self.ENG}
        self.last_w = {}
        self.readers = {}
        self.dsem = {}
        self.dcnt = {}

    def _deps(self, eng, reads, writes):
        deps = {}

        def add(d):
            if d is None:
                return
            s, v = d
            if deps.get(id(s), (None, 0))[1] < v:
                deps[id(s)] = (s, v)

        for k in reads:
            add(self.last_w.get(k))
        for k in writes:
            add(self.last_w.get(k))
            for d in self.readers.get(k, {}).values():
                add(d)
        waits = []
        for sid, (s, v) in deps.items():
            if self.waited[eng].get(sid, 0) < v:
                self.waited[eng][sid] = v
                waits.append((s, v))
        return waits

    def _mark(self, me, reads, writes):
        for k in reads:
            self.readers.setdefault(k, {})[id(me[0])] = me
        for k in writes:
            self.last_w[k] = me
            self.readers[k] = {}

    def op(self, eng, fn, reads=(), writes=()):
        waits = self._deps(eng, reads, writes)
        self.cnt[eng] += 1
        me = (self.sem[eng], self.cnt[eng])
        self.ops[eng].append((waits, fn, self.sem[eng], 1))
        self._mark(me, reads, writes)

    def dma(self, eng, fn, ndma, semname, reads=(), writes=()):
        if semname not in self.dsem:
            self.dsem[semname] = self.es.enter_context(self.nc.semaphore("d_" + semname))
            self.dcnt[semname] = 0
        waits = self._deps(eng, reads, writes)
        self.dcnt[semname] += 16 * ndma
        me = (self.dsem[semname], self.dcnt[semname])
        self.ops[eng].append((waits, fn, self.dsem[semname], None))
        self._mark(me, reads, writes)

    def barrier(self):
        allsem = [(self.sem[e], self.cnt[e]) for e in self.ENG if self.cnt[e] > 0]
        allsem += [(self.dsem[k], self.dcnt[k]) for k in self.dsem if self.dcnt[k] > 0]
        for e in self.ENG:
            waits = []
            for s, v in allsem:
                if self.waited[e].get(id(s), 0) < v:
                    self.waited[e][id(s)] = v
                    waits.append((s, v))
            if waits:
                self.ops[e].append((waits, None, None, 0))

    def flush(self):
        with self.nc.Block() as block:
            decos = {"pe": block.tensor, "act": block.scalar, "dve": block.vector,
                     "pool": block.gpsimd, "sp": block.sync}
            for e in self.ENG:
                ops = self.ops[e]
                if not ops:
                    continue

                def body(engine, ops=ops):
                    for waits, fn, sem, inc in ops:
                        for s, v in waits:
                            engine.wait_ge(s, v)
                        if fn is None:
                            continue
                        if inc is None:
                            fn(engine, sem)
                        else:
                            fn(engine).then_inc(sem, inc)

                decos[e](body)
                self.ops[e] = []


class _Stop(Exception):
    pass


def build(stop_after=None, n_exp_run=None, n_exp_decl=None):
    nc = bass.Bass("TRN2", target_bir_lowering=False)
    es = ExitStack()
    try:
        _body(nc, es, stop_after, n_exp_run, n_exp_decl)
    except _Stop:
        pass
    return nc


def _body(nc, es, stop_after, n_exp_run, n_exp_decl=None):
    S = Sched(nc, es)

    def din(name, shape, dt=F32):
        return nc.dram_tensor(name, list(shape), dt, kind="ExternalInput").ap()

    xT_own = din("xT_own", [DM, T])
    xT_pre = din("xT_pre", [DM, T])
    x_own = din("x_own", [T, DM])
    pT = din("pT", [256, T])
    w_in = din("w_in", [DM, 6144])
    w_out = din("w_out", [DM, DM])
    n_exp = NE if stop_after in (None, "moe", "ln2", "s5pre", "ple") else 0
    if n_exp_run is not None:
        n_exp = n_exp_run
    NEW = max(n_exp, 1) if n_exp_decl is None else n_exp_decl
    NG = (NEW + EG - 1) // EG
    w_gate = [din("w_gate_%d" % g, [min(EG, NEW - g * EG), 4, 128, 8192]) for g in range(NG)]
    w_up = [din("w_up_%d" % g, [min(EG, NEW - g * EG), 4, 128, 8192]) for g in range(NG)]
    w_down = [din("w_down_%d" % g, [min(EG, NEW - g * EG), 4, 128, 8192]) for g in range(NG)]
    w_pg = din("w_ple_gate", [DM, DM])
    w_pp = din("w_ple_proj", [256, DM])
    w_r = din("w_router", [DM, NE])
    lru_wa = din("lru_wa", [8, 128, 128])
    lru_wx = din("lru_wx", [8, 128, 128])
    lruvec = din("lruvec", [128, 8, 8])
    bgu = din("bgu", [128, 2, NE, 16])
    b_down = din("b_down", [NE, DM])
    rep_ln1w = din("rep_ln1w", [128, DM])
    rep_ln1b = din("rep_ln1b", [128, DM])
    rep_ln2w = din("rep_ln2w", [128, DM])
    rep_ln2b = din("rep_ln2b", [128, DM])
    rep_plew = din("rep_plew", [128, DM])
    rep_gnw = din("rep_gnw", [128, 1024])
    rep_br = din("rep_br", [128, NE])
    pf_in = din("pf", [128, 1])
    rope_own = din("rope_own", [128, 4, NTC, 64])
    rope_pre = din("rope_pre", [128, 2, NTC, 64])
    decT = din("decT", [128, 8, 128])
    kdec = din("kdec", [128, 8])
    qdiag = din("qdiag", [128, 9, 128], BF16)
    ident_f = din("ident_f", [128, 128])
    tri = din("tri", [128, 2, 128], BF16)
    iota_c = din("iota_c", [128, CAP])
    iota_p = din("iota_p", [128, 2])
    pidx_in = din("pidx", [NE, 128])

    out = nc.dram_tensor("out", [T, DM], F32, kind="ExternalOutput").ap()
    dbg = None
    if stop_after is not None:
        dbg = nc.dram_tensor("dbg", [128, 8192], F32, kind="ExternalOutput").ap()

    def sb(name, shape, dt=F32, stack=None):
        return (stack or es).enter_context(nc.sbuf_tensor(name, list(shape), dt))

    ps = [es.enter_context(nc.psum_tensor("ps%d" % i, [128, 512], F32)) for i in range(8)]
    wslot = [sb("wslot%d" % i, [128, NDC, 512], BF16) for i in range(NSLOT)]
    c_identf = sb("c_identf", [128, 128])
    c_qdiag = sb("c_qdiag", [128, 9, 128], BF16)

    piece_no = [0]

    def load_piece(src_ap):
        i = piece_no[0] % NSLOT
        piece_no[0] += 1
        key = "wslot%d" % i
        dst = wslot[i]

        def fn(eng, sem, src_ap=src_ap, dst=dst):
            if tuple(src_ap.shape) == (128, 8192):
                v = src_ap.rearrange("p (c f) -> p c f", c=NDC)
            else:
                v = src_ap.rearrange("(c p) f -> p c f", p=128)
            for hh in range(2):
                eng.dma_start(out=dst[:, hh * 8:(hh + 1) * 8, :], in_=v[:, hh * 8:(hh + 1) * 8, :]).then_inc(sem, 16)

        S.dma("pool", fn, 2, key, reads=(), writes=(key,))
        return i

    consts = []

    def cload(dst_t, src_ap, key, eng="sp"):
        def fn(engine, sem, dst_t=dst_t, src_ap=src_ap):
            engine.dma_start(out=dst_t[:], in_=src_ap).then_inc(sem, 16)
        S.dma(eng, fn, 1, "c_" + eng, writes=(key,))
        consts.append(key)

    def cfinal():
        for eng in ("sp", "pool"):
            nm = "c_" + eng
            if nm in S.dsem:
                for k in consts:
                    if S.last_w[k][0] is S.dsem[nm]:
                        S.last_w[k] = (S.dsem[nm], S.dcnt[nm])
        consts.clear()

    cload(c_identf, ident_f, "c_identf")
    cload(c_qdiag, qdiag, "c_qdiag")
    c_eps = sb("c_eps", [128, 1])

    def f_eps(pool):
        return pool.memset(c_eps[:, :], LN_EPS)
    S.op("pool", f_eps, writes=("c_eps",))
    c_dummy = sb("c_dummy", [128, 8], BF16)
    cload(c_dummy, kdec, "c_dummy", eng="pool")
    cfinal()
    S.flush()

    def mm(out_ap, pairs, reads, writes):
        def fn(pe, out_ap=out_ap, pairs=pairs):
            n = len(pairs)
            ins = None
            for i, (l, r) in enumerate(pairs):
                ins = pe.matmul(out_ap, l, r, start=(i == 0), stop=(i == n - 1))
            return ins
        S.op("pe", fn, reads=reads, writes=writes)

    def dump(ap_list, keys):
        off = 0
        for ap, n in ap_list:
            def fn(engine, sem, ap=ap, off=off, n=n):
                engine.dma_start(out=dbg[:, off:off + n], in_=ap).then_inc(sem, 16)
            S.dma("sp", fn, 1, "dbg", reads=keys)
            off += n
        S.barrier()
        S.flush()
        raise _Stop()

    arA = sb("arA", [128, 16384])
    arB = sb("arB", [128, 8192])
    xTo = arA[:, 0:8192].bitcast(BF16).rearrange("p (c t) -> p c t", c=NDC)
    xTp = arA[:, 8192:16384].bitcast(BF16).rearrange("p (c t) -> p c t", c=NDC)
    mixL = arB[:, 0:4096].bitcast(BF16).rearrange("p (c t) -> p c t", c=8)
    mixR = arB[:, 4096:8192].bitcast(BF16).rearrange("p (c t) -> p c t", c=8)
    acc = arA[:, :].rearrange("p (c d) -> p c d", c=NTC)
    x1bf = arB[:, :].bitcast(BF16).rearrange("p (c d) -> p c d", c=NTC)
    s12 = ExitStack()
    c_lruvec = sb("c_lruvec", [128, 8, 8], F32, s12)
    c_pf = sb("c_pf", [128, 1], F32, s12)
    state_f = sb("state_f", [128, 8, 128], F32, s12)
    state_b = sb("state_b", [128, 8, 128], BF16, s12)
    cload(c_lruvec, lruvec, "c_lruvec")
    cload(c_pf, pf_in, "c_pf")

    def load_xT(dst, src, key):
        def fn(eng, sem):
            v = src.rearrange("(c p) t -> p c t", p=128)
            for q4 in range(4):
                eng.dma_start(out=dst[:, q4 * 4:(q4 + 1) * 4, :], in_=v[:, q4 * 4:(q4 + 1) * 4, :]).then_inc(sem, 16)
        S.dma("pool", fn, 4, key, writes=(key,))

    with ExitStack() as s1:
        load_xT(xTp, xT_pre, "xTp")
        load_xT(xTo, xT_own, "xTo")
        c_wa = sb("c_wa", [128, 8, 128], BF16, s1)
        c_wx = sb("c_wx", [128, 8, 128], BF16, s1)
        cload(c_wa, lru_wa.rearrange("h i j -> i h j"), "c_wa", eng="pool")
        cload(c_wx, lru_wx.rearrange("h i j -> i h j"), "c_wx", eng="pool")
        cfinal()
        c8 = sb("c8", [128, 8, 2], F32, s1)

        def f_sig(act):
            return act.activation(out=c8[:, :, 0], in_=c_lruvec[:, :, 7], func=AF.Sigmoid)
        S.op("act", f_sig, reads=("c_lruvec",), writes=("c8a",))

        def f_ln(act):
            return act.activation(out=c8[:, :, 1], in_=c8[:, :, 0], func=AF.Ln)
        S.op("act", f_ln, reads=("c8a",), writes=("c8b",))

        def f_c8(dve):
            return dve.tensor_scalar(out=c8[:, :, 0], in0=c8[:, :, 1], scalar1=8.0, scalar2=None, op0=ALU.mult)
        S.op("dve", f_c8, reads=("c8b",), writes=("c8a",))

        def f_c16(dve):
            return dve.tensor_scalar(out=c8[:, :, 1], in0=c8[:, :, 0], scalar1=2.0, scalar2=None, op0=ALU.mult)
        S.op("dve", f_c16, reads=("c8a",), writes=("c8",))

        with ExitStack() as s1a:
            X = sb("lX", [128, 3 + 2 * T], F32, s1a)
            C = sb("lC", [128, 2 * T], F32, s1a)
            R = arB[:, 4096:6144]
            CB = arB[:, 6144:7168].bitcast(BF16)
            G = sb("lG", [128, 2 * T], F32, s1a)
            hb0 = sb("lh0", [128, 1], F32, s1a)

            def f_zero(pool):
                return pool.memset(X[:, 0:3], 0.0)
            S.op("pool", f_zero, writes=("X",))

            for hb in range(8):
                cg = 8 + hb // 4
                cc = hb % 4
                if cc == 0:
                    sl_x = load_piece(w_in[:, cg * 512:(cg + 1) * 512])
                    sl_y = load_piece(w_in[:, (cg + 2) * 512:(cg + 3) * 512])
                kx, ky = "wslot%d" % sl_x, "wslot%d" % sl_y
                for tt in range(4):
                    src, skey = (xTp, "xTp") if tt < 2 else (xTo, "xTo")
                    t0 = (tt % 2) * 512
                    pk = "ps%d" % (tt % 2)
                    mm(ps[tt % 2][:, :],
                       [(wslot[sl_x][:, dc, cc * 128:(cc + 1) * 128], src[:, dc, t0:t0 + 512]) for dc in range(NDC)],
                       reads=(kx, skey), writes=(pk,))

                    def f_ev(act, tt=tt):
                        return act.copy(out=X[:, 3 + tt * 512:3 + (tt + 1) * 512], in_=ps[tt % 2][:, :])
                    S.op("act", f_ev, reads=(pk,), writes=("X",))
                lv = c_lruvec

                def f_c0(dve, hb=hb):
                    return dve.tensor_scalar(out=C[:, :], in0=X[:, 0:2 * T], scalar1=lv[:, hb, 0:1], scalar2=lv[:, hb, 4:5],
                                             op0=ALU.mult, op1=ALU.add)
                S.op("dve", f_c0, reads=("X", "c_lruvec"), writes=("C",))
                for j in range(1, 4):
                    def f_cj(dve, hb=hb, j=j):
                        return dve.scalar_tensor_tensor(out=C[:, :], in0=X[:, j:j + 2 * T], scalar=lv[:, hb, j:j + 1], in1=C[:, :],
                                                        op0=ALU.mult, op1=ALU.add)
                    S.op("dve", f_cj, reads=("X", "C"), writes=("C",))

                def f_cb(act):
                    return act.copy(out=CB[:, :], in_=C[:, :])
                S.op("act", f_cb, reads=("C",), writes=("CB",))
                for gi_, (wt, wk, dstb, dk, bcol) in enumerate(((c_wa, "c_wa", R, "R", 5), (c_wx, "c_wx", G, "G", 6))):
                    for tt in range(4):
                        pk = "ps%d" % (2 + (gi_ * 4 + tt) % 4)
                        pst = ps[2 + (gi_ * 4 + tt) % 4]
                        mm(pst[:, :], [(wt[:, hb, :], CB[:, tt * 512:(tt + 1) * 512])], reads=(wk, "CB"), writes=(pk,))

                        def f_sg(act, pst=pst, dstb=dstb, tt=tt, hb=hb, bcol=bcol):
                            return act.activation(out=dstb[:, tt * 512:(tt + 1) * 512], in_=pst[:, :], func=AF.Sigmoid,
                                                  bias=lv[:, hb, bcol:bcol + 1], scale=1.0)
                        S.op("act", f_sg, reads=(pk, "c_lruvec"), writes=(dk,))

                def f_a(act, hb=hb):
                    return act.activation(out=X[:, 3:3 + 2 * T], in_=R[:, :], func=AF.Exp, scale=c8[:, hb, 0:1])
                S.op("act", f_a, reads=("R", "c8"), writes=("X",))

                def f_a2(act, hb=hb):
                    return act.activation(out=R[:, :], in_=R[:, :], func=AF.Exp, scale=c8[:, hb, 1:2])
                S.op("act", f_a2, reads=("R", "c8"), writes=("R",))

                def f_om(dve):
                    return dve.tensor_scalar(out=R[:, :], in0=R[:, :], scalar1=1.0, scalar2=-1.0, op0=ALU.min, op1=ALU.mult)
                S.op("dve", f_om, reads=("R",), writes=("R",))

                def f_sq(act):
                    return act.activation(out=R[:, :], in_=R[:, :], func=AF.Sqrt, bias=1.0, scale=1.0)
                S.op("act", f_sq, reads=("R",), writes=("R",))

                def f_g2(pool):
                    return pool.tensor_tensor(out=G[:, :], in0=G[:, :], in1=C[:, :], op=ALU.mult)
                S.op("pool", f_g2, reads=("G", "C"), writes=("G",))

                def f_bx(dve):
                    return dve.tensor_tensor(out=G[:, :], in0=G[:, :], in1=R[:, :], op=ALU.mult)
                S.op("dve", f_bx, reads=("G", "R"), writes=("G",))

                def f_s1(dve):
                    return dve.tensor_tensor_scan(out=C[:, 0:T], data0=X[:, 3:3 + T], data1=G[:, 0:T], initial=0.0,
                                                  op0=ALU.mult, op1=ALU.add)
                S.op("dve", f_s1, reads=("X", "G", "C"), writes=("C",))

                def f_h0(dve):
                    return dve.tensor_scalar(out=hb0[:, :], in0=C[:, T - 1:T], scalar1=c_pf[:, 0:1], scalar2=None, op0=ALU.mult)
                S.op("dve", f_h0, reads=("C", "c_pf"), writes=("hb0",))

                def f_s2(dve):
                    return dve.tensor_tensor_scan(out=C[:, T:2 * T], data0=X[:, 3 + T:3 + 2 * T], data1=G[:, T:2 * T],
                                                  initial=hb0[:, 0:1], op0=ALU.mult, op1=ALU.add)
                S.op("dve", f_s2, reads=("X", "G", "C", "hb0"), writes=("C",))

                for tt in range(2):
                    pk = "ps%d" % (6 + tt)
                    mm(ps[6 + tt][:, :],
                       [(wslot[sl_y][:, dc, cc * 128:(cc + 1) * 128], xTo[:, dc, tt * 512:(tt + 1) * 512]) for dc in range(NDC)],
                       reads=(ky, "xTo"), writes=(pk,))
                    ysl = slice(tt * 512, (tt + 1) * 512)

                    def f_y(act, tt=tt, ysl=ysl):
                        return act.copy(out=R[:, ysl], in_=ps[6 + tt][:, :])
                    S.op("act", f_y, reads=(pk,), writes=("R",))

                    def f_y2(act, tt=tt, ysl=ysl):
                        return act.activation(out=G[:, ysl], in_=ps[6 + tt][:, :], func=AF.Square)
                    S.op("act", f_y2, reads=(pk,), writes=("G",))
                ysl = slice(0, T)

                def f_y3(dve):
                    return dve.scalar_tensor_tensor(out=G[:, ysl], in0=G[:, ysl], scalar=0.044715, in1=R[:, ysl],
                                                    op0=ALU.mult, op1=ALU.mult)
                S.op("dve", f_y3, reads=("G", "R"), writes=("G",))

                def f_y4(pool):
                    return pool.tensor_tensor(out=G[:, ysl], in0=G[:, ysl], in1=R[:, ysl], op=ALU.add)
                S.op("pool", f_y4, reads=("G", "R"), writes=("G",))

                def f_y5(act):
                    return act.activation(out=G[:, ysl], in_=G[:, ysl], func=AF.Sigmoid, scale=1.5957691216057308)
                S.op("act", f_y5, reads=("G",), writes=("G",))

                def f_y6(pool):
                    return pool.tensor_tensor(out=G[:, ysl], in0=G[:, ysl], in1=R[:, ysl], op=ALU.mult)
                S.op("pool", f_y6, reads=("G", "R"), writes=("G",))

                def f_y7(dve, hb=hb):
                    return dve.tensor_tensor(out=mixL[:, hb, :], in0=G[:, ysl], in1=C[:, T:2 * T], op=ALU.mult)
                S.op("dve", f_y7, reads=("G", "C"), writes=("mixL",))

            if stop_after == "lru":
                def f_d(dve):
                    return dve.tensor_copy(out=R[:, 0:T], in_=mixL[:, 7, :])
                S.op("dve", f_d, reads=("mixL",), writes=("R",))

                def f_d2(dve):
                    return dve.tensor_copy(out=R[:, T:2 * T], in_=mixL[:, 0, :])
                S.op("dve", f_d2, reads=("mixL",), writes=("R",))
                return dump([(R[:, 0:2 * T], 2 * T), (C[:, :], 2 * T)], ("R", "C"))
            S.barrier()
            S.flush()

        _, chunk_dec = _consts()
        with ExitStack() as s1b:
            c_ropep = sb("c_ropep", [128, 2, NTC, 64], F32, s1b)
            c_kdec = sb("c_kdec", [128, 8], F32, s1b)
            cload(c_ropep, rope_pre, "c_ropep")
            cload(c_kdec, kdec, "c_kdec")
            cfinal()
            k_p = arB[:, 4096:8192].bitcast(BF16).rearrange("p (c t) -> p c t", c=NTC)
            vd_p = sb("vd_p", [128, NTC, 1024], BF16, s1b)
            rt = [sb("rt%d" % i, [128, 4, 64], F32, s1b) for i in range(4)]

            def f_z(pool):
                return pool.memset(state_f[:, :, :], 0.0)
            S.op("pool", f_z, writes=tuple("st%d" % h for h in range(8)))

            def f_zb(pool):
                return pool.memset(state_b[:, :, :], 0.0)
            S.op("pool", f_zb, writes=tuple("sb%d" % h for h in range(8)))

            def rope_evac(pst, pk, cos_ap, sin_ap, dst_ap, dkey):
                pv = pst[:, :].rearrange("p (h two d) -> p h two d", h=4, two=2)
                dv = dst_ap.rearrange("p (h two d) -> p h two d", h=4, two=2)
                cb = cos_ap.broadcast_to([128, 4, 64])
                sn = sin_ap.broadcast_to([128, 4, 64])
                x1, x2 = pv[:, :, 0, :], pv[:, :, 1, :]
                for (ta, a, ca) in ((0, x1, cb), (1, x2, sn), (2, x1, sn), (3, x2, cb)):
                    def f(dve, ta=ta, a=a, ca=ca):
                        return dve.tensor_tensor(out=rt[ta][:, :, :], in0=a, in1=ca, op=ALU.mult)
                    S.op("dve", f, reads=(pk, "c_rope"), writes=("rt%d" % ta,))

                def f1(pool):
                    return pool.tensor_tensor(out=dv[:, :, 0, :], in0=rt[0][:, :, :], in1=rt[1][:, :, :], op=ALU.subtract)
                S.op("pool", f1, reads=("rt0", "rt1"), writes=(dkey,))

                def f2(pool):
                    return pool.tensor_tensor(out=dv[:, :, 1, :], in0=rt[2][:, :, :], in1=rt[3][:, :, :], op=ALU.add)
                S.op("pool", f2, reads=("rt2", "rt3"), writes=(dkey,))

            S.last_w["c_rope"] = S.last_w["c_ropep"]
            for half in range(2):
                slk = load_piece(w_in[:, (2 + half) * 512:(3 + half) * 512])
                slv = load_piece(w_in[:, (4 + half) * 512:(5 + half) * 512])
                for tc in range(NTC):
                    pk = "ps%d" % (tc % 2)
                    mm(ps[tc % 2][:, :], [(xTp[:, dc, tc * 128:(tc + 1) * 128], wslot[slk][:, dc, :]) for dc in range(NDC)],
                       reads=("xTp", "wslot%d" % slk), writes=(pk,))
                    rope_evac(ps[tc % 2], pk, c_ropep[:, 0, tc:tc + 1, :], c_ropep[:, 1, tc:tc + 1, :],
                              k_p[:, tc, half * 512:(half + 1) * 512], "k_p")
                for tc in range(NTC):
                    pk = "ps%d" % (2 + tc % 2)
                    pst = ps[2 + tc % 2]
                    mm(pst[:, :], [(xTp[:, dc, tc * 128:(tc + 1) * 128], wslot[slv][:, dc, :]) for dc in range(NDC)],
                       reads=("xTp", "wslot%d" % slv), writes=(pk,))
                    for h4 in range(4):
                        def f(act, pst=pst, tc=tc, h4=h4, half=half):
                            h = half * 4 + h4
                            return act.activation(out=vd_p[:, tc, h * 128:(h + 1) * 128], in_=pst[:, h4 * 128:(h4 + 1) * 128],
                                                  func=AF.Copy, scale=c_kdec[:, h:h + 1])
                        S.op("act", f, reads=(pk, "c_kdec"), writes=("vd_p",))
            for n in range(NTC):
                for h in range(8):
                    pk = "ps%d" % (4 + h % 4)
                    pst = ps[4 + h % 4]
                    mm(pst[:, 0:128], [(k_p[:, n, h * 128:(h + 1) * 128], vd_p[:, n, h * 128:(h + 1) * 128])],
                       reads=("k_p", "vd_p"), writes=(pk,))

                    def f(dve, pst=pst, h=h):
                        return dve.scalar_tensor_tensor(out=state_f[:, h, :], in0=state_f[:, h, :], scalar=float(chunk_dec[h]),
                                                        in1=pst[:, 0:128], op0=ALU.mult, op1=ALU.add)
                    S.op("dve", f, reads=(pk, "st%d" % h), writes=("st%d" % h,))
            for h in range(8):
                def f(act, h=h):
                    return act.copy(out=state_b[:, h, :], in_=state_f[:, h, :])
                S.op("act", f, reads=("st%d" % h,), writes=("sb%d" % h,))
            if stop_after == "pre":
                return dump([(state_f[:, :, :].rearrange("p h e -> p (h e)"), 1024)], tuple("st%d" % h for h in range(8)))
            S.barrier()
            S.flush()

    with ExitStack() as s2:
        c_rope = sb("c_rope", [128, 4, NTC, 64], F32, s2)
        c_kdec = sb("c_kdec2", [128, 8], F32, s2)
        c_decT = sb("c_decT", [128, 8, 128], F32, s2)
        c_gnw = sb("c_gnw", [128, 1024], F32, s2)
        cload(c_rope, rope_own, "c_rope")
        cload(c_kdec, kdec, "c_kdec")
        cload(c_decT, decT, "c_decT")
        cload(c_gnw, rep_gnw, "c_gnw")
        cfinal()
        qkvv = arA[:, 8192:16384].bitcast(BF16).rearrange("p (w c t) -> p w c t", w=4, c=NTC)
        q_o, k_o, v_o, vd_o = qkvv[:, 0], qkvv[:, 1], qkvv[:, 2], qkvv[:, 3]
        sg_o = sb("sg_o", [128, NTC, 512], BF16, s2)
        rt = [sb("rt2_%d" % i, [128, 4, 64], F32, s2) for i in range(4)]
        trb = [sb("trb%d" % i, [128, 384], BF16, s2) for i in range(2)]
        sdt = [sb("sdt%d" % i, [128, 128], BF16, s2) for i in range(2)]
        gst = sb("gst", [128, 4, 6], F32, s2)
        gmv = sb("gmv", [128, 4, 2], F32, s2)
        grs = sb("grs", [128, 4], F32, s2)
        rn = sb("rn", [128, 512], F32, s2)
        mtok = sb("mtok", [128, 512], BF16, s2)

        def rope_evac2(pst, pk, cos_ap, sin_ap, dst_ap, dkey):
            pv = pst[:, :].rearrange("p (h two d) -> p h two d", h=4, two=2)
            dv = dst_ap.rearrange("p (h two d) -> p h two d", h=4, two=2)
            cb = cos_ap.broadcast_to([128, 4, 64])
            sn = sin_ap.broadcast_to([128, 4, 64])
            x1, x2 = pv[:, :, 0, :], pv[:, :, 1, :]
            for (ta, a, ca) in ((0, x1, cb), (1, x2, sn), (2, x1, sn), (3, x2, cb)):
                def f(dve, ta=ta, a=a, ca=ca):
                    return dve.tensor_tensor(out=rt[ta][:, :, :], in0=a, in1=ca, op=ALU.mult)
                S.op("dve", f, reads=(pk, "c_rope"), writes=("rt%d" % ta,))

            def f1(pool):
                return pool.tensor_tensor(out=dv[:, :, 0, :], in0=rt[0][:, :, :], in1=rt[1][:, :, :], op=ALU.subtract)
            S.op("pool", f1, reads=("rt0", "rt1"), writes=(dkey,))

            def f2(pool):
                return pool.tensor_tensor(out=dv[:, :, 1, :], in0=rt[2][:, :, :], in1=rt[3][:, :, :], op=ALU.add)
            S.op("pool", f2, reads=("rt2", "rt3"), writes=(dkey,))

        ident_b = c_qdiag[:, 0, :]
        for hg in range(2):
            slq = load_piece(w_in[:, (0 + hg) * 512:(1 + hg) * 512])
            slk = load_piece(w_in[:, (2 + hg) * 512:(3 + hg) * 512])
            slv = load_piece(w_in[:, (4 + hg) * 512:(5 + hg) * 512])
            slg = load_piece(w_in[:, (6 + hg) * 512:(7 + hg) * 512])
            cnt = 0
            for which, sl in (("q", slq), ("k", slk), ("v", slv), ("g", slg)):
                for tc in range(NTC):
                    pb = cnt % 2
                    cnt += 1
                    pk = "ps%d" % pb
                    pst = ps[pb]
                    mm(pst[:, :], [(xTo[:, dc, tc * 128:(tc + 1) * 128], wslot[sl][:, dc, :]) for dc in range(NDC)],
                       reads=("xTo", "wslot%d" % sl), writes=(pk,))
                    if which == "q":
                        rope_evac2(pst, pk, c_rope[:, 0, tc:tc + 1, :], c_rope[:, 1, tc:tc + 1, :], q_o[:, tc, :], "q_o")
                    elif which == "k":
                        rope_evac2(pst, pk, c_rope[:, 2, tc:tc + 1, :], c_rope[:, 3, tc:tc + 1, :], k_o[:, tc, :], "k_o")
                    elif which == "v":
                        def f(act, pst=pst, tc=tc):
                            return act.copy(out=v_o[:, tc, :], in_=pst[:, :])
                        S.op("act", f, reads=(pk,), writes=("v_o",))
                        for h4 in range(4):
                            def f(act, pst=pst, tc=tc, h4=h4, hg=hg):
                                h = hg * 4 + h4
                                return act.activation(out=vd_o[:, tc, h4 * 128:(h4 + 1) * 128], in_=pst[:, h4 * 128:(h4 + 1) * 128],
                                                      func=AF.Copy, scale=c_kdec[:, h:h + 1])
                            S.op("act", f, reads=(pk, "c_kdec"), writes=("vd_o",))
                    else:
                        def f(act, pst=pst, tc=tc):
                            return act.activation(out=sg_o[:, tc, :], in_=pst[:, :], func=AF.Silu)
                        S.op("act", f, reads=(pk,), writes=("sg_o",))
            for n in range(NTC):
                rb = 6 + n % 2
                rk = "ps%d" % rb
                for h4 in range(4):
                    h = hg * 4 + h4
                    hs = slice(h4 * 128, (h4 + 1) * 128)
                    tb = 2 + h4 % 2
                    tk = "ps%d" % tb
                    trs = trb[h4 % 2]
                    trk = "trb%d" % (h4 % 2)

                    def f_tr(pe, tb=tb, n=n, hs=hs, h=h):
                        pe.matmul(ps[tb][:, 0:128], q_o[:, n, hs], ident_b, start=True, stop=True)
                        pe.matmul(ps[tb][:, 128:256], q_o[:, n, hs], c_qdiag[:, 1 + h, :], start=True, stop=True)
                        return pe.matmul(ps[tb][:, 256:384], k_o[:, n, hs], ident_b, start=True, stop=True)
                    S.op("pe", f_tr, reads=("q_o", "k_o", "c_qdiag"), writes=(tk,))

                    def f_te(act, tb=tb, trs=trs):
                        return act.copy(out=trs[:, :], in_=ps[tb][:, 0:384])
                    S.op("act", f_te, reads=(tk,), writes=(trk,))
                    sb_ = 4 + h4 % 2
                    sk = "ps%d" % sb_
                    mm(ps[sb_][:, 0:128], [(trs[:, 256:384], trs[:, 0:128])], reads=(trk,), writes=(sk,))
                    sd = sdt[h4 % 2]
                    sdk = "sdt%d" % (h4 % 2)

                    def f_sd(dve, sb_=sb_, sd=sd, h=h):
                        return dve.tensor_tensor(out=sd[:, :], in0=ps[sb_][:, 0:128], in1=c_decT[:, h, :], op=ALU.mult)
                    S.op("dve", f_sd, reads=(sk, "c_decT"), writes=(sdk,))
                    mm(ps[rb][:, hs], [(sd[:, :], v_o[:, n, hs]), (trs[:, 128:256], state_b[:, h, :])],
                       reads=(sdk, "v_o", trk, "sb%d" % h), writes=(rk,))
                    mm(ps[tb][:, 384:512], [(k_o[:, n, hs], vd_o[:, n, hs])], reads=("k_o", "vd_o"), writes=(tk,))

                    def f_su(dve, tb=tb, h=h):
                        return dve.scalar_tensor_tensor(out=state_f[:, h, :], in0=state_f[:, h, :], scalar=float(chunk_dec[h]),
                                                        in1=ps[tb][:, 384:512], op0=ALU.mult, op1=ALU.add)
                    S.op("dve", f_su, reads=(tk, "st%d" % h), writes=("st%d" % h,))

                    def f_sbc(act, h=h):
                        return act.copy(out=state_b[:, h, :], in_=state_f[:, h, :])
                    S.op("act", f_sbc, reads=("st%d" % h,), writes=("sb%d" % h,))
                for h4 in range(4):
                    def f_bs(dve, h4=h4, rb=rb):
                        return dve.bn_stats(out=gst[:, h4, :], in_=ps[rb][:, h4 * 128:(h4 + 1) * 128])
                    S.op("dve", f_bs, reads=(rk,), writes=("gst",))
                for h4 in range(4):
                    def f_ba(dve, h4=h4):
                        return dve.bn_aggr(out=gmv[:, h4, :], in_=gst[:, h4, :])
                    S.op("dve", f_ba, reads=("gst",), writes=("gmv",))

                def f_sd2(act):
                    return act.activation(out=grs[:, :], in_=gmv[:, :, 1], func=AF.Sqrt, bias=c_eps[:, 0:1], scale=1.0)
                S.op("act", f_sd2, reads=("gmv", "c_eps"), writes=("grs",))

                def f_rc(dve):
                    return dve.reciprocal(out=grs[:, :], in_=grs[:, :])
                S.op("dve", f_rc, reads=("grs",), writes=("grs",))
                for h4 in range(4):
                    def f_nm(dve, h4=h4, rb=rb):
                        return dve.tensor_scalar(out=rn[:, h4 * 128:(h4 + 1) * 128], in0=ps[rb][:, h4 * 128:(h4 + 1) * 128],
                                                 scalar1=gmv[:, h4, 0:1], scalar2=grs[:, h4:h4 + 1], op0=ALU.subtract, op1=ALU.mult)
                    S.op("dve", f_nm, reads=(rk, "gmv", "grs"), writes=("rn",))

                def f_gw(pool, hg=hg):
                    return pool.tensor_tensor(out=rn[:, :], in0=rn[:, :], in1=c_gnw[:, hg * 512:(hg + 1) * 512], op=ALU.mult)
                S.op("pool", f_gw, reads=("rn", "c_gnw"), writes=("rn",))

                def f_sgm(pool, n=n):
                    return pool.tensor_tensor(out=mtok[:, :], in0=rn[:, :], in1=sg_o[:, n, :], op=ALU.mult)
                S.op("pool", f_sgm, reads=("rn", "sg_o"), writes=("mtok",))
                mb = n % 2
                mk = "ps%d" % mb

                def f_mt(pe, mb=mb):
                    ins = None
                    for h4 in range(4):
                        ins = pe.matmul(ps[mb][:, h4 * 128:(h4 + 1) * 128], mtok[:, h4 * 128:(h4 + 1) * 128], ident_b, start=True, stop=True)
                    return ins
                S.op("pe", f_mt, reads=("mtok", "c_qdiag"), writes=(mk,))

                def f_me(act, mb=mb, hg=hg, n=n):
                    return act.copy(out=mixR[:, hg * 4:hg * 4 + 4, n * 128:(n + 1) * 128],
                                    in_=ps[mb][:, :].rearrange("p (h i) -> p h i", h=4))
                S.op("act", f_me, reads=(mk,), writes=("mixR",))
        if stop_after == "ret":
            dbgbuf = sb("dbgbuf", [128, T], F32, s2)

            def f_d(dve):
                return dve.tensor_copy(out=dbgbuf[:, 0:512], in_=mixR[:, 0, 0:512])
            S.op("dve", f_d, reads=("mixR",), writes=("dbgbuf",))

            def f_d2(dve):
                return dve.tensor_copy(out=dbgbuf[:, 512:T], in_=mixR[:, 5, 512:T])
            S.op("dve", f_d2, reads=("mixR",), writes=("dbgbuf",))
            return dump([(dbgbuf[:, 0:T], T)], ("dbgbuf",))
        S.barrier()
        S.flush()

    s12.close()

    srcs = [w_out[:, dg * 512:(dg + 1) * 512] for dg in range(4)]
    for e in range(n_exp):
        for fg in range(4):
            ee = e % NEW
            srcs.append(w_gate[ee // EG][ee % EG, fg])
            srcs.append(w_up[ee // EG][ee % EG, fg])
        for dg in range(4):
            srcs.append(w_down[ee // EG][ee % EG, dg])
    for dg in range(4):
        srcs.append(w_pg[:, dg * 512:(dg + 1) * 512])
    stream = {"issued": 0, "slots": {}}

    def get_pieces(k, n=1):
        while stream["issued"] < min(k + NSLOT, len(srcs)):
            i = stream["issued"]
            stream["slots"][i] = load_piece(srcs[i])
            stream["issued"] += 1
        return [stream["slots"][k + i] for i in range(n)]

    def get_piece(k):
        return get_pieces(k, 1)[0]

    def layer_norm(src_ap, skey, w_t, b_t, wkeys, dst_ap, dkey, lst, lmv, lrs, tag):
        for k4 in range(4):
            def f(dve, k4=k4):
                return dve.bn_stats(out=lst[:, k4, :], in_=src_ap[:, k4 * 512:(k4 + 1) * 512])
            S.op("dve", f, reads=(skey,), writes=("lst" + tag,))

        def f(dve):
            return dve.bn_aggr(out=lmv[:, :], in_=lst[:, :, :].rearrange("p a b -> p (a b)"))
        S.op("dve", f, reads=("lst" + tag,), writes=("lmv" + tag,))

        def f(act):
            return act.activation(out=lrs[:, :], in_=lmv[:, 1:2], func=AF.Sqrt, bias=c_eps[:, 0:1], scale=1.0)
        S.op("act", f, reads=("lmv" + tag, "c_eps"), writes=("lrs" + tag,))

        def f(dve):
            return dve.reciprocal(out=lrs[:, :], in_=lrs[:, :])
        S.op("dve", f, reads=("lrs" + tag,), writes=("lrs" + tag,))

        def f(dve):
            return dve.tensor_scalar(out=dst_ap, in0=src_ap, scalar1=lmv[:, 0:1], scalar2=lrs[:, 0:1],
                                     op0=ALU.subtract, op1=ALU.mult)
        S.op("dve", f, reads=(skey, "lmv" + tag, "lrs" + tag), writes=(dkey,))

        def f(pool):
            return pool.tensor_tensor(out=dst_ap, in0=dst_ap, in1=w_t[:, :], op=ALU.mult)
        S.op("pool", f, reads=(dkey,) + wkeys, writes=(dkey,))

        def f(pool):
            return pool.tensor_tensor(out=dst_ap, in0=dst_ap, in1=b_t[:, :], op=ALU.add)
        S.op("pool", f, reads=(dkey,) + wkeys, writes=(dkey,))

    gates = sb("gates", [128, NTC, NE])
    posm = sb("posm", [128, NTC, NE])
    posmT = sb("posmT", [NE, T], BF16)
    c_bgu = sb("c_bgu", [128, 2, NE, 16])
    c_iotac = sb("c_iotac", [128, CAP])
    c_iotap = sb("c_iotap", [128, 2])
    c_pidx = sb("c_pidx", [NE, 128])
    cload(c_bgu, bgu, "c_bgu")
    cload(c_iotac, iota_c, "c_iotac")
    cload(c_iotap, iota_p, "c_iotap")
    cload(c_pidx, pidx_in, "c_pidx")
    cfinal()

    with ExitStack() as s3:
        xcb = [sb("xcb%d" % i, [128, 512], F32, s3) for i in range(2)]
        cnt = 0
        for dg in range(4):
            sl = get_piece(dg)
            for tc in range(NTC):
                pb = cnt % 2
                xb = xcb[cnt % 2]
                xk = "xcb%d" % (cnt % 2)
                cnt += 1

                def f(eng, sem, xb=xb, tc=tc, dg=dg):
                    eng.dma_start(out=xb[:, :], in_=x_own[tc * 128:(tc + 1) * 128, dg * 512:(dg + 1) * 512]).then_inc(sem, 16)
                S.dma("sp", f, 1, xk, writes=(xk,))
                pk = "ps%d" % pb
                mm(ps[pb][:, :],
                   [((mixR if fc < 8 else mixL)[:, fc % 8, tc * 128:(tc + 1) * 128], wslot[sl][:, fc, :]) for fc in range(NDC)],
                   reads=("mixR", "mixL", "wslot%d" % sl), writes=(pk,))

                def f(dve, xb=xb, pb=pb, tc=tc, dg=dg):
                    return dve.scalar_tensor_tensor(out=acc[:, tc, dg * 512:(dg + 1) * 512], in0=xb[:, :], scalar=DN_ALPHA,
                                                    in1=ps[pb][:, :], op0=ALU.mult, op1=ALU.add)
                S.op("dve", f, reads=(xk, pk), writes=("acc%d" % tc,))
        S.barrier()
        S.flush()

    with ExitStack() as s3b:
        c_lw = sb("c_lw", [128, DM], F32, s3b)
        c_lb = sb("c_lb", [128, DM], F32, s3b)
        c_wr = sb("c_wr", [128, NDC, NE], F32, s3b)
        c_br = sb("c_br", [128, NE], F32, s3b)
        c_tri = sb("c_tri", [128, 2, 128], BF16, s3b)
        cload(c_lw, rep_ln1w, "c_lw")
        cload(c_lb, rep_ln1b, "c_lb")
        cload(c_wr, w_r.rearrange("(c p) e -> p c e", p=128), "c_wr")
        cload(c_br, rep_br, "c_br")
        cload(c_tri, tri, "c_tri")
        cfinal()
        lst = sb("lst", [128, 4, 6], F32, s3b)
        lmv = sb("lmv", [128, 2], F32, s3b)
        lrs = sb("lrs", [128, 1], F32, s3b)
        x1Tq = [sb("x1Tq%d" % i, [128, 2, 4, 128], BF16, s3b) for i in range(2)]
        xlo = sb("xlo", [128, DM], BF16, s3b)
        wr_hl = sb("wr_hl", [128, 2, NDC, NE], BF16, s3b)
        logit = sb("logit", [128, NTC, NE], F32, s3b)
        top8 = sb("top8", [128, NTC, 8], F32, s3b)
        nmx = sb("nmx", [128, NTC], F32, s3b)
        mask = sb("mask", [128, NTC, NE], F32, s3b)
        maskb = sb("maskb", [128, NTC, NE], BF16, s3b)
        posmb = sb("posmb", [128, NTC, NE], BF16, s3b)
        den = sb("den", [128, NTC], F32, s3b)
        def f(act):
            return act.copy(out=wr_hl[:, 0, :, :], in_=c_wr[:, :, :])
        S.op("act", f, reads=("c_wr",), writes=("wr_hi",))

        def f(dve):
            return dve.tensor_tensor(out=wr_hl[:, 1, :, :], in0=c_wr[:, :, :], in1=wr_hl[:, 0, :, :], op=ALU.subtract)
        S.op("dve", f, reads=("c_wr", "wr_hi"), writes=("wr_lo",))
        for tc in range(NTC):
            ak = "acc%d" % tc
            x1f = acc[:, tc, :]
            layer_norm(x1f, ak, c_lw, c_lb, ("c_lw", "c_lb"), x1f, ak, lst, lmv, lrs, "1")

            def f(act, tc=tc, x1f=x1f):
                return act.copy(out=x1bf[:, tc, :], in_=x1f)
            S.op("act", f, reads=(ak,), writes=("x1bf",))
            def f(dve, tc=tc, x1f=x1f):
                return dve.tensor_tensor(out=xlo[:, :], in0=x1f, in1=x1bf[:, tc, :], op=ALU.subtract)
            S.op("dve", f, reads=(ak, "x1bf"), writes=("xlo",))
            for q4 in range(4):
                xq = x1Tq[q4 % 2]
                xqk = "x1Tq%d" % (q4 % 2)
                for hl in range(2):
                    pb = hl
                    pk = "ps%d" % pb

                    def f(pe, q4=q4, pb=pb, hl=hl, tc=tc):
                        ins = None
                        for i in range(4):
                            dc = q4 * 4 + i
                            src = x1bf[:, tc, dc * 128:(dc + 1) * 128] if hl == 0 else xlo[:, dc * 128:(dc + 1) * 128]
                            ins = pe.matmul(ps[pb][:, i * 128:(i + 1) * 128], src, c_qdiag[:, 0, :], start=True, stop=True)
                        return ins
                    S.op("pe", f, reads=("x1bf", "xlo", "c_qdiag"), writes=(pk,))

                    def f(act, xq=xq, pb=pb, hl=hl):
                        return act.copy(out=xq[:, hl, :, :], in_=ps[pb][:, :].rearrange("p (a t) -> p a t", a=4))
                    S.op("act", f, reads=(pk,), writes=(xqk,))

                def f(pe, q4=q4, xq=xq):
                    ins = None
                    for i in range(4):
                        dc = q4 * 4 + i
                        for j, (xh, wh) in enumerate(((0, 0), (0, 1), (1, 0))):
                            ins = pe.matmul(ps[2][:, 0:NE], xq[:, xh, i, :], wr_hl[:, wh, dc, :],
                                            start=(dc == 0 and j == 0), stop=(dc == NDC - 1 and j == 2))
                    return ins
                S.op("pe", f, reads=(xqk, "wr_hi", "wr_lo"), writes=("ps2",))

            def f(act, tc=tc, x1f=x1f):
                return act.mul(out=x1f, in_=x1f, mul=DN_ALPHA)
            S.op("act", f, reads=(ak,), writes=(ak,))

            def f(dve, tc=tc):
                return dve.tensor_tensor(out=logit[:, tc, :], in0=ps[2][:, 0:NE], in1=c_br[:, :], op=ALU.add)
            S.op("dve", f, reads=("ps2", "c_br"), writes=("logit",))
        if stop_after == "ln1":
            return dump([(acc[:, 0, :], DM), (acc[:, 7, :], DM), (logit[:, :, :].rearrange("p a b -> p (a b)"), NTC * NE)],
                        ("acc0", "acc7", "logit"))
        for tc in range(NTC):
            def f(dve, tc=tc):
                return dve.max(out=top8[:, tc, :], in_=logit[:, tc, :])
            S.op("dve", f, reads=("logit",), writes=("top8",))
        for tc in range(NTC):
            def f(dve, tc=tc):
                return dve.tensor_scalar(out=mask[:, tc, :], in0=logit[:, tc, :], scalar1=top8[:, tc, 3:4], scalar2=None, op0=ALU.is_ge)
            S.op("dve", f, reads=("logit", "top8"), writes=("mask",))

        def f(dve):
            return dve.tensor_scalar(out=nmx[:, :], in0=top8[:, :, 0], scalar1=-1.0, scalar2=None, op0=ALU.mult)
        S.op("dve", f, reads=("top8",), writes=("nmx",))
        for tc in range(NTC):
            def f(act, tc=tc):
                return act.activation(out=gates[:, tc, :], in_=logit[:, tc, :], func=AF.Exp, bias=nmx[:, tc:tc + 1], scale=1.0)
            S.op("act", f, reads=("logit", "nmx"), writes=("gates",))

        def f(dve):
            return dve.tensor_tensor(out=gates[:, :, :], in0=gates[:, :, :], in1=mask[:, :, :], op=ALU.mult)
        S.op("dve", f, reads=("gates", "mask"), writes=("gates",))

        def f(dve):
            return dve.tensor_reduce(out=den[:, :], in_=gates[:, :, :], axis=mybir.AxisListType.X, op=ALU.add)
        S.op("dve", f, reads=("gates",), writes=("den",))

        def f(dve):
            return dve.reciprocal(out=den[:, :], in_=den[:, :])
        S.op("dve", f, reads=("den",), writes=("den",))
        for tc in range(NTC):
            def f(dve, tc=tc):
                return dve.tensor_scalar(out=gates[:, tc, :], in0=gates[:, tc, :], scalar1=den[:, tc:tc + 1], scalar2=None, op0=ALU.mult)
            S.op("dve", f, reads=("gates", "den"), writes=("gates",))

        def f(act):
            return act.copy(out=maskb[:, :, :], in_=mask[:, :, :])
        S.op("act", f, reads=("mask",), writes=("maskb",))
        for tc in range(NTC):
            pb = 4 + tc % 2
            pk = "ps%d" % pb
            pairs = [(c_tri[:, 0, :], maskb[:, t2, :]) for t2 in range(tc)] + [(c_tri[:, 1, :], maskb[:, tc, :])]
            mm(ps[pb][:, 0:NE], pairs, reads=("maskb", "c_tri"), writes=(pk,))

            def f(dve, tc=tc, pb=pb):
                return dve.scalar_tensor_tensor(out=posm[:, tc, :], in0=ps[pb][:, 0:NE], scalar=1.0, in1=mask[:, tc, :],
                                                op0=ALU.add, op1=ALU.mult)
            S.op("dve", f, reads=(pk, "mask"), writes=("posm",))

        def f(dve):
            return dve.tensor_scalar(out=posm[:, :, :], in0=posm[:, :, :], scalar1=-1.0, scalar2=float(CAP), op0=ALU.add, op1=ALU.min)
        S.op("dve", f, reads=("posm",), writes=("posm",))

        def f(act):
            return act.copy(out=posmb[:, :, :], in_=posm[:, :, :])
        S.op("act", f, reads=("posm",), writes=("posmb",))
        for half in range(2):
            pb = 6 + half
            pk = "ps%d" % pb

            def f(pe, half=half, pb=pb):
                ins = None
                for i in range(4):
                    tc = half * 4 + i
                    ins = pe.matmul(ps[pb][0:NE, i * 128:(i + 1) * 128], posmb[:, tc, :], c_qdiag[:, 0, :], start=True, stop=True)
                return ins
            S.op("pe", f, reads=("posmb", "c_qdiag"), writes=(pk,))

            def f(act, half=half, pb=pb):
                return act.copy(out=posmT[:, half * 512:(half + 1) * 512], in_=ps[pb][0:NE, :])
            S.op("act", f, reads=(pk,), writes=("posmT",))
        if stop_after == "route":
            return dump([(gates[:, :, :].rearrange("p a b -> p (a b)"), NTC * NE), (posm[:, :, :].rearrange("p a b -> p (a b)"), NTC * NE)],
                        ("gates", "posm"))
        S.barrier()
        S.flush()

    with ExitStack() as s4:
        sel_e = [sb("sel_e%d" % i, [NE, 128], BF16, s4) for i in range(2)]
        Sg = sb("Sg", [128, NTC, CAP], BF16, s4)
        ST = sb("ST", [128, 2, T], BF16, s4)
        XeT = sb("XeT", [128, NDC, CAP], BF16, s4)
        HT = sb("HT", [128, NDC, CAP], BF16, s4)
        Yb = [sb("Yb%d" % i, [128, 2, 512], BF16, s4) for i in range(2)]
        tA = [sb("tA%d" % i, [128, CAP], F32, s4) for i in range(2)]
        tB = [sb("tB0", [128, CAP], F32, s4)] * 2
        tC = [sb("tC%d" % i, [128, CAP], F32, s4) for i in range(2)]
        tD = [sb("tD0", [128, CAP], F32, s4)] * 2

        def scatter(e, dg):
            yb = Yb[dg % 2]
            yk = "Yb%d" % (dg % 2)
            for tc in range(NTC):
                pb = 6 + tc % 2
                pk = "ps%d" % pb
                mm(ps[pb][:, :], [(ST[0:rows, jc, tc * 128:(tc + 1) * 128], yb[0:rows, jc, :]) for jc, (_, rows) in enumerate(JB)],
                   reads=("ST", yk), writes=(pk,))

                def f(dve, pb=pb, tc=tc, e=e, dg=dg):
                    return dve.scalar_tensor_tensor(out=acc[:, tc, dg * 512:(dg + 1) * 512], in0=ps[pb][:, :],
                                                    scalar=gates[:, tc, e:e + 1], in1=acc[:, tc, dg * 512:(dg + 1) * 512],
                                                    op0=ALU.mult, op1=ALU.add)
                S.op("dve", f, reads=(pk, "gates", "acc%d" % tc), writes=("acc%d" % tc,))

        for e in range(n_exp):
            base = 4 + e * 12
            for tc in range(NTC):
                def f(pool, tc=tc, e=e):
                    return pool.tensor_scalar(out=Sg[:, tc, :], in0=c_iotac[:, :], scalar1=posm[:, tc, e:e + 1], scalar2=None, op0=ALU.is_equal)
                S.op("pool", f, reads=("c_iotac", "posm"), writes=("Sg",))
            se = sel_e[e % 2]
            sek = "sel_e%d" % (e % 2)

            def f(pool, se=se, e=e):
                return pool.tensor_scalar(out=se[:, :], in0=c_pidx[:, :], scalar1=float(e), scalar2=None, op0=ALU.is_equal)
            S.op("pool", f, reads=("c_pidx",), writes=(sek,))
            for half in range(2):
                pb = half
                pk = "ps%d" % pb
                mm(ps[pb][:, :], [(se[:, :], posmT[:, half * 512:(half + 1) * 512])], reads=(sek, "posmT"), writes=(pk,))
                for jc in range(2):
                    def f(dve, pb=pb, jc=jc, half=half):
                        return dve.tensor_scalar(out=ST[:, jc, half * 512:(half + 1) * 512], in0=ps[pb][:, :],
                                                 scalar1=c_iotap[:, jc:jc + 1], scalar2=None, op0=ALU.is_equal)
                    S.op("dve", f, reads=(pk, "c_iotap"), writes=("ST",))
            for dp in range(NDC // 2):
                pb = dp % 2
                pk = "ps%d" % pb

                def f(pe, dp=dp, pb=pb):
                    ins = None
                    for i in range(2):
                        dc = dp * 2 + i
                        for tc in range(NTC):
                            ins = pe.matmul(ps[pb][:, i * CAP:(i + 1) * CAP], x1bf[:, tc, dc * 128:(dc + 1) * 128], Sg[:, tc, :],
                                            start=(tc == 0), stop=(tc == NTC - 1))
                    return ins
                S.op("pe", f, reads=("x1bf", "Sg"), writes=(pk,))

                def f(act, dp=dp, pb=pb):
                    return act.copy(out=XeT[:, dp * 2:dp * 2 + 2, :], in_=ps[pb][:, 0:2 * CAP].rearrange("p (a j) -> p a j", a=2))
                S.op("act", f, reads=(pk,), writes=("XeT",))
            for fg in range(4):
                slg, slu = get_pieces(base + fg * 2, 2)
                for fcl in range(4):
                    fc = fg * 4 + fcl
                    par = fc % 2
                    pb = 2 + par
                    pk = "ps%d" % pb

                    def f(pe, pb=pb, slg=slg, slu=slu, fcl=fcl):
                        ins = None
                        for dc in range(NDC):
                            ins = pe.matmul(ps[pb][:, 0:CAP], wslot[slg][:, dc, fcl * 128:(fcl + 1) * 128], XeT[:, dc, :],
                                            start=(dc == 0), stop=(dc == NDC - 1))
                        for dc in range(NDC):
                            ins = pe.matmul(ps[pb][:, CAP:2 * CAP], wslot[slu][:, dc, fcl * 128:(fcl + 1) * 128], XeT[:, dc, :],
                                            start=(dc == 0), stop=(dc == NDC - 1))
                        return ins
                    S.op("pe", f, reads=("XeT", "wslot%d" % slg, "wslot%d" % slu), writes=(pk,))
                    a_, b_, c_, d_ = tA[par], tB[par], tC[par], tD[par]
                    ka, kb, kc, kd = "tA%d" % par, "tB0", "tC%d" % par, "tD0"

                    def f(dve, pb=pb, a_=a_, e=e, fc=fc):
                        return dve.tensor_scalar(out=a_[:, :], in0=ps[pb][:, 0:CAP], scalar1=c_bgu[:, 0, e, fc:fc + 1], scalar2=7.0,
                                                 op0=ALU.add, op1=ALU.min)
                    S.op("dve", f, reads=(pk, "c_bgu"), writes=(ka,))

                    def f(act, a_=a_, b_=b_):
                        return act.activation(out=b_[:, :], in_=a_[:, :], func=AF.Sigmoid, scale=1.702)
                    S.op("act", f, reads=(ka,), writes=(kb,))

                    def f(dve, pb=pb, c_=c_, e=e, fc=fc):
                        return dve.tensor_scalar(out=c_[:, :], in0=ps[pb][:, CAP:2 * CAP], scalar1=c_bgu[:, 1, e, fc:fc + 1], scalar2=7.0,
                                                 op0=ALU.add, op1=ALU.min)
                    S.op("dve", f, reads=(pk, "c_bgu"), writes=(kc,))

                    def f(dve, c_=c_):
                        return dve.tensor_scalar(out=c_[:, :], in0=c_[:, :], scalar1=-7.0, scalar2=1.0, op0=ALU.max, op1=ALU.add)
                    S.op("dve", f, reads=(kc,), writes=(kc,))

                    def f(pool, a_=a_, b_=b_, d_=d_):
                        return pool.tensor_tensor(out=d_[:, :], in0=a_[:, :], in1=b_[:, :], op=ALU.mult)
                    S.op("pool", f, reads=(ka, kb), writes=(kd,))

                    def f(pool, c_=c_, d_=d_, fc=fc):
                        return pool.tensor_tensor(out=HT[:, fc, :], in0=d_[:, :], in1=c_[:, :], op=ALU.mult)
                    S.op("pool", f, reads=(kc, kd), writes=("HT",))
            for dg in range(4):
                sld = get_piece(base + 8 + dg)
                yb = Yb[dg % 2]
                yk = "Yb%d" % (dg % 2)
                for jc, (j0, rows) in enumerate(JB):
                    pb = 4 + jc
                    pk = "ps%d" % pb
                    mm(ps[pb][0:rows, :], [(HT[:, fc, j0:j0 + rows], wslot[sld][:, fc, :]) for fc in range(NDC)],
                       reads=("HT", "wslot%d" % sld), writes=(pk,))

                    def f(act, pb=pb, yb=yb, jc=jc, rows=rows):
                        return act.copy(out=yb[0:rows, jc, :], in_=ps[pb][0:rows, :])
                    S.op("act", f, reads=(pk,), writes=(yk,))
                if dg >= 1:
                    scatter(e, dg - 1)
            scatter(e, 3)
        S.barrier()
        S.flush()
    with ExitStack() as s4:
        gatesT = sb("gatesT", [NE, 2, T], BF16, s4)
        g_hl = sb("g_hl", [128, 2, NTC, NE], BF16, s4)
        c_bdn = sb("c_bdn", [NE, DM], F32, s4)
        b_hl = sb("b_hl", [NE, 2, DM], BF16, s4)
        cload(c_bdn, b_down, "c_bdn")
        cfinal()

        def f(act):
            return act.copy(out=g_hl[:, 0, :, :], in_=gates[:, :, :])
        S.op("act", f, reads=("gates",), writes=("g_hi",))

        def f(dve):
            return dve.tensor_tensor(out=g_hl[:, 1, :, :], in0=gates[:, :, :], in1=g_hl[:, 0, :, :], op=ALU.subtract)
        S.op("dve", f, reads=("gates", "g_hi"), writes=("g_lo",))

        def f(act):
            return act.copy(out=b_hl[:, 0, :], in_=c_bdn[:, :])
        S.op("act", f, reads=("c_bdn",), writes=("b_hi",))

        def f(dve):
            return dve.tensor_tensor(out=b_hl[:, 1, :], in0=c_bdn[:, :], in1=b_hl[:, 0, :], op=ALU.subtract)
        S.op("dve", f, reads=("c_bdn", "b_hi"), writes=("b_lo",))
        for hl in range(2):
            for half in range(2):
                pb = 2 + half
                pk = "ps%d" % pb

                def f(pe, half=half, pb=pb, hl=hl):
                    ins = None
                    for i in range(4):
                        tc = half * 4 + i
                        ins = pe.matmul(ps[pb][0:NE, i * 128:(i + 1) * 128], g_hl[:, hl, tc, :], c_qdiag[:, 0, :], start=True, stop=True)
                    return ins
                S.op("pe", f, reads=("g_hi", "g_lo", "c_qdiag"), writes=(pk,))

                def f(act, half=half, pb=pb, hl=hl):
                    return act.copy(out=gatesT[:, hl, half * 512:(half + 1) * 512], in_=ps[pb][0:NE, :])
                S.op("act", f, reads=(pk,), writes=("gatesT",))
        for tc in range(NTC):
            for dg in range(4):
                pb = (tc * 4 + dg) % 2
                pk = "ps%d" % pb
                mm(ps[pb][:, :], [(gatesT[:, gh, tc * 128:(tc + 1) * 128], b_hl[:, bh, dg * 512:(dg + 1) * 512])
                                  for gh, bh in ((0, 0), (0, 1), (1, 0))],
                   reads=("gatesT", "b_hi", "b_lo"), writes=(pk,))

                def f(dve, pb=pb, tc=tc, dg=dg):
                    return dve.tensor_tensor(out=acc[:, tc, dg * 512:(dg + 1) * 512], in0=ps[pb][:, :],
                                             in1=acc[:, tc, dg * 512:(dg + 1) * 512], op=ALU.add)
                S.op("dve", f, reads=(pk, "acc%d" % tc), writes=("acc%d" % tc,))
        if stop_after == "moe":
            return dump([(acc[:, 0, :], DM), (acc[:, 7, :], DM)], ("acc0", "acc7"))
        S.barrier()
        S.flush()

    with ExitStack() as s5:
        c_lw = sb("c_lw2", [128, DM], F32, s5)
        c_lb = sb("c_lb2", [128, DM], F32, s5)
        c_pw = arB[:, 0:2048]
        c_pp = arB[:, 2048:4096].bitcast(BF16).rearrange("p (c d) -> p c d", c=2)
        c_pT = sb("c_pT", [128, 2, T], BF16, s5)
        cload(c_lw, rep_ln2w, "c_lw2")
        cload(c_lb, rep_ln2b, "c_lb2")
        cload(c_pw, rep_plew, "c_pw")
        cload(c_pp, w_pp.rearrange("(c p) d -> p c d", p=128), "c_pp", eng="pool")
        cload(c_pT, pT.rearrange("(c p) t -> p c t", p=128), "c_pT", eng="pool")
        cfinal()
        lst = sb("lst2", [128, 4, 6], F32, s5)
        lmv = sb("lmv2", [128, 2], F32, s5)
        lrs = sb("lrs2", [128, 1], F32, s5)
        x2b = sb("x2b", [128, DM], BF16, s5)
        x2T = sb("x2T", [128, NDC, 128], BF16, s5)
        ebuf = arB[:, 4096:6144]
        esq = sb("esq", [128, 512], F32, s5)
        ess = sb("ess", [128, 4], F32, s5)
        ers = sb("ers", [128, 1], F32, s5)
        gbuf = arB[:, 6144:8192]
        pslots = get_pieces(len(srcs) - 4, 4)

        if stop_after == "s5pre":
            return dump([(acc[:, 0, :], DM), (c_pw[:, :], DM)], ("acc0", "c_pw", "c_pp", "c_pT", "c_lw2", "c_lb2"))
        for tc in range(NTC):
            ak = "acc%d" % tc
            x2 = acc[:, tc, :]
            layer_norm(x2, ak, c_lw, c_lb, ("c_lw2", "c_lb2"), x2, ak, lst, lmv, lrs, "2")

            def f(act, x2=x2):
                return act.copy(out=x2b[:, :], in_=x2)
            S.op("act", f, reads=(ak,), writes=("x2b",))
            if stop_after == "ln2":
                continue
            for q4 in range(4):
                pb = q4 % 2
                pk = "ps%d" % pb

                def f(pe, q4=q4, pb=pb):
                    ins = None
                    for i in range(4):
                        dc = q4 * 4 + i
                        ins = pe.matmul(ps[pb][:, i * 128:(i + 1) * 128], x2b[:, dc * 128:(dc + 1) * 128], c_qdiag[:, 0, :], start=True, stop=True)
                    return ins
                S.op("pe", f, reads=("x2b", "c_qdiag"), writes=(pk,))

                def f(act, q4=q4, pb=pb):
                    return act.copy(out=x2T[:, q4 * 4:(q4 + 1) * 4, :], in_=ps[pb][:, :].rearrange("p (a t) -> p a t", a=4))
                S.op("act", f, reads=(pk,), writes=("x2T",))
            for dg in range(4):
                pb = 2 + dg % 2
                pk = "ps%d" % pb
                mm(ps[pb][:, :], [(c_pT[:, pc, tc * 128:(tc + 1) * 128], c_pp[:, pc, dg * 512:(dg + 1) * 512]) for pc in range(2)],
                   reads=("c_pT", "c_pp"), writes=(pk,))

                def f(act, pb=pb, dg=dg):
                    return act.copy(out=ebuf[:, dg * 512:(dg + 1) * 512], in_=ps[pb][:, :])
                S.op("act", f, reads=(pk,), writes=("ebuf",))

                def f(dve, dg=dg):
                    return dve.tensor_tensor(out=esq[:, :], in0=ebuf[:, dg * 512:(dg + 1) * 512], in1=ebuf[:, dg * 512:(dg + 1) * 512], op=ALU.mult)
                S.op("dve", f, reads=("ebuf",), writes=("esq",))

                def f(dve, dg=dg):
                    return dve.tensor_reduce(out=ess[:, dg:dg + 1], in_=esq[:, :], axis=mybir.AxisListType.X, op=ALU.add)
                S.op("dve", f, reads=("esq",), writes=("ess",))

            def f(dve):
                return dve.tensor_reduce(out=ers[:, :], in_=ess[:, :], axis=mybir.AxisListType.X, op=ALU.add)
            S.op("dve", f, reads=("ess",), writes=("ers",))

            def f(act):
                return act.activation(out=ers[:, :], in_=ers[:, :], func=AF.Sqrt, bias=c_eps[:, 0:1], scale=1.0 / DM)
            S.op("act", f, reads=("ers", "c_eps"), writes=("ers",))

            def f(dve):
                return dve.reciprocal(out=ers[:, :], in_=ers[:, :])
            S.op("dve", f, reads=("ers",), writes=("ers",))

            def f(dve):
                return dve.scalar_tensor_tensor(out=ebuf[:, :], in0=ebuf[:, :], scalar=ers[:, 0:1], in1=c_pw[:, :], op0=ALU.mult, op1=ALU.mult)
            S.op("dve", f, reads=("ebuf", "ers", "c_pw"), writes=("ebuf",))
            for dg in range(4):
                pb = 4 + dg % 2
                pk = "ps%d" % pb
                sl = pslots[dg]
                mm(ps[pb][:, :], [(x2T[:, dc, :], wslot[sl][:, dc, :]) for dc in range(NDC)],
                   reads=("x2T", "wslot%d" % sl), writes=(pk,))

                def f(act, pb=pb, dg=dg):
                    return act.activation(out=gbuf[:, dg * 512:(dg + 1) * 512], in_=ps[pb][:, :], func=AF.Sigmoid)
                S.op("act", f, reads=(pk,), writes=("gbuf",))

            def f(pool):
                return pool.tensor_tensor(out=gbuf[:, :], in0=gbuf[:, :], in1=ebuf[:, :], op=ALU.mult)
            S.op("pool", f, reads=("gbuf", "ebuf"), writes=("gbuf",))

            def f(pool, x2=x2):
                return pool.tensor_tensor(out=x2, in0=x2, in1=gbuf[:, :], op=ALU.add)
            S.op("pool", f, reads=("gbuf", ak), writes=(ak,))

            if stop_after == "ple":
                continue

            def f(eng, sem, tc=tc, x2=x2):
                eng.dma_start(out=out[tc * 128:(tc + 1) * 128, :], in_=x2).then_inc(sem, 16)
            S.dma("pool", f, 1, "outst", reads=(ak,))
        if stop_after in ("ln2", "ple"):
            return dump([(acc[:, 0, :], DM), (acc[:, 7, :], DM)], ("acc0", "acc7"))
        S.barrier()
        S.flush()


def _consts():
    h = np.arange(8, dtype=np.float64)
    log_gamma = np.log1p(-np.exp2(-5.0 - h))
    idx = np.arange(128, dtype=np.float64)
    diff = idx[None, :] - idx[:, None]
    decT = np.where(diff[:, None, :] >= 0, np.exp(np.maximum(diff, 0.0)[:, None, :] * log_gamma[None, :, None]), 0.0)
    kdec = np.exp((127.0 - idx)[:, None] * log_gamma[None, :])
    qdec = np.exp((idx[:, None] + 1.0) * log_gamma[None, :])
    chunk_dec = np.exp(128.0 * log_gamma)
    qdiag = np.zeros((128, 9, 128), np.float32)
    qdiag[:, 0, :] = np.eye(128)
    for hh in range(8):
        qdiag[:, 1 + hh, :] = np.diag(qdec[:, hh])
    tri = np.zeros((128, 2, 128), np.float32)
    tri[:, 0, :] = 1.0
    tri[:, 1, :] = (idx[:, None] < idx[None, :]).astype(np.float32)
    return dict(
        decT=decT.astype(np.float32), kdec=kdec.astype(np.float32),
        qdiag=qdiag.astype(ml_dtypes.bfloat16), ident_f=np.eye(128, dtype=np.float32),
        tri=tri.astype(ml_dtypes.bfloat16),
        iota_c=np.broadcast_to(np.arange(CAP, dtype=np.float32)[None, :], (128, CAP)).copy(),
        iota_p=np.stack([np.arange(128, dtype=np.float32), np.arange(128, dtype=np.float32) + 128], axis=1),
        pidx=np.broadcast_to(np.arange(NE, dtype=np.float32)[:, None], (NE, 128)).copy(),
    ), chunk_dec


def _rope_tables(pos0):
    inv = 10000.0 ** (-np.arange(0, 128, 2, dtype=np.float32) / 128)
    pos = pos0 + np.arange(T, dtype=np.float32)
    ang = pos[:, None] * inv[None, :]
    cos = np.cos(ang).astype(np.float32)
    sin = np.sin(ang).astype(np.float32)
    return cos, sin


def _prep_inputs(inp, n_exp_decl=None):
    f = lambda a: np.ascontiguousarray(np.asarray(a, dtype=np.float32))
    x = f(inp["x"])
    p = f(inp["p"])[0]
    cst, _ = _consts()
    rep = lambda v, n=128: np.ascontiguousarray(np.broadcast_to(f(v).reshape(1, -1), (n, f(v).size)))
    lruvec = np.zeros((128, 8, 8), np.float32)
    cw = f(inp["conv_w"])[0]
    for j in range(4):
        lruvec[:, :, j] = cw[j].reshape(8, 128).T
    lruvec[:, :, 4] = f(inp["conv_b"])[0].reshape(8, 128).T
    lruvec[:, :, 5] = f(inp["lru_ba"])[0].T
    lruvec[:, :, 6] = f(inp["lru_bx"])[0].T
    lruvec[:, :, 7] = f(inp["lru_lam"])[0].reshape(8, 128).T
    bgu = np.zeros((128, 2, NE, 16), np.float32)
    bgu[:, 0] = f(inp["b_gate"])[0].reshape(NE, 16, 128).transpose(2, 0, 1)
    bgu[:, 1] = f(inp["b_up"])[0].reshape(NE, 16, 128).transpose(2, 0, 1)
    shared = dict(
        w_in=f(inp["w_in"])[0], w_out=f(inp["w_out"])[0], w_ple_gate=f(inp["w_ple_gate"])[0], w_ple_proj=f(inp["w_ple_proj"])[0],
        w_router=f(inp["w_router"])[0], lru_wa=f(inp["lru_wa"])[0], lru_wx=f(inp["lru_wx"])[0],
        lruvec=lruvec, bgu=bgu, b_down=f(inp["b_down"])[0],
        rep_ln1w=rep(inp["ln1_w"]), rep_ln1b=rep(inp["ln1_b"]), rep_ln2w=rep(inp["ln2_w"]), rep_ln2b=rep(inp["ln2_b"]),
        rep_plew=rep(inp["ple_norm_w"]), rep_gnw=rep(inp["ret_gn_w"]), rep_br=rep(inp["b_router"]),
        **cst,
    )
    for nm in ("w_gate", "w_up", "w_down"):
        ne = NE if n_exp_decl is None else n_exp_decl
        w = f(inp[nm])[0][:ne]
        w = np.ascontiguousarray(w.reshape(ne, NDC, 128, 4, 512).transpose(0, 3, 2, 1, 4)).reshape(ne, 4, 128, 8192)
        for g in range((ne + EG - 1) // EG):
            shared["%s_%d" % (nm, g)] = w[g * EG:min((g + 1) * EG, ne)]
    scale_k = 128.0 ** -0.5
    in_maps = []
    for c in range(NCORES):
        b, hf = c // 2, c % 2
        own = slice(hf * T, (hf + 1) * T)
        m = dict(shared)
        m["xT_own"] = np.ascontiguousarray(x[b, own, :].T)
        m["xT_pre"] = np.ascontiguousarray(x[b, 0:T, :].T) if hf == 1 else np.zeros((DM, T), np.float32)
        m["x_own"] = np.ascontiguousarray(x[b, own, :])
        m["pT"] = np.ascontiguousarray(p[b, own, :].T)
        m["pf"] = np.full((128, 1), float(hf), np.float32)
        cos_o, sin_o = _rope_tables(float(hf * T))
        cos_p, sin_p = _rope_tables(0.0)
        lay = lambda a: a.reshape(NTC, 128, 64).transpose(1, 0, 2)
        m["rope_own"] = np.ascontiguousarray(np.stack([lay(cos_o), lay(sin_o), lay(cos_o * scale_k), lay(sin_o * scale_k)], axis=1))
        m["rope_pre"] = np.ascontiguousarray(np.stack([lay(cos_p * scale_k), lay(sin_p * scale_k)], axis=1))
        in_maps.append(m)
    return in_maps


_NC_CACHE = {}


def kernel(**inputs):
    in_maps = _prep_inputs(inputs)
    if "nc" not in _NC_CACHE:
        _NC_CACHE["nc"] = build()
    nc = _NC_CACHE["nc"]
    res = run_bass_kernel_spmd(nc, in_maps, core_ids=list(range(NCORES)))
    outp = np.zeros((4, 2048, DM), np.float32)
    for c in range(NCORES):
        b, hf = c // 2, c % 2
        outp[b, hf * T:(hf + 1) * T, :] = res.results[c]["out"]
    return outp
```

```python
import math
from contextlib import ExitStack

import numpy as np
import ml_dtypes

import concourse.bass as bass
import concourse.mybir as mybir
from concourse.bass_utils import run_bass_kernel_spmd

F32 = mybir.dt.float32
BF16 = mybir.dt.bfloat16
ALU = mybir.AluOpType
AF = mybir.ActivationFunctionType

NCORES = 8
DM = 2048
T = 1024
NTC = 8
NDC = 16
NE = 32
CAP = 192
JB = ((0, 128), (128, 128))
LN_EPS = 1e-5
DN_ALPHA = 2.0 ** 0.25
NSLOT = 4
EG = 8


class Sched:
    ENG = ("pe", "act", "dve", "pool", "sp")

    def __init__(self, nc, es):
        self.nc = nc
        self.es = es
        self.sem = {e: es.enter_context(nc.semaphore("s_" + e)) for e in self.ENG}
        self.cnt = {e: 0 for e in self.ENG}
        self.waited = {e: {} for e in self.ENG}
        self.ops = {e: [] for e in self.ENG}
        self.last_w = {}
        self.readers = {}
        self.dsem = {}
        self.dcnt = {}

    def _deps(self, eng, reads, writes):
        deps = {}

        def add(d):
            if d is None:
                return
            s, v = d
            if deps.get(id(s), (None, 0))[1] < v:
                deps[id(s)] = (s, v)

        for k in reads:
            add(self.last_w.get(k))
        for k in writes:
            add(self.last_w.get(k))
            for d in self.readers.get(k, {}).values():
                add(d)
        waits = []
        for sid, (s, v) in deps.items():
            if self.waited[eng].get(sid, 0) < v:
                self.waited[eng][sid] = v
                waits.append((s, v))
        return waits

    def _mark(self, me, reads, writes):
        for k in reads:
            self.readers.setdefault(k, {})[id(me[0])] = me
        for k in writes:
            self.last_w[k] = me
            self.readers[k] = {}

    def op(self, eng, fn, reads=(), writes=()):
        waits = self._deps(eng, reads, writes)
        self.cnt[eng] += 1
        me = (self.sem[eng], self.cnt[eng])
        self.ops[eng].append((waits, fn, self.sem[eng], 1))
        self._mark(me, reads, writes)

    def dma(self, eng, fn, ndma, semname, reads=(), writes=()):
        if semname not in self.dsem:
            self.dsem[semname] = self.es.enter_context(self.nc.semaphore("d_" + semname))
            self.dcnt[semname] = 0
        waits = self._deps(eng, reads, writes)
        self.dcnt[semname] += 16 * ndma
        me = (self.dsem[semname], self.dcnt[semname])
        self.ops[eng].append((waits, fn, self.dsem[semname], None))
        self._mark(me, reads, writes)

    def barrier(self):
        allsem = [(self.sem[e], self.cnt[e]) for e in self.ENG if self.cnt[e] > 0]
        allsem += [(self.dsem[k], self.dcnt[k]) for k in self.dsem if self.dcnt[k] > 0]
        for e in self.ENG:
            waits = []
            for s, v in allsem:
                if self.waited[e].get(id(s), 0) < v:
                    self.waited[e][id(s)] = v
                    waits.append((s, v))
            if waits:
                self.ops[e].append((waits, None, None, 0))

    def flush(self):
        with self.nc.Block() as block:
            decos = {"pe": block.tensor, "act": block.scalar, "dve": block.vector,
                     "pool": block.gpsimd, "sp": block.sync}
            for e in self.ENG:
                ops = self.ops[e]
                if not ops:
                    continue

                def body(engine, ops=ops):
                    for waits, fn, sem, inc in ops:
                        for s, v in waits:
                            engine.wait_ge(s, v)
                        if fn is None:
                            continue
                        if inc is None:
                            fn(engine, sem)
                        else:
                            fn(engine).then_inc(sem, inc)

                decos[e](body)
                self.ops[e] = []


class _Stop(Exception):
    pass


def build(stop_after=None, n_exp_run=None, n_exp_decl=None):
    nc = bass.Bass("TRN2", target_bir_lowering=False)
    es = ExitStack()
    try:
        _body(nc, es, stop_after, n_exp_run, n_exp_decl)
    except _Stop:
        pass
    return nc


def _body(nc, es, stop_after, n_exp_run, n_exp_decl=None):
    S = Sched(nc, es)

    def din(name, shape, dt=F32):
        return nc.dram_tensor(name, list(shape), dt, kind="ExternalInput").ap()

    xT_own = din("xT_own", [DM, T])
    xT_pre = din("xT_pre", [DM, T])
    x_own = din("x_own", [T, DM])
    pT = din("pT", [256, T])
    w_in = din("w_in", [DM, 6144])
    w_out = din("w_out", [DM, DM])
    n_exp = NE if stop_after in (None, "moe", "ln2", "s5pre", "ple") else 0
    if n_exp_run is not None:
        n_exp = n_exp_run
    NEW = max(n_exp, 1) if n_exp_decl is None else n_exp_decl
    NG = (NEW + EG - 1) // EG
    w_gate = [din("w_gate_%d" % g, [min(EG, NEW - g * EG), 4, 128, 8192]) for g in range(NG)]
    w_up = [din("w_up_%d" % g, [min(EG, NEW - g * EG), 4, 128, 8192]) for g in range(NG)]
    w_down = [din("w_down_%d" % g, [min(EG, NEW - g * EG), 4, 128, 8192]) for g in range(NG)]
    w_pg = din("w_ple_gate", [DM, DM])
    w_pp = din("w_ple_proj", [256, DM])
    w_r = din("w_router", [DM, NE])
    lru_wa = din("lru_wa", [8, 128, 128])
    lru_wx = din("lru_wx", [8, 128, 128])
    lruvec = din("lruvec", [128, 8, 8])
    bgu = din("bgu", [128, 2, NE, 16])
    b_down = din("b_down", [NE, DM])
    rep_ln1w = din("rep_ln1w", [128, DM])
    rep_ln1b = din("rep_ln1b", [128, DM])
    rep_ln2w = din("rep_ln2w", [128, DM])
    rep_ln2b = din("rep_ln2b", [128, DM])
    rep_plew = din("rep_plew", [128, DM])
    rep_gnw = din("rep_gnw", [128, 1024])
    rep_br = din("rep_br", [128, NE])
    pf_in = din("pf", [128, 1])
    rope_own = din("rope_own", [128, 4, NTC, 64])
    rope_pre = din("rope_pre", [128, 2, NTC, 64])
    decT = din("decT", [128, 8, 128])
    kdec = din("kdec", [128, 8])
    qdiag = din("qdiag", [128, 9, 128], BF16)
    ident_f = din("ident_f", [128, 128])
    tri = din("tri", [128, 2, 128], BF16)
    iota_c = din("iota_c", [128, CAP])
    iota_p = din("iota_p", [128, 2])
    pidx_in = din("pidx", [128, 128])

    out = nc.dram_tensor("out", [T, DM], F32, kind="ExternalOutput").ap()
    dbg = None
    if stop_after is not None:
        dbg = nc.dram_tensor("dbg", [128, 8192], F32, kind="ExternalOutput").ap()

    def sb(name, shape, dt=F32, stack=None):
        return (stack or es).enter_context(nc.sbuf_tensor(name, list(shape), dt))

    ps = [es.enter_context(nc.psum_tensor("ps%d" % i, [128, 512], F32)) for i in range(8)]
    wslot = [sb("wslot%d" % i, [128, NDC, 512], BF16) for i in range(NSLOT)]
    c_identf = sb("c_identf", [128, 128])
    c_qdiag = sb("c_qdiag", [128, 9, 128], BF16)

    piece_no = [0]

    def load_piece(src_ap):
        i = piece_no[0] % NSLOT
        piece_no[0] += 1
        key = "wslot%d" % i
        dst = wslot[i]

        def fn(eng, sem, src_ap=src_ap, dst=dst):
            if tuple(src_ap.shape) == (128, 8192):
                v = src_ap.rearrange("p (c f) -> p c f", c=NDC)
            else:
                v = src_ap.rearrange("(c p) f -> p c f", p=128)
            for hh in range(2):
                eng.dma_start(out=dst[:, hh * 8:(hh + 1) * 8, :], in_=v[:, hh * 8:(hh + 1) * 8, :]).then_inc(sem, 16)

        S.dma("pool", fn, 2, key, reads=(), writes=(key,))
        return i

    consts = []

    def cload(dst_t, src_ap, key, eng="sp"):
        def fn(engine, sem, dst_t=dst_t, src_ap=src_ap):
            engine.dma_start(out=dst_t[:], in_=src_ap).then_inc(sem, 16)
        S.dma(eng, fn, 1, "c_" + eng, writes=(key,))
        consts.append(key)

    def cfinal():
        for eng in ("sp", "pool"):
            nm = "c_" + eng
            if nm in S.dsem:
                for k in consts:
                    if S.last_w[k][0] is S.dsem[nm]:
                        S.last_w[k] = (S.dsem[nm], S.dcnt[nm])
        consts.clear()

    cload(c_identf, ident_f, "c_identf")
    cload(c_qdiag, qdiag, "c_qdiag")
    c_eps = sb("c_eps", [128, 1])

    def f_eps(pool):
        return pool.memset(c_eps[:, :], LN_EPS)
    S.op("pool", f_eps, writes=("c_eps",))
    c_dummy = sb("c_dummy", [128, 8], BF16)
    cload(c_dummy, kdec, "c_dummy", eng="pool")
    cfinal()
    S.flush()

    def mm(out_ap, pairs, reads, writes):
        def fn(pe, out_ap=out_ap, pairs=pairs):
            n = len(pairs)
            ins = None
            for i, (l, r) in enumerate(pairs):
                ins = pe.matmul(out_ap, l, r, start=(i == 0), stop=(i == n - 1))
            return ins
        S.op("pe", fn, reads=reads, writes=writes)

    def dump(ap_list, keys):
        off = 0
        for ap, n in ap_list:
            def fn(engine, sem, ap=ap, off=off, n=n):
                engine.dma_start(out=dbg[:, off:off + n], in_=ap).then_inc(sem, 16)
            S.dma("sp", fn, 1, "dbg", reads=keys)
            off += n
        S.barrier()
        S.flush()
        raise _Stop()

    arA = sb("arA", [128, 16384])
    arB = sb("arB", [128, 8192])
    xTo = arA[:, 0:8192].bitcast(BF16).rearrange("p (c t) -> p c t", c=NDC)
    xTp = arA[:, 8192:16384].bitcast(BF16).rearrange("p (c t) -> p c t", c=NDC)
    mixL = arB[:, 0:4096].bitcast(BF16).rearrange("p (c t) -> p c t", c=8)
    mixR = arB[:, 4096:8192].bitcast(BF16).rearrange("p (c t) -> p c t", c=8)
    acc = arA[:, :].rearrange("p (c d) -> p c d", c=NTC)
    x1bf = arB[:, :].bitcast(BF16).rearrange("p (c d) -> p c d", c=NTC)
    s12 = ExitStack()
    c_lruvec = sb("c_lruvec", [128, 8, 8], F32, s12)
    c_pf = sb("c_pf", [128, 1], F32, s12)
    state_f = sb("state_f", [128, 8, 128], F32, s12)
    state_b = sb("state_b", [128, 8, 128], BF16, s12)
    cload(c_lruvec, lruvec, "c_lruvec")
    cload(c_pf, pf_in, "c_pf")

    def load_xT(dst, src, key):
        def fn(eng, sem):
            v = src.rearrange("(c p) t -> p c t", p=128)
            for q4 in range(4):
                eng.dma_start(out=dst[:, q4 * 4:(q4 + 1) * 4, :], in_=v[:, q4 * 4:(q4 + 1) * 4, :]).then_inc(sem, 16)
        S.dma("pool", fn, 4, key, writes=(key,))

    with ExitStack() as s1:
        load_xT(xTp, xT_pre, "xTp")
        load_xT(xTo, xT_own, "xTo")
        c_wa = sb("c_wa", [128, 8, 128], BF16, s1)
        c_wx = sb("c_wx", [128, 8, 128], BF16, s1)
        cload(c_wa, lru_wa.rearrange("h i j -> i h j"), "c_wa", eng="pool")
        cload(c_wx, lru_wx.rearrange("h i j -> i h j"), "c_wx", eng="pool")
        cfinal()
        c8 = sb("c8", [128, 8, 2], F32, s1)

        def f_sig(act):
            return act.activation(out=c8[:, :, 0], in_=c_lruvec[:, :, 7], func=AF.Sigmoid)
        S.op("act", f_sig, reads=("c_lruvec",), writes=("c8a",))

        def f_ln(act):
            return act.activation(out=c8[:, :, 1], in_=c8[:, :, 0], func=AF.Ln)
        S.op("act", f_ln, reads=("c8a",), writes=("c8b",))

        def f_c8(dve):
            return dve.tensor_scalar(out=c8[:, :, 0], in0=c8[:, :, 1], scalar1=8.0, scalar2=None, op0=ALU.mult)
        S.op("dve", f_c8, reads=("c8b",), writes=("c8a",))

        def f_c16(dve):
            return dve.tensor_scalar(out=c8[:, :, 1], in0=c8[:, :, 0], scalar1=2.0, scalar2=None, op0=ALU.mult)
        S.op("dve", f_c16, reads=("c8a",), writes=("c8",))

        with ExitStack() as s1a:
            Xs = [sb("lX%d" % i, [128, 3 + 2 * T], F32, s1a) for i in range(2)]
            C = sb("lC", [128, 2 * T], F32, s1a)
            R = arB[:, 4096:6144]
            CB = arB[:, 6144:7168].bitcast(BF16)
            G = sb("lG", [128, 2 * T], F32, s1a)
            hb0 = sb("lh0", [128, 1], F32, s1a)

            for i in range(2):
                def f_zero(pool, i=i):
                    return pool.memset(Xs[i][:, 0:3], 0.0)
                S.op("pool", f_zero, writes=("X%d" % i,))

            for hb in range(8):
                X = Xs[hb % 2]
                XK = "X%d" % (hb % 2)
                cg = 8 + hb // 4
                cc = hb % 4
                if cc == 0:
                    sl_x = load_piece(w_in[:, cg * 512:(cg + 1) * 512])
                    sl_y = load_piece(w_in[:, (cg + 2) * 512:(cg + 3) * 512])
                kx, ky = "wslot%d" % sl_x, "wslot%d" % sl_y
                for tt in range(4):
                    src, skey = (xTp, "xTp") if tt < 2 else (xTo, "xTo")
                    t0 = (tt % 2) * 512
                    pk = "ps%d" % (tt % 2)
                    mm(ps[tt % 2][:, :],
                       [(wslot[sl_x][:, dc, cc * 128:(cc + 1) * 128], src[:, dc, t0:t0 + 512]) for dc in range(NDC)],
                       reads=(kx, skey), writes=(pk,))

                    def f_ev(act, tt=tt, X=X):
                        return act.copy(out=X[:, 3 + tt * 512:3 + (tt + 1) * 512], in_=ps[tt % 2][:, :])
                    S.op("act", f_ev, reads=(pk,), writes=(XK,))
                lv = c_lruvec

                def f_c0(dve, hb=hb, X=X):
                    return dve.tensor_scalar(out=C[:, :], in0=X[:, 0:2 * T], scalar1=lv[:, hb, 0:1], scalar2=lv[:, hb, 4:5],
                                             op0=ALU.mult, op1=ALU.add)
                S.op("dve", f_c0, reads=(XK, "c_lruvec"), writes=("C",))
                for j in range(1, 4):
                    def f_cj(dve, hb=hb, j=j, X=X):
                        return dve.scalar_tensor_tensor(out=C[:, :], in0=X[:, j:j + 2 * T], scalar=lv[:, hb, j:j + 1], in1=C[:, :],
                                                        op0=ALU.mult, op1=ALU.add)
                    S.op("dve", f_cj, reads=(XK, "C"), writes=("C",))

                def f_cb(act):
                    return act.copy(out=CB[:, :], in_=C[:, :])
                S.op("act", f_cb, reads=("C",), writes=("CB",))
                for gi_, (wt, wk, dstb, dk, bcol) in enumerate(((c_wa, "c_wa", R, "R", 5), (c_wx, "c_wx", G, "G", 6))):
                    for tt in range(4):
                        pk = "ps%d" % (2 + (gi_ * 4 + tt) % 4)
                        pst = ps[2 + (gi_ * 4 + tt) % 4]
                        mm(pst[:, :], [(wt[:, hb, :], CB[:, tt * 512:(tt + 1) * 512])], reads=(wk, "CB"), writes=(pk,))

                        def f_sg(act, pst=pst, dstb=dstb, tt=tt, hb=hb, bcol=bcol):
                            return act.activation(out=dstb[:, tt * 512:(tt + 1) * 512], in_=pst[:, :], func=AF.Sigmoid,
                                                  bias=lv[:, hb, bcol:bcol + 1], scale=1.0)
                        S.op("act", f_sg, reads=(pk, "c_lruvec"), writes=(dk,))

                def f_a(act, hb=hb, X=X):
                    return act.activation(out=X[:, 3:3 + 2 * T], in_=R[:, :], func=AF.Exp, scale=c8[:, hb, 0:1])
                S.op("act", f_a, reads=("R", "c8"), writes=(XK,))

                def f_a2(act, hb=hb):
                    return act.activation(out=R[:, :], in_=R[:, :], func=AF.Exp, scale=c8[:, hb, 1:2])
                S.op("act", f_a2, reads=("R", "c8"), writes=("R",))

                def f_om(dve):
                    return dve.tensor_scalar(out=R[:, :], in0=R[:, :], scalar1=1.0, scalar2=-1.0, op0=ALU.min, op1=ALU.mult)
                S.op("dve", f_om, reads=("R",), writes=("R",))

                def f_sq(act):
                    return act.activation(out=R[:, :], in_=R[:, :], func=AF.Sqrt, bias=1.0, scale=1.0)
                S.op("act", f_sq, reads=("R",), writes=("R",))

                def f_g2(pool):
                    return pool.tensor_tensor(out=G[:, :], in0=G[:, :], in1=C[:, :], op=ALU.mult)
                S.op("pool", f_g2, reads=("G", "C"), writes=("G",))

                def f_bx(dve):
                    return dve.tensor_tensor(out=G[:, :], in0=G[:, :], in1=R[:, :], op=ALU.mult)
                S.op("dve", f_bx, reads=("G", "R"), writes=("G",))

                def f_s1(dve, X=X):
                    return dve.tensor_tensor_scan(out=C[:, 0:T], data0=X[:, 3:3 + T], data1=G[:, 0:T], initial=0.0,
                                                  op0=ALU.mult, op1=ALU.add)
                S.op("dve", f_s1, reads=(XK, "G", "C"), writes=("C",))

                def f_h0(dve):
                    return dve.tensor_scalar(out=hb0[:, :], in0=C[:, T - 1:T], scalar1=c_pf[:, 0:1], scalar2=None, op0=ALU.mult)
                S.op("dve", f_h0, reads=("C", "c_pf"), writes=("hb0",))

                def f_s2(dve, X=X):
                    return dve.tensor_tensor_scan(out=C[:, T:2 * T], data0=X[:, 3 + T:3 + 2 * T], data1=G[:, T:2 * T],
                                                  initial=hb0[:, 0:1], op0=ALU.mult, op1=ALU.add)
                S.op("dve", f_s2, reads=(XK, "G", "C", "hb0"), writes=("C",))

                for tt in range(2):
                    pk = "ps%d" % (6 + tt)
                    mm(ps[6 + tt][:, :],
                       [(wslot[sl_y][:, dc, cc * 128:(cc + 1) * 128], xTo[:, dc, tt * 512:(tt + 1) * 512]) for dc in range(NDC)],
                       reads=(ky, "xTo"), writes=(pk,))
                    ysl = slice(tt * 512, (tt + 1) * 512)

                    def f_y(act, tt=tt, ysl=ysl):
                        return act.copy(out=R[:, ysl], in_=ps[6 + tt][:, :])
                    S.op("act", f_y, reads=(pk,), writes=("R",))

                    def f_y2(act, tt=tt, ysl=ysl):
                        return act.activation(out=G[:, ysl], in_=ps[6 + tt][:, :], func=AF.Square)
                    S.op("act", f_y2, reads=(pk,), writes=("G",))
                ysl = slice(0, T)

                def f_y3(dve):
                    return dve.scalar_tensor_tensor(out=G[:, ysl], in0=G[:, ysl], scalar=0.044715, in1=R[:, ysl],
                                                    op0=ALU.mult, op1=ALU.mult)
                S.op("dve", f_y3, reads=("G", "R"), writes=("G",))

                def f_y4(pool):
                    return pool.tensor_tensor(out=G[:, ysl], in0=G[:, ysl], in1=R[:, ysl], op=ALU.add)
                S.op("pool", f_y4, reads=("G", "R"), writes=("G",))

                def f_y5(act):
                    return act.activation(out=G[:, ysl], in_=G[:, ysl], func=AF.Sigmoid, scale=1.5957691216057308)
                S.op("act", f_y5, reads=("G",), writes=("G",))

                def f_y6(pool):
                    return pool.tensor_tensor(out=G[:, ysl], in0=G[:, ysl], in1=R[:, ysl], op=ALU.mult)
                S.op("pool", f_y6, reads=("G", "R"), writes=("G",))

                def f_y7(dve, hb=hb):
                    return dve.tensor_tensor(out=mixL[:, hb, :], in0=G[:, ysl], in1=C[:, T:2 * T], op=ALU.mult)
                S.op("dve", f_y7, reads=("G", "C"), writes=("mixL",))

            if stop_after == "lru":
                def f_d(dve):
                    return dve.tensor_copy(out=R[:, 0:T], in_=mixL[:, 7, :])
                S.op("dve", f_d, reads=("mixL",), writes=("R",))

                def f_d2(dve):
                    return dve.tensor_copy(out=R[:, T:2 * T], in_=mixL[:, 0, :])
                S.op("dve", f_d2, reads=("mixL",), writes=("R",))
                return dump([(R[:, 0:2 * T], 2 * T), (C[:, :], 2 * T)], ("R", "C"))
            S.barrier()
            S.flush()

        _, chunk_dec = _consts()
        with ExitStack() as s1b:
            c_ropep = sb("c_ropep", [128, 2, NTC, 64], F32, s1b)
            c_kdec = sb("c_kdec", [128, 8], F32, s1b)
            cload(c_ropep, rope_pre, "c_ropep")
            cload(c_kdec, kdec, "c_kdec")
            cfinal()
            k_p = arB[:, 4096:8192].bitcast(BF16).rearrange("p (c t) -> p c t", c=NTC)
            vd_p = sb("vd_p", [128, NTC, 1024], BF16, s1b)
            rt = [sb("rt%d" % i, [128, 4, 64], F32, s1b) for i in range(4)]

            def f_z(pool):
                return pool.memset(state_f[:, :, :], 0.0)
            S.op("pool", f_z, writes=tuple("st%d" % h for h in range(8)))

            def f_zb(pool):
                return pool.memset(state_b[:, :, :], 0.0)
            S.op("pool", f_zb, writes=tuple("sb%d" % h for h in range(8)))

            def rope_evac(pst, pk, cos_ap, sin_ap, dst_ap, dkey):
                pv = pst[:, :].rearrange("p (h two d) -> p h two d", h=4, two=2)
                dv = dst_ap.rearrange("p (h two d) -> p h two d", h=4, two=2)
                cb = cos_ap.broadcast_to([128, 4, 64])
                sn = sin_ap.broadcast_to([128, 4, 64])
                x1, x2 = pv[:, :, 0, :], pv[:, :, 1, :]
                for (ta, a, ca) in ((0, x1, cb), (1, x2, sn), (2, x1, sn), (3, x2, cb)):
                    def f(dve, ta=ta, a=a, ca=ca):
                        return dve.tensor_tensor(out=rt[ta][:, :, :], in0=a, in1=ca, op=ALU.mult)
                    S.op("dve", f, reads=(pk, "c_rope"), writes=("rt%d" % ta,))

                def f1(pool):
                    return pool.tensor_tensor(out=dv[:, :, 0, :], in0=rt[0][:, :, :], in1=rt[1][:, :, :], op=ALU.subtract)
                S.op("pool", f1, reads=("rt0", "rt1"), writes=(dkey,))

                def f2(pool):
                    return pool.tensor_tensor(out=dv[:, :, 1, :], in0=rt[2][:, :, :], in1=rt[3][:, :, :], op=ALU.add)
                S.op("pool", f2, reads=("rt2", "rt3"), writes=(dkey,))

            S.last_w["c_rope"] = S.last_w["c_ropep"]
            for half in range(2):
                slk = load_piece(w_in[:, (2 + half) * 512:(3 + half) * 512])
                slv = load_piece(w_in[:, (4 + half) * 512:(5 + half) * 512])
                for tc in range(NTC):
                    pk = "ps%d" % (tc % 2)
                    mm(ps[tc % 2][:, :], [(xTp[:, dc, tc * 128:(tc + 1) * 128], wslot[slk][:, dc, :]) for dc in range(NDC)],
                       reads=("xTp", "wslot%d" % slk), writes=(pk,))
                    rope_evac(ps[tc % 2], pk, c_ropep[:, 0, tc:tc + 1, :], c_ropep[:, 1, tc:tc + 1, :],
                              k_p[:, tc, half * 512:(half + 1) * 512], "k_p")
                for tc in range(NTC):
                    pk = "ps%d" % (2 + tc % 2)
                    pst = ps[2 + tc % 2]
                    mm(pst[:, :], [(xTp[:, dc, tc * 128:(tc + 1) * 128], wslot[slv][:, dc, :]) for dc in range(NDC)],
                       reads=("xTp", "wslot%d" % slv), writes=(pk,))
                    for h4 in range(4):
                        def f(act, pst=pst, tc=tc, h4=h4, half=half):
                            h = half * 4 + h4
                            return act.activation(out=vd_p[:, tc, h * 128:(h + 1) * 128], in_=pst[:, h4 * 128:(h4 + 1) * 128],
                                                  func=AF.Copy, scale=c_kdec[:, h:h + 1])
                        S.op("act", f, reads=(pk, "c_kdec"), writes=("vd_p",))
            for n in range(NTC):
                for h in range(8):
                    pk = "ps%d" % (4 + h % 4)
                    pst = ps[4 + h % 4]
                    mm(pst[:, 0:128], [(k_p[:, n, h * 128:(h + 1) * 128], vd_p[:, n, h * 128:(h + 1) * 128])],
                       reads=("k_p", "vd_p"), writes=(pk,))

                    def f(dve, pst=pst, h=h):
                        return dve.scalar_tensor_tensor(out=state_f[:, h, :], in0=state_f[:, h, :], scalar=float(chunk_dec[h]),
                                                        in1=pst[:, 0:128], op0=ALU.mult, op1=ALU.add)
                    S.op("dve", f, reads=(pk, "st%d" % h), writes=("st%d" % h,))
            for h in range(8):
                def f(act, h=h):
                    return act.copy(out=state_b[:, h, :], in_=state_f[:, h, :])
                S.op("act", f, reads=("st%d" % h,), writes=("sb%d" % h,))
            if stop_after == "pre":
                return dump([(state_f[:, :, :].rearrange("p h e -> p (h e)"), 1024)], tuple("st%d" % h for h in range(8)))
            S.barrier()
            S.flush()

    with ExitStack() as s2:
        c_rope = sb("c_rope", [128, 4, NTC, 64], F32, s2)
        c_kdec = sb("c_kdec2", [128, 8], F32, s2)
        c_decT = sb("c_decT", [128, 8, 128], F32, s2)
        c_gnw = sb("c_gnw", [128, 1024], F32, s2)
        cload(c_rope, rope_own, "c_rope")
        cload(c_kdec, kdec, "c_kdec")
        cload(c_decT, decT, "c_decT")
        cload(c_gnw, rep_gnw, "c_gnw")
        cfinal()
        qkvv = arA[:, 8192:16384].bitcast(BF16).rearrange("p (w c t) -> p w c t", w=4, c=NTC)
        q_o, k_o, v_o, vd_o = qkvv[:, 0], qkvv[:, 1], qkvv[:, 2], qkvv[:, 3]
        sg_o = sb("sg_o", [128, NTC, 512], BF16, s2)
        rt = [sb("rt2_%d" % i, [128, 4, 64], F32, s2) for i in range(4)]
        trb = [sb("trb%d" % i, [128, 384], BF16, s2) for i in range(2)]
        sdt = [sb("sdt%d" % i, [128, 128], BF16, s2) for i in range(2)]
        gst = sb("gst", [128, 4, 6], F32, s2)
        gmv = sb("gmv", [128, 4, 2], F32, s2)
        grs = sb("grs", [128, 4], F32, s2)
        rn = sb("rn", [128, 512], F32, s2)
        mtok = sb("mtok", [128, 512], BF16, s2)

        def rope_evac2(pst, pk, cos_ap, sin_ap, dst_ap, dkey):
            pv = pst[:, :].rearrange("p (h two d) -> p h two d", h=4, two=2)
            dv = dst_ap.rearrange("p (h two d) -> p h two d", h=4, two=2)
            cb = cos_ap.broadcast_to([128, 4, 64])
            sn = sin_ap.broadcast_to([128, 4, 64])
            x1, x2 = pv[:, :, 0, :], pv[:, :, 1, :]
            for (ta, a, ca) in ((0, x1, cb), (1, x2, sn), (2, x1, sn), (3, x2, cb)):
                def f(dve, ta=ta, a=a, ca=ca):
                    return dve.tensor_tensor(out=rt[ta][:, :, :], in0=a, in1=ca, op=ALU.mult)
                S.op("dve", f, reads=(pk, "c_rope"), writes=("rt%d" % ta,))

            def f1(pool):
                return pool.tensor_tensor(out=dv[:, :, 0, :], in0=rt[0][:, :, :], in1=rt[1][:, :, :], op=ALU.subtract)
            S.op("pool", f1, reads=("rt0", "rt1"), writes=(dkey,))

            def f2(pool):
                return pool.tensor_tensor(out=dv[:, :, 1, :], in0=rt[2][:, :, :], in1=rt[3][:, :, :], op=ALU.add)
            S.op("pool", f2, reads=("rt2", "rt3"), writes=(dkey,))

        ident_b = c_qdiag[:, 0, :]
        for hg in range(2):
            slq = load_piece(w_in[:, (0 + hg) * 512:(1 + hg) * 512])
            slk = load_piece(w_in[:, (2 + hg) * 512:(3 + hg) * 512])
            slv = load_piece(w_in[:, (4 + hg) * 512:(5 + hg) * 512])
            slg = load_piece(w_in[:, (6 + hg) * 512:(7 + hg) * 512])
            cnt = 0
            for which, sl in (("q", slq), ("k", slk), ("v", slv), ("g", slg)):
                for tc in range(NTC):
                    pb = cnt % 2
                    cnt += 1
                    pk = "ps%d" % pb
                    pst = ps[pb]
                    mm(pst[:, :], [(xTo[:, dc, tc * 128:(tc + 1) * 128], wslot[sl][:, dc, :]) for dc in range(NDC)],
                       reads=("xTo", "wslot%d" % sl), writes=(pk,))
                    if which == "q":
                        rope_evac2(pst, pk, c_rope[:, 0, tc:tc + 1, :], c_rope[:, 1, tc:tc + 1, :], q_o[:, tc, :], "q_o")
                    elif which == "k":
                        rope_evac2(pst, pk, c_rope[:, 2, tc:tc + 1, :], c_rope[:, 3, tc:tc + 1, :], k_o[:, tc, :], "k_o")
                    elif which == "v":
                        def f(act, pst=pst, tc=tc):
                            return act.copy(out=v_o[:, tc, :], in_=pst[:, :])
                        S.op("act", f, reads=(pk,), writes=("v_o",))
                        for h4 in range(4):
                            def f(act, pst=pst, tc=tc, h4=h4, hg=hg):
                                h = hg * 4 + h4
                                return act.activation(out=vd_o[:, tc, h4 * 128:(h4 + 1) * 128], in_=pst[:, h4 * 128:(h4 + 1) * 128],
                                                      func=AF.Copy, scale=c_kdec[:, h:h + 1])
                            S.op("act", f, reads=(pk, "c_kdec"), writes=("vd_o",))
                    else:
                        def f(act, pst=pst, tc=tc):
                            return act.activation(out=sg_o[:, tc, :], in_=pst[:, :], func=AF.Silu)
                        S.op("act", f, reads=(pk,), writes=("sg_o",))
            for n in range(NTC):
                rb = 6 + n % 2
                rk = "ps%d" % rb
                for h4 in range(4):
                    h = hg * 4 + h4
                    hs = slice(h4 * 128, (h4 + 1) * 128)
                    tb = 2 + h4 % 2
                    tk = "ps%d" % tb
                    trs = trb[h4 % 2]
                    trk = "trb%d" % (h4 % 2)

                    def f_tr(pe, tb=tb, n=n, hs=hs, h=h):
                        pe.matmul(ps[tb][:, 0:128], q_o[:, n, hs], ident_b, start=True, stop=True)
                        pe.matmul(ps[tb][:, 128:256], q_o[:, n, hs], c_qdiag[:, 1 + h, :], start=True, stop=True)
                        return pe.matmul(ps[tb][:, 256:384], k_o[:, n, hs], ident_b, start=True, stop=True)
                    S.op("pe", f_tr, reads=("q_o", "k_o", "c_qdiag"), writes=(tk,))

                    def f_te(act, tb=tb, trs=trs):
                        return act.copy(out=trs[:, :], in_=ps[tb][:, 0:384])
                    S.op("act", f_te, reads=(tk,), writes=(trk,))
                    sb_ = 4 + h4 % 2
                    sk = "ps%d" % sb_
                    mm(ps[sb_][:, 0:128], [(trs[:, 256:384], trs[:, 0:128])], reads=(trk,), writes=(sk,))
                    sd = sdt[h4 % 2]
                    sdk = "sdt%d" % (h4 % 2)

                    def f_sd(dve, sb_=sb_, sd=sd, h=h):
                        return dve.tensor_tensor(out=sd[:, :], in0=ps[sb_][:, 0:128], in1=c_decT[:, h, :], op=ALU.mult)
                    S.op("dve", f_sd, reads=(sk, "c_decT"), writes=(sdk,))
                    mm(ps[rb][:, hs], [(sd[:, :], v_o[:, n, hs]), (trs[:, 128:256], state_b[:, h, :])],
                       reads=(sdk, "v_o", trk, "sb%d" % h), writes=(rk,))
                    mm(ps[tb][:, 384:512], [(k_o[:, n, hs], vd_o[:, n, hs])], reads=("k_o", "vd_o"), writes=(tk,))

                    def f_su(dve, tb=tb, h=h):
                        return dve.scalar_tensor_tensor(out=state_f[:, h, :], in0=state_f[:, h, :], scalar=float(chunk_dec[h]),
                                                        in1=ps[tb][:, 384:512], op0=ALU.mult, op1=ALU.add)
                    S.op("dve", f_su, reads=(tk, "st%d" % h), writes=("st%d" % h,))

                    def f_sbc(act, h=h):
                        return act.copy(out=state_b[:, h, :], in_=state_f[:, h, :])
                    S.op("act", f_sbc, reads=("st%d" % h,), writes=("sb%d" % h,))
                for h4 in range(4):
                    def f_bs(dve, h4=h4, rb=rb):
                        return dve.bn_stats(out=gst[:, h4, :], in_=ps[rb][:, h4 * 128:(h4 + 1) * 128])
                    S.op("dve", f_bs, reads=(rk,), writes=("gst",))
                for h4 in range(4):
                    def f_ba(dve, h4=h4):
                        return dve.bn_aggr(out=gmv[:, h4, :], in_=gst[:, h4, :])
                    S.op("dve", f_ba, reads=("gst",), writes=("gmv",))

                def f_sd2(act):
                    return act.activation(out=grs[:, :], in_=gmv[:, :, 1], func=AF.Sqrt, bias=c_eps[:, 0:1], scale=1.0)
                S.op("act", f_sd2, reads=("gmv", "c_eps"), writes=("grs",))

                def f_rc(dve):
                    return dve.reciprocal(out=grs[:, :], in_=grs[:, :])
                S.op("dve", f_rc, reads=("grs",), writes=("grs",))
                for h4 in range(4):
                    def f_nm(dve, h4=h4, rb=rb):
                        return dve.tensor_scalar(out=rn[:, h4 * 128:(h4 + 1) * 128], in0=ps[rb][:, h4 * 128:(h4 + 1) * 128],
                                                 scalar1=gmv[:, h4, 0:1], scalar2=grs[:, h4:h4 + 1], op0=ALU.subtract, op1=ALU.mult)
                    S.op("dve", f_nm, reads=(rk, "gmv", "grs"), writes=("rn",))

                def f_gw(pool, hg=hg):
                    return pool.tensor_tensor(out=rn[:, :], in0=rn[:, :], in1=c_gnw[:, hg * 512:(hg + 1) * 512], op=ALU.mult)
                S.op("pool", f_gw, reads=("rn", "c_gnw"), writes=("rn",))

                def f_sgm(pool, n=n):
                    return pool.tensor_tensor(out=mtok[:, :], in0=rn[:, :], in1=sg_o[:, n, :], op=ALU.mult)
                S.op("pool", f_sgm, reads=("rn", "sg_o"), writes=("mtok",))
                mb = n % 2
                mk = "ps%d" % mb

                def f_mt(pe, mb=mb):
                    ins = None
                    for h4 in range(4):
                        ins = pe.matmul(ps[mb][:, h4 * 128:(h4 + 1) * 128], mtok[:, h4 * 128:(h4 + 1) * 128], ident_b, start=True, stop=True)
                    return ins
                S.op("pe", f_mt, reads=("mtok", "c_qdiag"), writes=(mk,))

                def f_me(act, mb=mb, hg=hg, n=n):
                    return act.copy(out=mixR[:, hg * 4:hg * 4 + 4, n * 128:(n + 1) * 128],
                                    in_=ps[mb][:, :].rearrange("p (h i) -> p h i", h=4))
                S.op("act", f_me, reads=(mk,), writes=("mixR",))
        if stop_after == "ret":
            dbgbuf = sb("dbgbuf", [128, T], F32, s2)

            def f_d(dve):
                return dve.tensor_copy(out=dbgbuf[:, 0:512], in_=mixR[:, 0, 0:512])
            S.op("dve", f_d, reads=("mixR",), writes=("dbgbuf",))

            def f_d2(dve):
                return dve.tensor_copy(out=dbgbuf[:, 512:T], in_=mixR[:, 5, 512:T])
            S.op("dve", f_d2, reads=("mixR",), writes=("dbgbuf",))
            return dump([(dbgbuf[:, 0:T], T)], ("dbgbuf",))
        S.barrier()
        S.flush()

    s12.close()

    srcs = [w_out[:, dg * 512:(dg + 1) * 512] for dg in range(4)]
    for e in range(n_exp):
        for fg in range(4):
            ee = e % NEW
            srcs.append(w_gate[ee // EG][ee % EG, fg])
            srcs.append(w_up[ee // EG][ee % EG, fg])
        for dg in range(4):
            srcs.append(w_down[ee // EG][ee % EG, dg])
    for dg in range(4):
        srcs.append(w_pg[:, dg * 512:(dg + 1) * 512])
    stream = {"issued": 0, "slots": {}}

    def get_pieces(k, n=1):
        while stream["issued"] < min(k + NSLOT, len(srcs)):
            i = stream["issued"]
            stream["slots"][i] = load_piece(srcs[i])
            stream["issued"] += 1
        return [stream["slots"][k + i] for i in range(n)]

    def get_piece(k):
        return get_pieces(k, 1)[0]

    def layer_norm(src_ap, skey, w_t, b_t, wkeys, dst_ap, dkey, lst, lmv, lrs, tag):
        for k4 in range(4):
            def f(dve, k4=k4):
                return dve.bn_stats(out=lst[:, k4, :], in_=src_ap[:, k4 * 512:(k4 + 1) * 512])
            S.op("dve", f, reads=(skey,), writes=("lst" + tag,))

        def f(dve):
            return dve.bn_aggr(out=lmv[:, :], in_=lst[:, :, :].rearrange("p a b -> p (a b)"))
        S.op("dve", f, reads=("lst" + tag,), writes=("lmv" + tag,))

        def f(act):
            return act.activation(out=lrs[:, :], in_=lmv[:, 1:2], func=AF.Sqrt, bias=c_eps[:, 0:1], scale=1.0)
        S.op("act", f, reads=("lmv" + tag, "c_eps"), writes=("lrs" + tag,))

        def f(dve):
            return dve.reciprocal(out=lrs[:, :], in_=lrs[:, :])
        S.op("dve", f, reads=("lrs" + tag,), writes=("lrs" + tag,))

        def f(dve):
            return dve.tensor_scalar(out=dst_ap, in0=src_ap, scalar1=lmv[:, 0:1], scalar2=lrs[:, 0:1],
                                     op0=ALU.subtract, op1=ALU.mult)
        S.op("dve", f, reads=(skey, "lmv" + tag, "lrs" + tag), writes=(dkey,))

        def f(pool):
            return pool.tensor_tensor(out=dst_ap, in0=dst_ap, in1=w_t[:, :], op=ALU.mult)
        S.op("pool", f, reads=(dkey,) + wkeys, writes=(dkey,))

        def f(pool):
            return pool.tensor_tensor(out=dst_ap, in0=dst_ap, in1=b_t[:, :], op=ALU.add)
        S.op("pool", f, reads=(dkey,) + wkeys, writes=(dkey,))

    gates = sb("gates", [128, NTC, NE])
    posm = sb("posm", [128, NTC, NE])
    posmT = sb("posmT", [128, T], BF16)
    c_bgu = sb("c_bgu", [128, 2, NE, 16])
    c_iotac = sb("c_iotac", [128, CAP])
    c_iotap = sb("c_iotap", [128, 2])
    c_pidx = sb("c_pidx", [128, 128])
    cload(c_bgu, bgu, "c_bgu")
    cload(c_iotac, iota_c, "c_iotac")
    cload(c_iotap, iota_p, "c_iotap")
    cload(c_pidx, pidx_in, "c_pidx")
    cfinal()

    def f(pool):
        return pool.memset(posmT[:, :], 0.0)
    S.op("pool", f, writes=("posmT",))

    with ExitStack() as s3:
        xcb = [sb("xcb%d" % i, [128, 512], F32, s3) for i in range(2)]
        cnt = 0
        for dg in range(4):
            sl = get_piece(dg)
            for tc in range(NTC):
                pb = cnt % 2
                xb = xcb[cnt % 2]
                xk = "xcb%d" % (cnt % 2)
                cnt += 1

                def f(eng, sem, xb=xb, tc=tc, dg=dg):
                    eng.dma_start(out=xb[:, :], in_=x_own[tc * 128:(tc + 1) * 128, dg * 512:(dg + 1) * 512]).then_inc(sem, 16)
                S.dma("sp", f, 1, xk, writes=(xk,))
                pk = "ps%d" % pb
                mm(ps[pb][:, :],
                   [((mixR if fc < 8 else mixL)[:, fc % 8, tc * 128:(tc + 1) * 128], wslot[sl][:, fc, :]) for fc in range(NDC)],
                   reads=("mixR", "mixL", "wslot%d" % sl), writes=(pk,))

                def f(dve, xb=xb, pb=pb, tc=tc, dg=dg):
                    return dve.scalar_tensor_tensor(out=acc[:, tc, dg * 512:(dg + 1) * 512], in0=xb[:, :], scalar=DN_ALPHA,
                                                    in1=ps[pb][:, :], op0=ALU.mult, op1=ALU.add)
                S.op("dve", f, reads=(xk, pk), writes=("acc%d" % tc,))
        S.barrier()
        S.flush()

    with ExitStack() as s3b:
        c_lw = sb("c_lw", [128, DM], F32, s3b)
        c_lb = sb("c_lb", [128, DM], F32, s3b)
        c_wr = sb("c_wr", [128, NDC, NE], F32, s3b)
        c_br = sb("c_br", [128, NE], F32, s3b)
        c_tri = sb("c_tri", [128, 2, 128], BF16, s3b)
        cload(c_lw, rep_ln1w, "c_lw")
        cload(c_lb, rep_ln1b, "c_lb")
        cload(c_wr, w_r.rearrange("(c p) e -> p c e", p=128), "c_wr")
        cload(c_br, rep_br, "c_br")
        cload(c_tri, tri, "c_tri")
        cfinal()
        lst = sb("lst", [128, 4, 6], F32, s3b)
        lmv = sb("lmv", [128, 2], F32, s3b)
        lrs = sb("lrs", [128, 1], F32, s3b)
        x1Tq = [sb("x1Tq%d" % i, [128, 2, 4, 128], BF16, s3b) for i in range(2)]
        xlo = sb("xlo", [128, DM], BF16, s3b)
        wr_hl = sb("wr_hl", [128, 2, NDC, NE], BF16, s3b)
        logit = sb("logit", [128, NTC, NE], F32, s3b)
        top8 = sb("top8", [128, NTC, 8], F32, s3b)
        nmx = sb("nmx", [128, NTC], F32, s3b)
        mask = sb("mask", [128, NTC, NE], F32, s3b)
        maskb = sb("maskb", [128, NTC, NE], BF16, s3b)
        posmb = sb("posmb", [128, NTC, NE], BF16, s3b)
        den = sb("den", [128, NTC], F32, s3b)
        def f(act):
            return act.copy(out=wr_hl[:, 0, :, :], in_=c_wr[:, :, :])
        S.op("act", f, reads=("c_wr",), writes=("wr_hi",))

        def f(dve):
            return dve.tensor_tensor(out=wr_hl[:, 1, :, :], in0=c_wr[:, :, :], in1=wr_hl[:, 0, :, :], op=ALU.subtract)
        S.op("dve", f, reads=("c_wr", "wr_hi"), writes=("wr_lo",))
        for tc in range(NTC):
            ak = "acc%d" % tc
            x1f = acc[:, tc, :]
            layer_norm(x1f, ak, c_lw, c_lb, ("c_lw", "c_lb"), x1f, ak, lst, lmv, lrs, "1")

            def f(act, tc=tc, x1f=x1f):
                return act.copy(out=x1bf[:, tc, :], in_=x1f)
            S.op("act", f, reads=(ak,), writes=("x1bf",))
            def f(dve, tc=tc, x1f=x1f):
                return dve.tensor_tensor(out=xlo[:, :], in0=x1f, in1=x1bf[:, tc, :], op=ALU.subtract)
            S.op("dve", f, reads=(ak, "x1bf"), writes=("xlo",))
            for q4 in range(4):
                xq = x1Tq[q4 % 2]
                xqk = "x1Tq%d" % (q4 % 2)
                for hl in range(2):
                    pb = hl
                    pk = "ps%d" % pb

                    def f(pe, q4=q4, pb=pb, hl=hl, tc=tc):
                        ins = None
                        for i in range(4):
                            dc = q4 * 4 + i
                            src = x1bf[:, tc, dc * 128:(dc + 1) * 128] if hl == 0 else xlo[:, dc * 128:(dc + 1) * 128]
                            ins = pe.matmul(ps[pb][:, i * 128:(i + 1) * 128], src, c_qdiag[:, 0, :], start=True, stop=True)
                        return ins
                    S.op("pe", f, reads=("x1bf", "xlo", "c_qdiag"), writes=(pk,))

                    def f(act, xq=xq, pb=pb, hl=hl):
                        return act.copy(out=xq[:, hl, :, :], in_=ps[pb][:, :].rearrange("p (a t) -> p a t", a=4))
                    S.op("act", f, reads=(pk,), writes=(xqk,))

                def f(pe, q4=q4, xq=xq):
                    ins = None
                    for i in range(4):
                        dc = q4 * 4 + i
                        for j, (xh, wh) in enumerate(((0, 0), (0, 1), (1, 0))):
                            ins = pe.matmul(ps[2][:, 0:NE], xq[:, xh, i, :], wr_hl[:, wh, dc, :],
                                            start=(dc == 0 and j == 0), stop=(dc == NDC - 1 and j == 2))
                    return ins
                S.op("pe", f, reads=(xqk, "wr_hi", "wr_lo"), writes=("ps2",))

            def f(act, tc=tc, x1f=x1f):
                return act.mul(out=x1f, in_=x1f, mul=DN_ALPHA)
            S.op("act", f, reads=(ak,), writes=(ak,))

            def f(dve, tc=tc):
                return dve.tensor_tensor(out=logit[:, tc, :], in0=ps[2][:, 0:NE], in1=c_br[:, :], op=ALU.add)
            S.op("dve", f, reads=("ps2", "c_br"), writes=("logit",))
        if stop_after == "ln1":
            return dump([(acc[:, 0, :], DM), (acc[:, 7, :], DM), (logit[:, :, :].rearrange("p a b -> p (a b)"), NTC * NE)],
                        ("acc0", "acc7", "logit"))
        for tc in range(NTC):
            def f(dve, tc=tc):
                return dve.max(out=top8[:, tc, :], in_=logit[:, tc, :])
            S.op("dve", f, reads=("logit",), writes=("top8",))
        for tc in range(NTC):
            def f(dve, tc=tc):
                return dve.tensor_scalar(out=mask[:, tc, :], in0=logit[:, tc, :], scalar1=top8[:, tc, 3:4], scalar2=None, op0=ALU.is_ge)
            S.op("dve", f, reads=("logit", "top8"), writes=("mask",))

        def f(dve):
            return dve.tensor_scalar(out=nmx[:, :], in0=top8[:, :, 0], scalar1=-1.0, scalar2=None, op0=ALU.mult)
        S.op("dve", f, reads=("top8",), writes=("nmx",))
        for tc in range(NTC):
            def f(act, tc=tc):
                return act.activation(out=gates[:, tc, :], in_=logit[:, tc, :], func=AF.Exp, bias=nmx[:, tc:tc + 1], scale=1.0)
            S.op("act", f, reads=("logit", "nmx"), writes=("gates",))

        def f(dve):
            return dve.tensor_tensor(out=gates[:, :, :], in0=gates[:, :, :], in1=mask[:, :, :], op=ALU.mult)
        S.op("dve", f, reads=("gates", "mask"), writes=("gates",))

        def f(dve):
            return dve.tensor_reduce(out=den[:, :], in_=gates[:, :, :], axis=mybir.AxisListType.X, op=ALU.add)
        S.op("dve", f, reads=("gates",), writes=("den",))

        def f(dve):
            return dve.reciprocal(out=den[:, :], in_=den[:, :])
        S.op("dve", f, reads=("den",), writes=("den",))
        for tc in range(NTC):
            def f(dve, tc=tc):
                return dve.tensor_scalar(out=gates[:, tc, :], in0=gates[:, tc, :], scalar1=den[:, tc:tc + 1], scalar2=None, op0=ALU.mult)
            S.op("dve", f, reads=("gates", "den"), writes=("gates",))

        def f(act):
            return act.copy(out=maskb[:, :, :], in_=mask[:, :, :])
        S.op("act", f, reads=("mask",), writes=("maskb",))
        for tc in range(NTC):
            pb = 4 + tc % 2
            pk = "ps%d" % pb
            pairs = [(c_tri[:, 0, :], maskb[:, t2, :]) for t2 in range(tc)] + [(c_tri[:, 1, :], maskb[:, tc, :])]
            mm(ps[pb][:, 0:NE], pairs, reads=("maskb", "c_tri"), writes=(pk,))

            def f(dve, tc=tc, pb=pb):
                return dve.scalar_tensor_tensor(out=posm[:, tc, :], in0=ps[pb][:, 0:NE], scalar=1.0, in1=mask[:, tc, :],
                                                op0=ALU.add, op1=ALU.mult)
            S.op("dve", f, reads=(pk, "mask"), writes=("posm",))

        def f(dve):
            return dve.tensor_scalar(out=posm[:, :, :], in0=posm[:, :, :], scalar1=-1.0, scalar2=256.0, op0=ALU.add, op1=ALU.min)
        S.op("dve", f, reads=("posm",), writes=("posm",))

        def f(act):
            return act.copy(out=posmb[:, :, :], in_=posm[:, :, :])
        S.op("act", f, reads=("posm",), writes=("posmb",))
        for half in range(2):
            pb = 6 + half
            pk = "ps%d" % pb

            def f(pe, half=half, pb=pb):
                ins = None
                for i in range(4):
                    tc = half * 4 + i
                    ins = pe.matmul(ps[pb][0:NE, i * 128:(i + 1) * 128], posmb[:, tc, :], c_qdiag[:, 0, :], start=True, stop=True)
                return ins
            S.op("pe", f, reads=("posmb", "c_qdiag"), writes=(pk,))

            def f(act, half=half, pb=pb):
                return act.copy(out=posmT[0:NE, half * 512:(half + 1) * 512], in_=ps[pb][0:NE, :])
            S.op("act", f, reads=(pk,), writes=("posmT",))
        if stop_after == "route":
            return dump([(gates[:, :, :].rearrange("p a b -> p (a b)"), NTC * NE), (posm[:, :, :].rearrange("p a b -> p (a b)"), NTC * NE)],
                        ("gates", "posm"))
        S.barrier()
        S.flush()

    with ExitStack() as s4:
        sel_e = [sb("sel_e%d" % i, [128, 128], BF16, s4) for i in range(2)]
        Sg = sb("Sg", [128, NTC, CAP], BF16, s4)
        ST = sb("ST", [128, 2, T], BF16, s4)
        XeT = sb("XeT", [128, NDC, CAP], BF16, s4)
        HT = sb("HT", [128, NDC, 256], BF16, s4)

        def f(pool):
            return pool.memset(HT[:, :, :], 0.0)
        S.op("pool", f, writes=("HT",))
        Yb = [sb("Yb%d" % i, [128, 2, 512], BF16, s4) for i in range(2)]
        tA = [sb("tA%d" % i, [128, CAP], F32, s4) for i in range(2)]
        tB = [sb("tB0", [128, CAP], F32, s4)] * 2
        tC = [sb("tC%d" % i, [128, CAP], F32, s4) for i in range(2)]
        tD = [sb("tD0", [128, CAP], F32, s4)] * 2

        def scatter(e, dg):
            yb = Yb[dg % 2]
            yk = "Yb%d" % (dg % 2)
            for tc in range(NTC):
                pb = 6 + tc % 2
                pk = "ps%d" % pb
                mm(ps[pb][:, :], [(ST[0:rows, jc, tc * 128:(tc + 1) * 128], yb[0:rows, jc, :]) for jc, (_, rows) in enumerate(JB)],
                   reads=("ST", yk), writes=(pk,))

                def f(dve, pb=pb, tc=tc, e=e, dg=dg):
                    return dve.scalar_tensor_tensor(out=acc[:, tc, dg * 512:(dg + 1) * 512], in0=ps[pb][:, :],
                                                    scalar=gates[:, tc, e:e + 1], in1=acc[:, tc, dg * 512:(dg + 1) * 512],
                                                    op0=ALU.mult, op1=ALU.add)
                S.op("dve", f, reads=(pk, "gates", "acc%d" % tc), writes=("acc%d" % tc,))

        for e in range(n_exp):
            base = 4 + e * 12
            for tc in range(NTC):
                def f(pool, tc=tc, e=e):
                    return pool.tensor_scalar(out=Sg[:, tc, :], in0=c_iotac[:, :], scalar1=posm[:, tc, e:e + 1], scalar2=None, op0=ALU.is_equal)
                S.op("pool", f, reads=("c_iotac", "posm"), writes=("Sg",))
            se = sel_e[e % 2]
            sek = "sel_e%d" % (e % 2)

            def f(pool, se=se, e=e):
                return pool.tensor_scalar(out=se[:, :], in0=c_pidx[:, :], scalar1=float(e), scalar2=None, op0=ALU.is_equal)
            S.op("pool", f, reads=("c_pidx",), writes=(sek,))
            for half in range(2):
                pb = half
                pk = "ps%d" % pb
                mm(ps[pb][:, :], [(se[:, :], posmT[:, half * 512:(half + 1) * 512])], reads=(sek, "posmT"), writes=(pk,))
                for jc in range(2):
                    def f(dve, pb=pb, jc=jc, half=half):
                        return dve.tensor_scalar(out=ST[:, jc, half * 512:(half + 1) * 512], in0=ps[pb][:, :],
                                                 scalar1=c_iotap[:, jc:jc + 1], scalar2=None, op0=ALU.is_equal)
                    S.op("dve", f, reads=(pk, "c_iotap"), writes=("ST",))
            for dp in range(NDC // 2):
                pb = dp % 2
                pk = "ps%d" % pb

                def f(pe, dp=dp, pb=pb):
                    ins = None
                    for i in range(2):
                        dc = dp * 2 + i
                        for tc in range(NTC):
                            ins = pe.matmul(ps[pb][:, i * CAP:(i + 1) * CAP], x1bf[:, tc, dc * 128:(dc + 1) * 128], Sg[:, tc, :],
                                            start=(tc == 0), stop=(tc == NTC - 1))
                    return ins
                S.op("pe", f, reads=("x1bf", "Sg"), writes=(pk,))

                def f(act, dp=dp, pb=pb):
                    return act.copy(out=XeT[:, dp * 2:dp * 2 + 2, :], in_=ps[pb][:, 0:2 * CAP].rearrange("p (a j) -> p a j", a=2))
                S.op("act", f, reads=(pk,), writes=("XeT",))
            for fg in range(4):
                slg, slu = get_pieces(base + fg * 2, 2)
                for fcl in range(4):
                    fc = fg * 4 + fcl
                    par = fc % 2
                    pb = 2 + par
                    pk = "ps%d" % pb

                    def f(pe, pb=pb, slg=slg, slu=slu, fcl=fcl):
                        ins = None
                        for dc in range(NDC):
                            ins = pe.matmul(ps[pb][:, 0:CAP], wslot[slg][:, dc, fcl * 128:(fcl + 1) * 128], XeT[:, dc, :],
                                            start=(dc == 0), stop=(dc == NDC - 1))
                        for dc in range(NDC):
                            ins = pe.matmul(ps[pb][:, CAP:2 * CAP], wslot[slu][:, dc, fcl * 128:(fcl + 1) * 128], XeT[:, dc, :],
                                            start=(dc == 0), stop=(dc == NDC - 1))
                        return ins
                    S.op("pe", f, reads=("XeT", "wslot%d" % slg, "wslot%d" % slu), writes=(pk,))
                    a_, b_, c_, d_ = tA[par], tB[par], tC[par], tD[par]
                    ka, kb, kc, kd = "tA%d" % par, "tB0", "tC%d" % par, "tD0"

                    def f(dve, pb=pb, a_=a_, e=e, fc=fc):
                        return dve.tensor_scalar(out=a_[:, :], in0=ps[pb][:, 0:CAP], scalar1=c_bgu[:, 0, e, fc:fc + 1], scalar2=7.0,
                                                 op0=ALU.add, op1=ALU.min)
                    S.op("dve", f, reads=(pk, "c_bgu"), writes=(ka,))

                    def f(act, a_=a_, b_=b_):
                        return act.activation(out=b_[:, :], in_=a_[:, :], func=AF.Sigmoid, scale=1.702)
                    S.op("act", f, reads=(ka,), writes=(kb,))

                    def f(dve, pb=pb, c_=c_, e=e, fc=fc):
                        return dve.tensor_scalar(out=c_[:, :], in0=ps[pb][:, CAP:2 * CAP], scalar1=c_bgu[:, 1, e, fc:fc + 1], scalar2=7.0,
                                                 op0=ALU.add, op1=ALU.min)
                    S.op("dve", f, reads=(pk, "c_bgu"), writes=(kc,))

                    def f(dve, c_=c_):
                        return dve.tensor_scalar(out=c_[:, :], in0=c_[:, :], scalar1=-7.0, scalar2=1.0, op0=ALU.max, op1=ALU.add)
                    S.op("dve", f, reads=(kc,), writes=(kc,))

                    def f(pool, a_=a_, b_=b_, d_=d_):
                        return pool.tensor_tensor(out=d_[:, :], in0=a_[:, :], in1=b_[:, :], op=ALU.mult)
                    S.op("pool", f, reads=(ka, kb), writes=(kd,))

                    def f(pool, c_=c_, d_=d_, fc=fc):
                        return pool.tensor_tensor(out=HT[:, fc, 0:CAP], in0=d_[:, :], in1=c_[:, :], op=ALU.mult)
                    S.op("pool", f, reads=(kc, kd), writes=("HT",))
            for dg in range(4):
                sld = get_piece(base + 8 + dg)
                yb = Yb[dg % 2]
                yk = "Yb%d" % (dg % 2)
                for jc, (j0, rows) in enumerate(JB):
                    pb = 4 + jc
                    pk = "ps%d" % pb
                    mm(ps[pb][0:rows, :], [(HT[:, fc, j0:j0 + rows], wslot[sld][:, fc, :]) for fc in range(NDC)],
                       reads=("HT", "wslot%d" % sld), writes=(pk,))

                    def f(act, pb=pb, yb=yb, jc=jc, rows=rows):
                        return act.copy(out=yb[0:rows, jc, :], in_=ps[pb][0:rows, :])
                    S.op("act", f, reads=(pk,), writes=(yk,))
                if dg >= 1:
                    scatter(e, dg - 1)
            scatter(e, 3)
        S.barrier()
        S.flush()
    with ExitStack() as s4:
        gatesT = sb("gatesT", [NE, 2, T], BF16, s4)
        g_hl = sb("g_hl", [128, 2, NTC, NE], BF16, s4)
        c_bdn = sb("c_bdn", [NE, DM], F32, s4)
        b_hl = sb("b_hl", [NE, 2, DM], BF16, s4)
        cload(c_bdn, b_down, "c_bdn")
        cfinal()

        def f(act):
            return act.copy(out=g_hl[:, 0, :, :], in_=gates[:, :, :])
        S.op("act", f, reads=("gates",), writes=("g_hi",))

        def f(dve):
            return dve.tensor_tensor(out=g_hl[:, 1, :, :], in0=gates[:, :, :], in1=g_hl[:, 0, :, :], op=ALU.subtract)
        S.op("dve", f, reads=("gates", "g_hi"), writes=("g_lo",))

        def f(act):
            return act.copy(out=b_hl[:, 0, :], in_=c_bdn[:, :])
        S.op("act", f, reads=("c_bdn",), writes=("b_hi",))

        def f(dve):
            return dve.tensor_tensor(out=b_hl[:, 1, :], in0=c_bdn[:, :], in1=b_hl[:, 0, :], op=ALU.subtract)
        S.op("dve", f, reads=("c_bdn", "b_hi"), writes=("b_lo",))
        for hl in range(2):
            for half in range(2):
                pb = 2 + half
                pk = "ps%d" % pb

                def f(pe, half=half, pb=pb, hl=hl):
                    ins = None
                    for i in range(4):
                        tc = half * 4 + i
                        ins = pe.matmul(ps[pb][0:NE, i * 128:(i + 1) * 128], g_hl[:, hl, tc, :], c_qdiag[:, 0, :], start=True, stop=True)
                    return ins
                S.op("pe", f, reads=("g_hi", "g_lo", "c_qdiag"), writes=(pk,))

                def f(act, half=half, pb=pb, hl=hl):
                    return act.copy(out=gatesT[:, hl, half * 512:(half + 1) * 512], in_=ps[pb][0:NE, :])
                S.op("act", f, reads=(pk,), writes=("gatesT",))
        for tc in range(NTC):
            for dg in range(4):
                pb = (tc * 4 + dg) % 2
                pk = "ps%d" % pb
                mm(ps[pb][:, :], [(gatesT[:, gh, tc * 128:(tc + 1) * 128], b_hl[:, bh, dg * 512:(dg + 1) * 512])
                                  for gh, bh in ((0, 0), (0, 1), (1, 0))],
                   reads=("gatesT", "b_hi", "b_lo"), writes=(pk,))

                def f(dve, pb=pb, tc=tc, dg=dg):
                    return dve.tensor_tensor(out=acc[:, tc, dg * 512:(dg + 1) * 512], in0=ps[pb][:, :],
                                             in1=acc[:, tc, dg * 512:(dg + 1) * 512], op=ALU.add)
                S.op("dve", f, reads=(pk, "acc%d" % tc), writes=("acc%d" % tc,))
        if stop_after == "moe":
            return dump([(acc[:, 0, :], DM), (acc[:, 7, :], DM)], ("acc0", "acc7"))
        S.barrier()
        S.flush()

    with ExitStack() as s5:
        c_lw = sb("c_lw2", [128, DM], F32, s5)
        c_lb = sb("c_lb2", [128, DM], F32, s5)
        c_pw = arB[:, 0:2048]
        c_pp = arB[:, 2048:4096].bitcast(BF16).rearrange("p (c d) -> p c d", c=2)
        c_pT = sb("c_pT", [128, 2, T], BF16, s5)
        cload(c_lw, rep_ln2w, "c_lw2")
        cload(c_lb, rep_ln2b, "c_lb2")
        cload(c_pw, rep_plew, "c_pw")
        cload(c_pp, w_pp.rearrange("(c p) d -> p c d", p=128), "c_pp", eng="pool")
        cload(c_pT, pT.rearrange("(c p) t -> p c t", p=128), "c_pT", eng="pool")
        cfinal()
        lst = sb("lst2", [128, 4, 6], F32, s5)
        lmv = sb("lmv2", [128, 2], F32, s5)
        lrs = sb("lrs2", [128, 1], F32, s5)
        x2b = sb("x2b", [128, DM], BF16, s5)
        x2T = sb("x2T", [128, NDC, 128], BF16, s5)
        ebuf = arB[:, 4096:6144]
        esq = sb("esq", [128, 512], F32, s5)
        ess = sb("ess", [128, 4], F32, s5)
        ers = sb("ers", [128, 1], F32, s5)
        gbuf = arB[:, 6144:8192]
        pslots = get_pieces(len(srcs) - 4, 4)

        if stop_after == "s5pre":
            return dump([(acc[:, 0, :], DM), (c_pw[:, :], DM)], ("acc0", "c_pw", "c_pp", "c_pT", "c_lw2", "c_lb2"))
        for tc in range(NTC):
            ak = "acc%d" % tc
            x2 = acc[:, tc, :]
            layer_norm(x2, ak, c_lw, c_lb, ("c_lw2", "c_lb2"), x2, ak, lst, lmv, lrs, "2")

            def f(act, x2=x2):
                return act.copy(out=x2b[:, :], in_=x2)
            S.op("act", f, reads=(ak,), writes=("x2b",))
            if stop_after == "ln2":
                continue
            for q4 in range(4):
                pb = q4 % 2
                pk = "ps%d" % pb

                def f(pe, q4=q4, pb=pb):
                    ins = None
                    for i in range(4):
                        dc = q4 * 4 + i
                        ins = pe.matmul(ps[pb][:, i * 128:(i + 1) * 128], x2b[:, dc * 128:(dc + 1) * 128], c_qdiag[:, 0, :], start=True, stop=True)
                    return ins
                S.op("pe", f, reads=("x2b", "c_qdiag"), writes=(pk,))

                def f(act, q4=q4, pb=pb):
                    return act.copy(out=x2T[:, q4 * 4:(q4 + 1) * 4, :], in_=ps[pb][:, :].rearrange("p (a t) -> p a t", a=4))
                S.op("act", f, reads=(pk,), writes=("x2T",))
            for dg in range(4):
                pb = 2 + dg % 2
                pk = "ps%d" % pb
                mm(ps[pb][:, :], [(c_pT[:, pc, tc * 128:(tc + 1) * 128], c_pp[:, pc, dg * 512:(dg + 1) * 512]) for pc in range(2)],
                   reads=("c_pT", "c_pp"), writes=(pk,))

                def f(act, pb=pb, dg=dg):
                    return act.copy(out=ebuf[:, dg * 512:(dg + 1) * 512], in_=ps[pb][:, :])
                S.op("act", f, reads=(pk,), writes=("ebuf",))

                def f(dve, dg=dg):
                    return dve.tensor_tensor(out=esq[:, :], in0=ebuf[:, dg * 512:(dg + 1) * 512], in1=ebuf[:, dg * 512:(dg + 1) * 512], op=ALU.mult)
                S.op("dve", f, reads=("ebuf",), writes=("esq",))

                def f(dve, dg=dg):
                    return dve.tensor_reduce(out=ess[:, dg:dg + 1], in_=esq[:, :], axis=mybir.AxisListType.X, op=ALU.add)
                S.op("dve", f, reads=("esq",), writes=("ess",))

            def f(dve):
                return dve.tensor_reduce(out=ers[:, :], in_=ess[:, :], axis=mybir.AxisListType.X, op=ALU.add)
            S.op("dve", f, reads=("ess",), writes=("ers",))

            def f(act):
                return act.activation(out=ers[:, :], in_=ers[:, :], func=AF.Sqrt, bias=c_eps[:, 0:1], scale=1.0 / DM)
            S.op("act", f, reads=("ers", "c_eps"), writes=("ers",))

            def f(dve):
                return dve.reciprocal(out=ers[:, :], in_=ers[:, :])
            S.op("dve", f, reads=("ers",), writes=("ers",))

            def f(dve):
                return dve.scalar_tensor_tensor(out=ebuf[:, :], in0=ebuf[:, :], scalar=ers[:, 0:1], in1=c_pw[:, :], op0=ALU.mult, op1=ALU.mult)
            S.op("dve", f, reads=("ebuf", "ers", "c_pw"), writes=("ebuf",))
            for dg in range(4):
                pb = 4 + dg % 2
                pk = "ps%d" % pb
                sl = pslots[dg]
                mm(ps[pb][:, :], [(x2T[:, dc, :], wslot[sl][:, dc, :]) for dc in range(NDC)],
                   reads=("x2T", "wslot%d" % sl), writes=(pk,))

                def f(act, pb=pb, dg=dg):
                    return act.activation(out=gbuf[:, dg * 512:(dg + 1) * 512], in_=ps[pb][:, :], func=AF.Sigmoid)
                S.op("act", f, reads=(pk,), writes=("gbuf",))

            def f(pool):
                return pool.tensor_tensor(out=gbuf[:, :], in0=gbuf[:, :], in1=ebuf[:, :], op=ALU.mult)
            S.op("pool", f, reads=("gbuf", "ebuf"), writes=("gbuf",))

            def f(pool, x2=x2):
                return pool.tensor_tensor(out=x2, in0=x2, in1=gbuf[:, :], op=ALU.add)
            S.op("pool", f, reads=("gbuf", ak), writes=(ak,))

            if stop_after == "ple":
                continue

            def f(eng, sem, tc=tc, x2=x2):
                eng.dma_start(out=out[tc * 128:(tc + 1) * 128, :], in_=x2).then_inc(sem, 16)
            S.dma("pool", f, 1, "outst", reads=(ak,))
        if stop_after in ("ln2", "ple"):
            return dump([(acc[:, 0, :], DM), (acc[:, 7, :], DM)], ("acc0", "acc7"))
        S.barrier()
        S.flush()


def _consts():
    h = np.arange(8, dtype=np.float64)
    log_gamma = np.log1p(-np.exp2(-5.0 - h))
    idx = np.arange(128, dtype=np.float64)
    diff = idx[None, :] - idx[:, None]
    decT = np.where(diff[:, None, :] >= 0, np.exp(np.maximum(diff, 0.0)[:, None, :] * log_gamma[None, :, None]), 0.0)
    kdec = np.exp((127.0 - idx)[:, None] * log_gamma[None, :])
    qdec = np.exp((idx[:, None] + 1.0) * log_gamma[None, :])
    chunk_dec = np.exp(128.0 * log_gamma)
    qdiag = np.zeros((128, 9, 128), np.float32)
    qdiag[:, 0, :] = np.eye(128)
    for hh in range(8):
        qdiag[:, 1 + hh, :] = np.diag(qdec[:, hh])
    tri = np.zeros((128, 2, 128), np.float32)
    tri[:, 0, :] = 1.0
    tri[:, 1, :] = (idx[:, None] < idx[None, :]).astype(np.float32)
    return dict(
        decT=decT.astype(np.float32), kdec=kdec.astype(np.float32),
        qdiag=qdiag.astype(ml_dtypes.bfloat16), ident_f=np.eye(128, dtype=np.float32),
        tri=tri.astype(ml_dtypes.bfloat16),
        iota_c=np.broadcast_to(np.arange(CAP, dtype=np.float32)[None, :], (128, CAP)).copy(),
        iota_p=np.stack([np.arange(128, dtype=np.float32), np.arange(128, dtype=np.float32) + 128], axis=1),
        pidx=np.broadcast_to(np.arange(128, dtype=np.float32)[:, None], (128, 128)).copy(),
    ), chunk_dec


def _rope_tables(pos0):
    inv = 10000.0 ** (-np.arange(0, 128, 2, dtype=np.float32) / 128)
    pos = pos0 + np.arange(T, dtype=np.float32)
    ang = pos[:, None] * inv[None, :]
    cos = np.cos(ang).astype(np.float32)
    sin = np.sin(ang).astype(np.float32)
    return cos, sin


def _prep_inputs(inp, n_exp_decl=None):
    f = lambda a: np.ascontiguousarray(np.asarray(a, dtype=np.float32))
    x = f(inp["x"])
    p = f(inp["p"])[0]
    cst, _ = _consts()
    rep = lambda v, n=128: np.ascontiguousarray(np.broadcast_to(f(v).reshape(1, -1), (n, f(v).size)))
    lruvec = np.zeros((128, 8, 8), np.float32)
    cw = f(inp["conv_w"])[0]
    for j in range(4):
        lruvec[:, :, j] = cw[j].reshape(8, 128).T
    lruvec[:, :, 4] = f(inp["conv_b"])[0].reshape(8, 128).T
    lruvec[:, :, 5] = f(inp["lru_ba"])[0].T
    lruvec[:, :, 6] = f(inp["lru_bx"])[0].T
    lruvec[:, :, 7] = f(inp["lru_lam"])[0].reshape(8, 128).T
    bgu = np.zeros((128, 2, NE, 16), np.float32)
    bgu[:, 0] = f(inp["b_gate"])[0].reshape(NE, 16, 128).transpose(2, 0, 1)
    bgu[:, 1] = f(inp["b_up"])[0].reshape(NE, 16, 128).transpose(2, 0, 1)
    shared = dict(
        w_in=f(inp["w_in"])[0], w_out=f(inp["w_out"])[0], w_ple_gate=f(inp["w_ple_gate"])[0], w_ple_proj=f(inp["w_ple_proj"])[0],
        w_router=f(inp["w_router"])[0], lru_wa=f(inp["lru_wa"])[0], lru_wx=f(inp["lru_wx"])[0],
        lruvec=lruvec, bgu=bgu, b_down=f(inp["b_down"])[0],
        rep_ln1w=rep(inp["ln1_w"]), rep_ln1b=rep(inp["ln1_b"]), rep_ln2w=rep(inp["ln2_w"]), rep_ln2b=rep(inp["ln2_b"]),
        rep_plew=rep(inp["ple_norm_w"]), rep_gnw=rep(inp["ret_gn_w"]), rep_br=rep(inp["b_router"]),
        **cst,
    )
    for nm in ("w_gate", "w_up", "w_down"):
        ne = NE if n_exp_decl is None else n_exp_decl
        w = f(inp[nm])[0][:ne]
        w = np.ascontiguousarray(w.reshape(ne, NDC, 128, 4, 512).transpose(0, 3, 2, 1, 4)).reshape(ne, 4, 128, 8192)
        for g in range((ne + EG - 1) // EG):
            shared["%s_%d" % (nm, g)] = w[g * EG:min((g + 1) * EG, ne)]
    scale_k = 128.0 ** -0.5
    in_maps = []
    for c in range(NCORES):
        b, hf = c // 2, c % 2
        own = slice(hf * T, (hf + 1) * T)
        m = dict(shared)
        m["xT_own"] = np.ascontiguousarray(x[b, own, :].T)
        m["xT_pre"] = np.ascontiguousarray(x[b, 0:T, :].T) if hf == 1 else np.zeros((DM, T), np.float32)
        m["x_own"] = np.ascontiguousarray(x[b, own, :])
        m["pT"] = np.ascontiguousarray(p[b, own, :].T)
        m["pf"] = np.full((128, 1), float(hf), np.float32)
        cos_o, sin_o = _rope_tables(float(hf * T))
        cos_p, sin_p = _rope_tables(0.0)
        lay = lambda a: a.reshape(NTC, 128, 64).transpose(1, 0, 2)
        m["rope_own"] = np.ascontiguousarray(np.stack([lay(cos_o), lay(sin_o), lay(cos_o * scale_k), lay(sin_o * scale_k)], axis=1))
        m["rope_pre"] = np.ascontiguousarray(np.stack([lay(cos_p * scale_k), lay(sin_p * scale_k)], axis=1))
        in_maps.append(m)
    return in_maps


_NC_CACHE = {}


def kernel(**inputs):
    in_maps = _prep_inputs(inputs)
    if "nc" not in _NC_CACHE:
        _NC_CACHE["nc"] = build()
    nc = _NC_CACHE["nc"]
    res = run_bass_kernel_spmd(nc, in_maps, core_ids=list(range(NCORES)))
    outp = np.zeros((4, 2048, DM), np.float32)
    for c in range(NCORES):
        b, hf = c // 2, c % 2
        outp[b, hf * T:(hf + 1) * T, :] = res.results[c]["out"]
    return outp
```
